# Optimizing a Trainium2 kernel written in Bass

```python
import math
import jax, jax.numpy as jnp
from jax import lax
import numpy as np

D_MODEL = 1024
BATCH = 8
SEQ = 4096
DEPTH = 1

D_MIX = D_MODEL
HEAD_DIM = 64
ATTN_HEADS = 8
ATTN_WIDTH = ATTN_HEADS * HEAD_DIM
DILATED_PATTERNS = ((128, 1), (512, 4), (2048, 16))
ROPE_THETA = 10000.0
SSD_HEADS = 8
SSD_HEAD_DIM = 64
SSD_WIDTH = SSD_HEADS * SSD_HEAD_DIM
SSD_GROUPS = 2
SSD_STATE = 128
SSD_CONV = 5
SSD_CHUNK = 128
SSD_CONV_CH = SSD_WIDTH + 2 * SSD_GROUPS * SSD_STATE
IN_PROJ_COLS = 3 * ATTN_WIDTH + SSD_WIDTH + SSD_CONV_CH + 2 * SSD_HEADS
N_EXPERTS = 32
TOP_K = 4
D_EXPERT = D_MODEL
SWIGLU_ALPHA = 1.702
SWIGLU_LIMIT = 7.0
DEEPNORM_ALPHA = (2.0 * DEPTH) ** 0.25
DEEPNORM_BETA = (8.0 * DEPTH) ** -0.25
NORM_EPS = 1e-5

kernel_name = "hymba_longnet_ssd_moe_deepnorm_encoder"


def layer_norm(x, w, b):
    xf = x.astype(jnp.float32)
    mu = jnp.mean(xf, axis=-1, keepdims=True)
    var = jnp.mean(jnp.square(xf - mu), axis=-1, keepdims=True)
    return (xf - mu) * lax.rsqrt(var + NORM_EPS) * w.astype(jnp.float32) + b.astype(jnp.float32)


def rms_norm(x, w):
    xf = x.astype(jnp.float32)
    return xf * lax.rsqrt(jnp.mean(jnp.square(xf), axis=-1, keepdims=True) + NORM_EPS) * w.astype(jnp.float32)


def rope_tables(s):
    pos = jnp.arange(s, dtype=jnp.float32)
    inv_freq = ROPE_THETA ** (-jnp.arange(0, HEAD_DIM, 2, dtype=jnp.float32) / HEAD_DIM)
    ang = pos[:, None] * inv_freq[None, :]
    return jnp.cos(ang), jnp.sin(ang)


def apply_rope(t, cos, sin):
    t1, t2 = jnp.split(t, 2, axis=-1)
    c = cos[None, :, None, :]
    s = sin[None, :, None, :]
    return jnp.concatenate([t1 * c - t2 * s, t2 * c + t1 * s], axis=-1)


def banded_window_attention(q, k, v, half):
    b, g, L, h, dh = q.shape
    blk = half
    nb = -(-L // blk)
    lp = nb * blk
    q = jnp.pad(q, ((0, 0), (0, 0), (0, lp - L), (0, 0), (0, 0)))
    kv_pad = ((0, 0), (0, 0), (blk, lp - L + blk), (0, 0), (0, 0))
    kb = jnp.pad(k, kv_pad).reshape(b, g, nb + 2, blk, h, dh)
    vb = jnp.pad(v, kv_pad).reshape(b, g, nb + 2, blk, h, dh)
    k_win = jnp.concatenate([kb[:, :, 0:nb], kb[:, :, 1:nb + 1], kb[:, :, 2:nb + 2]], axis=3)
    v_win = jnp.concatenate([vb[:, :, 0:nb], vb[:, :, 1:nb + 1], vb[:, :, 2:nb + 2]], axis=3)
    qb = q.reshape(b, g, nb, blk, h, dh)
    scores = jnp.einsum("bgnqhd,bgnkhd->bgnhqk", qb, k_win) * (dh ** -0.5)
    blocks = jnp.arange(nb)[:, None, None]
    qpos = blocks * blk + jnp.arange(blk)[None, :, None]
    kpos = (blocks - 1) * blk + jnp.arange(3 * blk)[None, None, :]
    valid = (jnp.abs(qpos - kpos) <= half) & (kpos >= 0) & (kpos < L)
    scores = jnp.where(valid[None, None, :, None], scores, -jnp.inf)
    mx = jnp.max(scores, axis=-1)
    p = jnp.exp(scores - mx[..., None])
    den = jnp.sum(p, axis=-1)
    num = jnp.einsum("bgnhqk,bgnkhd->bgnqhd", p, v_win).reshape(b, g, lp, h, dh)[:, :, :L]
    den = den.transpose(0, 1, 2, 4, 3).reshape(b, g, lp, h)[:, :, :L]
    mx = mx.transpose(0, 1, 2, 4, 3).reshape(b, g, lp, h)[:, :, :L]
    return num, den, mx


def dilated_attention(q, k, v):
    b, s, h, dh = q.shape
    nums, dens, mxs = [], [], []
    for window, dil in DILATED_PATTERNS:
        half = window // (2 * dil)
        L = s // dil
        qr = q.reshape(b, L, dil, h, dh).transpose(0, 2, 1, 3, 4)
        kr = k.reshape(b, L, dil, h, dh).transpose(0, 2, 1, 3, 4)
        vr = v.reshape(b, L, dil, h, dh).transpose(0, 2, 1, 3, 4)
        num, den, mx = banded_window_attention(qr, kr, vr, half)
        nums.append(num.transpose(0, 2, 1, 3, 4).reshape(b, s, h, dh))
        dens.append(den.transpose(0, 2, 1, 3).reshape(b, s, h))
        mxs.append(mx.transpose(0, 2, 1, 3).reshape(b, s, h))
    num_all = jnp.stack(nums)
    den_all = jnp.stack(dens)
    mx_all = jnp.stack(mxs)
    w = jnp.exp(mx_all - jnp.max(mx_all, axis=0, keepdims=True))
    return jnp.sum(w[..., None] * num_all, axis=0) / jnp.sum(w * den_all, axis=0)[..., None]


def ssd_chunked(xs, a, bm, cm):
    b, l, h, p = xs.shape
    n = bm.shape[-1]
    t = SSD_CHUNK
    c = l // t
    xs = xs.reshape(b, c, t, h, p)
    bm = bm.reshape(b, c, t, h, n)
    cm = cm.reshape(b, c, t, h, n)
    a = a.reshape(b, c, t, h).transpose(0, 3, 1, 2)
    a_cs = jnp.cumsum(a, axis=-1)
    seg = a_cs[..., :, None] - a_cs[..., None, :]
    tril = jnp.tril(jnp.ones((t, t), dtype=bool))
    lmat = jnp.exp(jnp.where(tril, seg, -jnp.inf))
    cb = jnp.einsum("bclhn,bcshn->bhcls", cm, bm)
    y_diag = jnp.einsum("bhcls,bcshp->bclhp", cb * lmat, xs)
    decay_states = jnp.exp(a_cs[..., -1:] - a_cs)
    states = jnp.einsum("bclhn,bhcl,bclhp->bchpn", bm, decay_states, xs)
    chunk_decay = jnp.exp(a_cs[..., -1])

    def step(state, inp):
        st, dec = inp
        return state * dec[..., None, None] + st, state

    init = jnp.zeros((b, h, p, n), dtype=jnp.float32)
    _, prev = lax.scan(step, init, (states.transpose(1, 0, 2, 3, 4), chunk_decay.transpose(2, 0, 1)))
    prev = prev.transpose(1, 0, 2, 3, 4)
    y_off = jnp.einsum("bclhn,bchpn,bhcl->bclhp", cm, prev, jnp.exp(a_cs))
    return (y_diag + y_off).reshape(b, l, h, p)


def bidirectional_ssd(z, xbc, dt_raw, conv_w, conv_b, dt_bias_fwd, a_log_fwd,
                      dt_bias_bwd, a_log_bwd, d_skip, norm_w):
    b, s, _ = z.shape
    xbc = lax.conv_general_dilated(
        xbc.astype(jnp.float32), conv_w.astype(jnp.float32)[:, None, :], (1,),
        [(SSD_CONV // 2, SSD_CONV // 2)], dimension_numbers=("NWC", "WIO", "NWC"),
        feature_group_count=SSD_CONV_CH) + conv_b.astype(jnp.float32)
    xbc = jax.nn.silu(xbc)
    gn = SSD_GROUPS * SSD_STATE
    xs = xbc[..., :SSD_WIDTH].reshape(b, s, SSD_HEADS, SSD_HEAD_DIM)
    bm = xbc[..., SSD_WIDTH:SSD_WIDTH + gn].reshape(b, s, SSD_GROUPS, SSD_STATE)
    cm = xbc[..., SSD_WIDTH + gn:].reshape(b, s, SSD_GROUPS, SSD_STATE)
    heads_per_group = SSD_HEADS // SSD_GROUPS
    bm = jnp.repeat(bm, heads_per_group, axis=2)
    cm = jnp.repeat(cm, heads_per_group, axis=2)
    dt_raw = dt_raw.astype(jnp.float32)
    dt_f = jax.nn.softplus(dt_raw[..., :SSD_HEADS] + dt_bias_fwd.astype(jnp.float32))
    dt_b = jax.nn.softplus(dt_raw[..., SSD_HEADS:] + dt_bias_bwd.astype(jnp.float32))
    a_f = -jnp.exp(a_log_fwd.astype(jnp.float32))
    a_b = -jnp.exp(a_log_bwd.astype(jnp.float32))
    y_f = ssd_chunked(xs * dt_f[..., None], dt_f * a_f, bm, cm)
    flip = lambda t: jnp.flip(t, axis=1)
    y_b = flip(ssd_chunked(flip(xs * dt_b[..., None]), flip(dt_b * a_b), flip(bm), flip(cm)))
    y = y_f + y_b + d_skip.astype(jnp.float32)[:, None] * xs
    y = y.reshape(b, s, SSD_WIDTH) * jax.nn.silu(z.astype(jnp.float32))
    yg = y.reshape(b, s, SSD_GROUPS, SSD_WIDTH // SSD_GROUPS)
    yg = yg * lax.rsqrt(jnp.mean(jnp.square(yg), axis=-1, keepdims=True) + NORM_EPS)
    return yg.reshape(b, s, SSD_WIDTH) * norm_w.astype(jnp.float32)


def moe_ffn(x, router_w, router_b, w_gate, b_gate, w_up, b_up, w_down, b_down):
    b, s, d = x.shape
    xt = x.reshape(b * s, d)
    logits = (xt @ router_w + router_b).astype(jnp.float32)
    top_vals, top_idx = lax.top_k(logits, TOP_K)
    gates = jax.nn.softmax(top_vals, axis=-1)
    combine = jnp.sum(jax.nn.one_hot(top_idx, N_EXPERTS, dtype=jnp.float32) * gates[..., None], axis=1)
    out = jnp.zeros((b * s, d), dtype=jnp.float32)
    for e in range(N_EXPERTS):
        g = (xt @ w_gate[e] + b_gate[e]).astype(jnp.float32)
        u = (xt @ w_up[e] + b_up[e]).astype(jnp.float32)
        g = jnp.minimum(g, SWIGLU_LIMIT)
        u = jnp.clip(u, -SWIGLU_LIMIT, SWIGLU_LIMIT)
        act = (u + 1.0) * g * jax.nn.sigmoid(SWIGLU_ALPHA * g)
        y = act.astype(x.dtype) @ w_down[e] + b_down[e]
        out = out + combine[:, e:e + 1] * y.astype(jnp.float32)
    return out.reshape(b, s, d)


def setup_inputs(seed: int = 0) -> dict:
    key = jax.random.key(seed)
    ks = jax.random.split(key, 24)
    f32 = jnp.float32
    nrm = lambda k, shape, scale: jax.random.normal(k, shape, f32) * scale
    dt = jnp.exp(jax.random.uniform(ks[4], (DEPTH, SSD_HEADS), f32, math.log(1e-3), math.log(1e-1)))
    dt2 = jnp.exp(jax.random.uniform(ks[6], (DEPTH, SSD_HEADS), f32, math.log(1e-3), math.log(1e-1)))
    return {
        "x": jax.random.normal(ks[0], (BATCH, SEQ, D_MODEL), f32),
        "w_in": nrm(ks[1], (DEPTH, D_MODEL, IN_PROJ_COLS), D_MODEL ** -0.5),
        "attn_norm_w": 1.0 + nrm(ks[2], (DEPTH, ATTN_WIDTH), 0.02),
        "conv_w": nrm(ks[3], (DEPTH, SSD_CONV, SSD_CONV_CH), SSD_CONV ** -0.5),
        "conv_b": nrm(ks[21], (DEPTH, SSD_CONV_CH), 0.02),
        "dt_bias_fwd": dt + jnp.log(-jnp.expm1(-dt)),
        "a_log_fwd": jnp.log(jax.random.uniform(ks[5], (DEPTH, SSD_HEADS), f32, 1.0, 16.0)),
        "dt_bias_bwd": dt2 + jnp.log(-jnp.expm1(-dt2)),
        "a_log_bwd": jnp.log(jax.random.uniform(ks[7], (DEPTH, SSD_HEADS), f32, 1.0, 16.0)),
        "d_skip": 1.0 + nrm(ks[8], (DEPTH, SSD_HEADS), 0.1),
        "ssd_norm_w": 1.0 + nrm(ks[9], (DEPTH, SSD_WIDTH), 0.02),
        "w_out": nrm(ks[10], (DEPTH, D_MIX, D_MODEL), DEEPNORM_BETA * D_MIX ** -0.5),
        "ln1_w": 1.0 + nrm(ks[11], (DEPTH, D_MODEL), 0.02),
        "ln1_b": nrm(ks[12], (DEPTH, D_MODEL), 0.02),
        "router_w": nrm(ks[13], (DEPTH, D_MODEL, N_EXPERTS), D_MODEL ** -0.5),
        "router_b": nrm(ks[14], (DEPTH, N_EXPERTS), 0.01),
        "w_gate": nrm(ks[15], (DEPTH, N_EXPERTS, D_MODEL, D_EXPERT), D_MODEL ** -0.5),
        "b_gate": nrm(ks[16], (DEPTH, N_EXPERTS, D_EXPERT), 0.02),
        "w_up": nrm(ks[17], (DEPTH, N_EXPERTS, D_MODEL, D_EXPERT), D_MODEL ** -0.5),
        "b_up": nrm(ks[18], (DEPTH, N_EXPERTS, D_EXPERT), 0.02),
        "w_down": nrm(ks[19], (DEPTH, N_EXPERTS, D_EXPERT, D_MODEL), DEEPNORM_BETA * D_EXPERT ** -0.5),
        "b_down": nrm(ks[20], (DEPTH, N_EXPERTS, D_MODEL), 0.02),
        "ln2_w": 1.0 + nrm(ks[22], (DEPTH, D_MODEL), 0.02),
        "ln2_b": nrm(ks[23], (DEPTH, D_MODEL), 0.02),
    }


def reference(x, w_in, attn_norm_w, conv_w, conv_b, dt_bias_fwd, a_log_fwd, dt_bias_bwd,
              a_log_bwd, d_skip, ssd_norm_w, w_out, ln1_w, ln1_b, router_w, router_b,
              w_gate, b_gate, w_up, b_up, w_down, b_down, ln2_w, ln2_b):
    b, s, _ = x.shape
    cos, sin = rope_tables(s)
    h = x
    q_end = ATTN_WIDTH
    k_end = 2 * ATTN_WIDTH
    v_end = 3 * ATTN_WIDTH
    z_end = v_end + SSD_WIDTH
    xbc_end = z_end + SSD_CONV_CH
    for layer in range(DEPTH):
        proj = h @ w_in[layer]
        q = proj[..., :q_end].reshape(b, s, ATTN_HEADS, HEAD_DIM).astype(jnp.float32)
        k = proj[..., q_end:k_end].reshape(b, s, ATTN_HEADS, HEAD_DIM).astype(jnp.float32)
        v = proj[..., k_end:v_end].reshape(b, s, ATTN_HEADS, HEAD_DIM).astype(jnp.float32)
        attn = dilated_attention(apply_rope(q, cos, sin), apply_rope(k, cos, sin), v)
        attn = rms_norm(attn.reshape(b, s, ATTN_WIDTH), attn_norm_w[layer])
        ssd = bidirectional_ssd(proj[..., v_end:z_end], proj[..., z_end:xbc_end], proj[..., xbc_end:],
                                conv_w[layer], conv_b[layer], dt_bias_fwd[layer], a_log_fwd[layer],
                                dt_bias_bwd[layer], a_log_bwd[layer], d_skip[layer], ssd_norm_w[layer])
        mix = jnp.concatenate([attn, ssd], axis=-1).astype(h.dtype) @ w_out[layer]
        h1 = layer_norm(DEEPNORM_ALPHA * h.astype(jnp.float32) + mix.astype(jnp.float32),
                        ln1_w[layer], ln1_b[layer]).astype(x.dtype)
        ffn = moe_ffn(h1, router_w[layer], router_b[layer], w_gate[layer], b_gate[layer],
                      w_up[layer], b_up[layer], w_down[layer], b_down[layer])
        h = layer_norm(DEEPNORM_ALPHA * h1.astype(jnp.float32) + ffn,
                       ln2_w[layer], ln2_b[layer]).astype(x.dtype)
    return h
```

```python
import contextlib
import numpy as np
import ml_dtypes
import concourse.bass as bass
import concourse.mybir as mybir
from concourse.bass_utils import run_bass_kernel_spmd

F32 = mybir.dt.float32
BF16 = mybir.dt.bfloat16
U32 = mybir.dt.uint32
I32 = mybir.dt.int32
AF = mybir.ActivationFunctionType
ALU = mybir.AluOpType

S_TOK = 4096
D = 1024
NCH = 32
WCOLS = 3072 + 1040
N_EXP = 32
CAP = 1024


class Sched:
    COMPUTE = ("pe", "act", "dve", "pool")

    def __init__(self, nc, es, n_ring=12):
        self.nc = nc
        self.streams = {e: [] for e in ("pe", "act", "dve", "pool", "sp")}
        self.sem = {}
        for e in self.COMPUTE:
            self.sem[e] = es.enter_context(nc.semaphore("sem_" + e))
        self.cnt = {e: 0 for e in self.COMPUTE}
        self.ring = {}
        self.ring_i = {}
        self.ring_tot = {}
        for q, n in (("sp", n_ring), ("pool", 8), ("act", 6), ("dve", 6)):
            self.ring[q] = [es.enter_context(nc.semaphore(f"dq_{q}_{i}")) for i in range(n)]
            self.ring_i[q] = 0
        self.last_writer = {}
        self.readers = {}
        self.waited = {e: {} for e in self.streams}
        self.n_ins = 0

    def _deps(self, reads, writes):
        deps = {}

        def add(tok):
            k, v, e = tok
            if k not in deps or deps[k][0] < v:
                deps[k] = (v, e)
        for r in reads:
            if r in self.last_writer:
                add(self.last_writer[r])
        for w in writes:
            if w in self.last_writer:
                add(self.last_writer[w])
            for k, (v, e) in self.readers.get(w, {}).items():
                add((k, v, e))
        return deps

    def _emit_waits(self, eng, deps):
        for k, (v, src) in deps.items():
            if src == eng and eng == "pe":
                continue
            if self.waited[eng].get(k, 0) >= v:
                continue
            self.waited[eng][k] = v
            self.streams[eng].append(("wait", k, v))

    def _commit(self, tok, reads, writes):
        k, v, e = tok
        for w in writes:
            self.last_writer[w] = tok
            self.readers[w] = {}
        for r in reads:
            d = self.readers.setdefault(r, {})
            if k not in d or d[k][0] < v:
                d[k] = (v, e)

    def cond_begin(self, flag_ap):
        import copy
        self._cond = dict(n={e: 0 for e in self.streams}, rings={e: {} for e in self.streams},
                          snap=copy.deepcopy(self.waited))
        for e in self.streams:
            self.streams[e].append(("cond_begin", flag_ap))

    def cond_end(self):
        c = self._cond
        for e in self.streams:
            self.streams[e].append(("cond_end", c["n"][e], c["rings"][e]))
        self.waited = c["snap"]
        self._cond = None

    def op(self, eng, fn, reads=(), writes=()):
        deps = self._deps(reads, writes)
        self._emit_waits(eng, deps)
        if getattr(self, "_cond", None):
            self._cond["n"][eng] += 1
        self.cnt[eng] += 1
        key = "c_" + eng
        tok = (key, self.cnt[eng], eng)
        self.streams[eng].append(("ins", fn, self.sem[eng], 1))
        self._commit(tok, reads, writes)
        self.n_ins += 1

    def dma(self, fn, reads=(), writes=(), q="sp"):
        deps = self._deps(reads, writes)
        self._emit_waits(q, deps)
        i = self.ring_i[q]
        self.ring_i[q] = (i + 1) % len(self.ring[q])
        key = f"d_{q}_{i}"
        tot = self.ring_tot.get(key, 0)
        if tot > 0 and self.waited[q].get(key, 0) < tot:
            self.waited[q][key] = tot
            self.streams[q].append(("wait", key, tot))
        tot += 16
        self.ring_tot[key] = tot
        if getattr(self, "_cond", None):
            ent = self._cond["rings"][q].setdefault(key, [0, tot - 16])
            ent[0] += 1
        tok = (key, tot, "dma")
        self.streams[q].append(("ins", fn, self.ring[q][i], 16))
        self._commit(tok, reads, writes)
        self.n_ins += 1

    def _semh(self, key):
        if key.startswith("c_"):
            return self.sem[key[2:]]
        _, q, i = key.split("_")
        return self.ring[q][int(i)]

    def finish(self):
        for key, tot in self.ring_tot.items():
            if self.waited["sp"].get(key, 0) < tot:
                self.streams["sp"].append(("wait", key, tot))
                self.waited["sp"][key] = tot
        for e in self.COMPUTE:
            if self.cnt[e] and self.waited["sp"].get("c_" + e, 0) < self.cnt[e]:
                self.streams["sp"].append(("wait", "c_" + e, self.cnt[e]))

    def barrier(self):
        for eng in self.streams:
            for key, tot in self.ring_tot.items():
                if self.waited[eng].get(key, 0) < tot:
                    self.streams[eng].append(("wait", key, tot))
                    self.waited[eng][key] = tot
            for e in self.COMPUTE:
                if e != eng and self.cnt[e] and self.waited[eng].get("c_" + e, 0) < self.cnt[e]:
                    self.streams[eng].append(("wait", "c_" + e, self.cnt[e]))
                    self.waited[eng]["c_" + e] = self.cnt[e]

    def bc(self, e):
        if getattr(self, "_bc", None) is None:
            self._bc = e.to_reg(NROWS - 1)
        return self._bc

    def end_phase(self):
        self._bc_prev = getattr(self, "_bc", None)
        print("phase end n_ins", self.n_ins, {k: len(v) for k, v in self.streams.items()}, flush=True)
        self.barrier()
        self.emit()
        self._bc = None
        self.streams = {e: [] for e in self.streams}
        self.last_writer = {}
        self.readers = {}

    def emit(self):
        nc = self.nc
        with nc.Block() as block:
            def mk(name):
                def body(eng):
                    items = self.streams[name]
                    reg = None
                    guard = None
                    i = 0
                    while i < len(items):
                        it = items[i]
                        if it[0] == "wait":
                            eng.wait_ge(self._semh(it[1]), it[2])
                        elif it[0] == "ins":
                            it[1](eng).then_inc(it[2], it[3])
                        elif it[0] == "cond_begin":
                            j = i + 1
                            while items[j][0] != "cond_end":
                                j += 1
                            if j == i + 1:
                                i = j + 1
                                continue
                            if reg is None:
                                self._uid = getattr(self, "_uid", 0) + 1
                                reg = eng.alloc_register(f"cr_{name}_{self._uid}")
                            eng.reg_load(reg, it[1])
                            guard = eng.If_ne(reg, 0)
                            guard.__enter__()
                        elif it[0] == "cond_end":
                            guard.__exit__(None, None, None)
                            g2 = eng.Else()
                            g2.__enter__()
                            if it[1] and name in self.sem:
                                left = it[1]
                                while left > 0:
                                    k_ = min(16, left)
                                    eng.drain().then_inc(self.sem[name], k_)
                                    left -= k_
                            for key, (cnt_, before_) in it[2].items():
                                if before_ > 0:
                                    eng.wait_ge(self._semh(key), before_)
                                for _ in range(cnt_):
                                    eng.drain().then_inc(self._semh(key), 16)
                            g2.__exit__(None, None, None)
                            guard = None
                        i += 1
                return body
            if self.streams["sp"]:
                block.sync(mk("sp"))
            if self.streams["pe"]:
                block.tensor(mk("pe"))
            if self.streams["act"]:
                block.scalar(mk("act"))
            if self.streams["dve"]:
                block.vector(mk("dve"))
            if self.streams["pool"]:
                block.gpsimd(mk("pool"))


def host_consts():
    c = {}
    pos = np.arange(S_TOK, dtype=np.float32)
    inv = (np.float32(10000.0) ** (-np.arange(0, 64, 2, dtype=np.float32) / np.float32(64))).astype(np.float32)
    ang = (pos[:, None] * inv[None, :]).astype(np.float32)
    cos = np.cos(ang).astype(np.float32).T
    sin = np.sin(ang).astype(np.float32).T
    cosT = np.concatenate([cos, cos, cos, cos], 0)
    sinT = np.concatenate([-sin, sin, -sin, sin], 0)
    kk = np.arange(128)[:, None]
    nn = np.arange(256)[None, :]
    c["negmask"] = np.where((kk <= nn) & (nn <= kk + 128), 0.0, -30000.0).astype(ml_dtypes.bfloat16)
    c["ident_bf"] = np.eye(128, dtype=np.float32).astype(ml_dtypes.bfloat16)
    sel = np.zeros((65, 64), np.float32)
    sel[64, :] = 1.0
    c["sel65"] = sel
    t_ = np.arange(128)
    c["c_SL"] = (t_[:, None] > t_[None, :]).astype(np.float32)
    c["c_UI"] = (t_[:, None] <= t_[None, :]).astype(np.float32)
    c["c_SU"] = (t_[:, None] < t_[None, :]).astype(np.float32)
    c["c_LI"] = (t_[:, None] >= t_[None, :]).astype(np.float32)
    c["c_ones"] = np.ones((128, 128), np.float32)
    c["cosT"] = np.ascontiguousarray(cosT)
    c["sinT"] = np.ascontiguousarray(sinT)
    return c


def phase_attn(C):
    nc, S, psum = C["nc"], C["S"], C["psum"]
    qT_d, kT_d, v_d = C["qT_d"], C["kT_d"], C["v_d"]
    negmask_d = C["din"]("negmask", [128, 256], BF16)
    ident_d = C["din"]("ident_bf", [128, 128], BF16)
    sel_d = C["din"]("sel65", [65, 64], F32)
    attnT_d = C["dscr"]("attnT_d", [512, S_TOK], BF16)
    C["attnT_d"] = attnT_d
    C["ident_d"] = ident_d
    with contextlib.ExitStack() as ph:
        def sba(name, shape, dt):
            return ph.enter_context(nc.sbuf_tensor(name, list(shape), dt))
        negmask = sba("negmask_sb", [128, 256], BF16)
        ident = sba("ident_sb", [128, 128], BF16)
        sel = sba("sel_sb", [65, 64], F32)
        V = [sba(f"V{i}", [128, 32, 4, 65], BF16) for i in range(3)]
        vst = [sba(f"vst{i}", [128, 8, 256], BF16) for i in range(2)]
        acc = [sba(f"acc{i}", [128, S_TOK], F32) for i in range(2)]
        qh = [sba(f"qh{i}", [128, S_TOK], BF16) for i in range(2)]
        kh = [sba(f"kh{i}", [128, S_TOK], BF16) for i in range(2)]
        pT = [sba(f"pT{i}", [128, 256], BF16) for i in range(4)]
        rec = [sba(f"rec{i}", [64, 512], F32) for i in range(2)]
        outb = [sba(f"aob{i}", [64, 512], BF16) for i in range(2)]
        S.dma(lambda e: e.dma_start(out=negmask[:], in_=negmask_d), writes=["negmask"])
        S.dma(lambda e: e.dma_start(out=ident[:], in_=ident_d), writes=["ident"])
        S.dma(lambda e: e.dma_start(out=sel[:], in_=sel_d), writes=["sel"])
        for i in range(3):
            S.op("pool", lambda e, i=i: e.memset(V[i][:], 1.0), writes=[f"V{i}"])
        PAT = (1, 4, 16)
        scb = [psum[0], psum[1], C["psT"][:, :].bitcast(F32)]
        out_banks = (2, 3, 4)
        cnt = dict(vs=0, sc=0, pt=0, ob=0, nb=0)

        for i in range(2):
            S.op("pool", lambda e, i=i: e.memset(kh[i][:], 0.0), writes=[f"kh{i}"])

        def load_qk(h):
            b = h % 2
            if h % 2 == 0:
                p = h // 2
                S.dma(lambda e: e.dma_start(out=qh[p % 2][:], in_=qT_d[p * 128:(p + 1) * 128, :]), writes=[f"qh{p % 2}"])
            S.dma(lambda e: e.dma_start(out=kh[b][b * 64:(b + 1) * 64, :], in_=kT_d[h * 64:(h + 1) * 64, :]), writes=[f"kh{b}"])

        def load_V(hh):
            for pi, r in enumerate(PAT):
                n_t = 32 // r
                view4 = v_d.rearrange("(u k r) c -> k r u c", r=r, k=128)
                for gi in range(4):
                    b = cnt["vs"] % 2
                    cnt["vs"] += 1
                    cs = slice(hh * 256, (hh + 1) * 256)
                    if r == 1:
                        src = view4[:, 0, 8 * gi:8 * gi + 8, cs]
                        dst = vst[b][:]
                    elif r == 4:
                        src = view4[:, gi, :, cs]
                        dst = vst[b][:]
                    else:
                        src = None
                        for j in range(4):
                            srcj = view4[:, 4 * gi + j, :, cs]
                            dstj = vst[b][:, 2 * j:2 * j + 2, :]
                            S.dma(lambda e, src=srcj, dst=dstj: e.dma_start(out=dst, in_=src), writes=[f"vst{b}"])
                    if src is not None:
                        S.dma(lambda e, src=src, dst=dst: e.dma_start(out=dst, in_=src), writes=[f"vst{b}"])
                    eng = ("pool", "dve")[gi % 2]
                    S.op(eng, lambda e, pi=pi, gi=gi, b=b: e.tensor_copy(
                        out=V[pi][:, 8 * gi:8 * gi + 8, :, 0:64],
                        in_=vst[b][:].rearrange("p t (h d) -> p t h d", h=4)),
                        reads=[f"vst{b}"], writes=[f"V{pi}"])

        def head(h):
            hb = h % 2
            hl = h % 4
            ab = h % 2
            accr = f"acc{ab}"
            tiles = []
            for pi, r in enumerate(PAT):
                L = S_TOK // r
                n_t = L // 128
                for rho in range(r):
                    nbanks = (L + 64 + 511) // 512
                    base = cnt["nb"]
                    cnt["nb"] += nbanks
                    for u in range(n_t):
                        tiles.append((pi, r, rho, u, n_t, L, base, nbanks))

            def score(tl):
                pi, r, rho, u, n_t, L, base, nbanks = tl
                m_lo = max(0, 128 * u - 64)
                m_hi = min(L, 128 * u + 192)
                N = m_hi - m_lo
                c0 = m_lo - (128 * u - 64)
                sl_ = cnt["sc"] % 3
                cnt["sc"] += 1
                sbk, so = sl_, 0
                pb_ = cnt["pt"] % 4
                cnt["pt"] += 1
                k0 = rho + r * 128 * u
                kc = kh[hb][:, k0:k0 + r * 127 + 1:r]
                qb_ = (h // 2) % 2
                qc = qh[qb_][:, rho + r * m_lo:rho + r * (m_hi - 1) + 1:r]
                S.op("pe", lambda e: e.matmul(scb[sbk][:, so:so + N], lhsT=kc, rhs=qc, start=True, stop=False),
                     reads=[f"qh{qb_}", f"kh{hb}"], writes=[f"sc{sl_}"])
                S.op("pe", lambda e: e.matmul(scb[sbk][:, so:so + N], lhsT=ident[:], rhs=negmask[:, c0:c0 + N],
                                               start=False, stop=True),
                     reads=["ident", "negmask"], writes=[f"sc{sl_}"])
                S.op("act", lambda e: e.activation(out=pT[pb_][:, 0:N], in_=scb[sbk][:, so:so + N], func=AF.Exp, scale=0.125),
                     reads=[f"sc{sl_}"], writes=[f"pT{pb_}"])
                return (pb_, m_lo, m_hi)

            def pv(tl, info):
                pi, r, rho, u, n_t, L, base, nbanks = tl
                pb_, m_lo, m_hi = info

                def phys(gb):
                    return out_banks[(base + gb) % 3]
                ti = rho * n_t + u
                vl = V[pi][:, ti, hl, :]
                nA = (128 * u + 64) - m_lo
                cA = m_lo + 64
                gA, colA = cA // 512, cA % 512
                pA = phys(gA)
                S.op("pe", lambda e: e.matmul(psum[pA][0:65, colA:colA + nA], lhsT=vl, rhs=pT[pb_][:, 0:nA],
                                               start=(u == 0), stop=True),
                     reads=[f"V{pi}", f"pT{pb_}"], writes=[f"ps{pA}"])
                nB = m_hi - (128 * u + 64)
                cB = 128 * (u + 1)
                gB, colB = cB // 512, cB % 512
                pB = phys(gB)
                lastu = (u == n_t - 1)
                S.op("pe", lambda e: e.matmul(psum[pB][0:65, colB:colB + nB], lhsT=vl, rhs=pT[pb_][:, nA:nA + nB],
                                               start=True, stop=lastu),
                     reads=[f"V{pi}", f"pT{pb_}"], writes=[f"ps{pB}"])
                for gb in range(nbanks):
                    if min(4 * gb + 3, n_t - 1) == u:
                        ma = max(0, 512 * gb - 64)
                        mb = min(L, 512 * gb + 448)
                        ca = ma + 64 - 512 * gb
                        n = mb - ma
                        asl = acc[ab][0:65, rho + r * ma:rho + r * (mb - 1) + 1:r]
                        pg_ = phys(gb)
                        psl = psum[pg_][0:65, ca:ca + n]
                        if pi == 0:
                            S.op("dve", lambda e, asl=asl, psl=psl: e.tensor_copy(out=asl, in_=psl),
                                 reads=[f"ps{pg_}"], writes=[accr])
                        else:
                            S.op("dve", lambda e, asl=asl, psl=psl: e.tensor_tensor(out=asl, in0=asl, in1=psl, op=ALU.add),
                                 reads=[f"ps{pg_}", accr], writes=[accr])

            LA = 2
            infos = []
            for k_ in range(min(LA, len(tiles))):
                infos.append(score(tiles[k_]))
            for k_ in range(len(tiles)):
                if k_ + LA < len(tiles):
                    infos.append(score(tiles[k_ + LA]))
                pv(tiles[k_], infos[k_])
            for c in range(8):
                db = 5 + (c % 2)
                rb = c % 2
                S.op("pe", lambda e, c=c, db=db: e.matmul(psum[db][0:64, :], lhsT=sel[:], rhs=acc[ab][0:65, c * 512:(c + 1) * 512],
                                                           start=True, stop=True),
                     reads=[accr, "sel"], writes=[f"ps{db}"])
                S.op("dve", lambda e, db=db, rb=rb: e.reciprocal(out=rec[rb][:], in_=psum[db][0:64, :]),
                     reads=[f"ps{db}"], writes=[f"rec{rb}"])
                S.op("pool", lambda e, c=c, rb=rb: e.tensor_tensor(out=outb[rb][:], in0=acc[ab][0:64, c * 512:(c + 1) * 512],
                                                                     in1=rec[rb][:], op=ALU.mult),
                     reads=[accr, f"rec{rb}"], writes=[f"aob{rb}"])
                S.dma(lambda e, c=c, rb=rb: e.dma_start(out=attnT_d[h * 64:(h + 1) * 64, c * 512:(c + 1) * 512], in_=outb[rb][:]),
                      reads=[f"aob{rb}"], writes=[])

        load_qk(0)
        for hh in range(2):
            load_V(hh)
            for hl_ in range(4):
                h = hh * 4 + hl_
                if h + 1 < 8:
                    load_qk(h + 1)
                head(h)
        S.end_phase()


def X(S, eng, meth, reads, writes, *a, **kw):
    S.op(eng, lambda e: getattr(e, meth)(*a, **kw), reads, writes)


def DM(S, out, in_, reads=(), writes=(), q="sp"):
    S.dma(lambda e: e.dma_start(out=out, in_=in_), reads, writes, q)


def phase_conv(C):
    nc, S, psum = C["nc"], C["S"], C["psum"]
    cwT_d = C["din"]("conv_wT", [128, 40])
    cbT_d = C["din"]("conv_bT", [128, 8])
    cbrow_d = C["din"]("conv_b_row", [1, 1024])
    bcT_d = C["dscr"]("bcT_d", [512, S_TOK], BF16)
    xsB_d = C["dscr"]("xsB_d", [S_TOK, 768], BF16)
    C["bcT_d"], C["xsB_d"] = bcT_d, xsB_d
    with contextlib.ExitStack() as ph:
        def sba(name, shape, dt):
            return ph.enter_context(nc.sbuf_tensor(name, list(shape), dt))
        xpre = sba("xpre", [128, 8, S_TOK + 4], BF16)
        dg = sba("dg", [128, 40, 128], BF16)
        ident = sba("ident_c", [128, 128], BF16)
        cwT = sba("cwT", [128, 40], F32)
        cbT = sba("cbT", [128, 8], F32)
        cbrow = sba("cbrow", [1, 1024], F32)
        cbrow_bf = sba("cbrow_bf", [1, 1024], BF16)
        ones1 = sba("ones1", [1, 128], BF16)
        ob = [sba(f"cob{i}", [128, 512], BF16) for i in range(3)]
        DM(S, ident[:], C["ident_d"], writes=["ident_c"])
        DM(S, cwT[:], cwT_d, writes=["cwT"])
        DM(S, cbT[:], cbT_d, writes=["cbT"])
        DM(S, cbrow[:], cbrow_d, writes=["cbrow"])
        xv = C["xbcT_d"].rearrange("(c p) t -> p c t", p=128)
        for j in range(8):
            DM(S, xpre[:, j, :], xv[:, j, :], writes=[f"xpre{j}"])
        X(S, "dve", "tensor_copy", ["cbrow"], ["cbrow_bf"], out=cbrow_bf[:], in_=cbrow[:])
        X(S, "pool", "memset", [], ["ones1"], ones1[:], 1.0)
        for jk in range(40):
            X(S, "dve", "tensor_scalar", ["ident_c", "cwT"], ["dg"], out=dg[:, jk, :], in0=ident[:],
              scalar1=cwT[:, jk:jk + 1], scalar2=None, op0=ALU.mult)
        bi = 0
        oi = 0
        for j in range(4, 8):
            for tc in range(8):
                b = bi % 2
                bi += 1
                for k in range(5):
                    X(S, "pe", "matmul", [f"xpre{j}", "dg"], [f"ps{b}"], psum[b][:, :], lhsT=dg[:, j * 5 + k, :],
                      rhs=xpre[:, j, tc * 512 + k:tc * 512 + k + 512], start=(k == 0), stop=(k == 4))
                o = oi % 3
                oi += 1
                X(S, "act", "activation", [f"ps{b}", "cbT"], [f"cob{o}"], out=ob[o][:], in_=psum[b][:, :], func=AF.Silu,
                  bias=cbT[:, j:j + 1])
                DM(S, bcT_d[(j - 4) * 128:(j - 3) * 128, tc * 512:(tc + 1) * 512], ob[o][:], reads=[f"cob{o}"])
        for tg in range(32):
            bA = 2 + (tg % 2) * 2
            bB = bA + 1
            for j in range(6):
                bank = bA if j < 4 else bB
                col = (j % 4) * 128
                for k in range(5):
                    X(S, "pe", "matmul", [f"xpre{j}", "dg"], [f"ps{bank}"], psum[bank][:, col:col + 128],
                      lhsT=xpre[:, j, tg * 128 + k:tg * 128 + k + 128], rhs=dg[:, j * 5 + k, :], start=(k == 0), stop=False)
                X(S, "pe", "matmul", ["ones1", "cbrow_bf"], [f"ps{bank}"], psum[bank][:, col:col + 128],
                  lhsT=ones1[0:1, :], rhs=cbrow_bf[0:1, j * 128:(j + 1) * 128], start=False, stop=True)
            o = oi % 3
            oi += 1
            X(S, "act", "activation", [f"ps{bA}"], [f"cob{o}"], out=ob[o][:], in_=psum[bA][:, :], func=AF.Silu)
            DM(S, xsB_d[tg * 128:(tg + 1) * 128, 0:512], ob[o][:], reads=[f"cob{o}"])
            o = oi % 3
            oi += 1
            X(S, "act", "activation", [f"ps{bB}"], [f"cob{o}"], out=ob[o][:, 0:256], in_=psum[bB][:, 0:256], func=AF.Silu)
            DM(S, xsB_d[tg * 128:(tg + 1) * 128, 512:768], ob[o][:, 0:256], reads=[f"cob{o}"])
        sz_d = C["dscr"]("sz_d", [S_TOK, 512], F32)
        C["sz_d"] = sz_d
        zin = [sba(f"zin{i}", [128, 512], F32) for i in range(2)]
        zou = [sba(f"zou{i}", [128, 512], F32) for i in range(2)]
        for tg in range(32):
            b = tg % 2
            DM(S, zin[b][:], C["z_d"][tg * 128:(tg + 1) * 128, :], writes=[f"zin{b}"])
            X(S, "act", "activation", [f"zin{b}"], [f"zou{b}"], out=zou[b][:], in_=zin[b][:], func=AF.Silu)
            DM(S, sz_d[tg * 128:(tg + 1) * 128, :], zou[b][:], reads=[f"zou{b}"])
        S.end_phase()


def phase_ssd(C):
    nc, S, psum = C["nc"], C["S"], C["psum"]
    psT = C["psT"]
    din = C["din"]
    cst = {k: din(k, [128, 128]) for k in ("c_SL", "c_UI", "c_SU", "c_LI", "c_ones")}
    dtb_d = din("dtb_rep", [128, 512])
    alog_d = din("alog_rep", [128, 512])
    dskip_d = din("dskip_rep", [128, 512])
    nw_d = din("ssdnw_rep", [128, 512])
    ssdT_d = C["dscr"]("ssdT_d", [512, S_TOK], BF16)
    C["ssdT_d"] = ssdT_d
    xsB_d, bcT_d, z_d, dt_d = C["xsB_d"], C["bcT_d"], C["sz_d"], C["dt_d"]
    with contextlib.ExitStack() as ph:
        def sba(name, shape, dt):
            return ph.enter_context(nc.sbuf_tensor(name, list(shape), dt))
        K = {k: sba("k_" + k, [128, 128], F32) for k in cst}
        for k in cst:
            DM(S, K[k][:], cst[k], writes=[k])
        ident = sba("ident_s", [128, 128], BF16)
        DM(S, ident[:], C["ident_d"], writes=["ident_s"])
        G = {}
        for nm in ("dtr", "dtb", "alog", "dskip", "nw", "u", "au", "dt", "eA", "a", "tot", "acs", "cd", "d1", "d2",
                   "sarg", "oarg", "sdec", "odec", "wdt"):
            G[nm] = sba("g_" + nm, [128, 512], F32)
        DM(S, G["dtr"][:], dt_d, writes=["dtr"])
        DM(S, G["dtb"][:], dtb_d, writes=["dtb"])
        DM(S, G["alog"][:], alog_d, writes=["alog"])
        DM(S, G["dskip"][:], dskip_d, writes=["dskip"])
        DM(S, G["nw"][:], nw_d, writes=["nw"])

        def g(nm):
            return G[nm][:]

        def g3(nm):
            return G[nm][:].rearrange("p (c h) -> p c h", h=16)
        X(S, "dve", "tensor_tensor", ["dtr", "dtb"], ["u"], out=g("u"), in0=g("dtr"), in1=g("dtb"), op=ALU.add)
        X(S, "act", "activation", ["u"], ["au"], out=g("au"), in_=g("u"), func=AF.Exp)
        X(S, "act", "activation", ["au"], ["dt"], out=g("dt"), in_=g("au"), func=AF.Ln, bias=1.0)
        X(S, "act", "activation", ["alog"], ["eA"], out=g("eA"), in_=g("alog"), func=AF.Exp)
        X(S, "dve", "scalar_tensor_tensor", ["dt", "eA"], ["a"], out=g("a"), in0=g("dt"), scalar=-1.0, in1=g("eA"),
          op0=ALU.mult, op1=ALU.mult)
        X(S, "pe", "matmul", ["c_ones", "a"], ["ps0"], psum[0][:, :], lhsT=K["c_ones"][:], rhs=g("a"), start=True, stop=True)
        X(S, "pe", "matmul", ["c_UI", "a"], ["ps1"], psum[1][:, :], lhsT=K["c_UI"][:], rhs=g("a"), start=True, stop=True)
        X(S, "act", "copy", ["ps0"], ["tot"], out=g("tot"), in_=psum[0][:, :])
        X(S, "act", "copy", ["ps1"], ["acs"], out=g("acs"), in_=psum[1][:, :])
        X(S, "act", "activation", ["tot"], ["cd"], out=g("cd"), in_=g("tot"), func=AF.Exp)
        X(S, "dve", "tensor_tensor", ["tot", "acs"], ["d1"], out=g("d1"), in0=g("tot"), in1=g("acs"), op=ALU.subtract)
        X(S, "dve", "tensor_tensor", ["acs", "a"], ["d2"], out=g("d2"), in0=g("acs"), in1=g("a"), op=ALU.subtract)
        X(S, "pool", "tensor_copy", ["d1"], ["sarg"], out=g3("sarg")[:, :, 0:8], in_=g3("d1")[:, :, 0:8])
        X(S, "pool", "tensor_copy", ["d2"], ["sarg"], out=g3("sarg")[:, :, 8:16], in_=g3("d2")[:, :, 8:16])
        X(S, "pool", "tensor_copy", ["acs"], ["oarg"], out=g3("oarg")[:, :, 0:8], in_=g3("acs")[:, :, 0:8])
        X(S, "dve", "tensor_tensor", ["d1", "a"], ["oarg"], out=g3("oarg")[:, :, 8:16], in0=g3("d1")[:, :, 8:16],
          in1=g3("a")[:, :, 8:16], op=ALU.add)
        X(S, "act", "activation", ["sarg"], ["sdec"], out=g("sdec"), in_=g("sarg"), func=AF.Exp)
        X(S, "act", "activation", ["oarg"], ["odec"], out=g("odec"), in_=g("oarg"), func=AF.Exp)
        X(S, "dve", "tensor_tensor", ["dt", "sdec"], ["wdt"], out=g("wdt"), in0=g("dt"), in1=g("sdec"), op=ALU.mult)

        Fs = [[sba(f"Fs{d}_{k}", [128, 512], F32) for k in range(2)] for d in range(2)]
        Fstore = [sba(f"Fstore{d}", [128, 32, 512], BF16) for d in range(2)]
        xsB = [sba(f"xsB{i}", [128, 768], BF16) for i in range(4)]
        xsd = [sba(f"xsd{i}", [128, 512], BF16) for i in range(4)]
        for d in range(2):
            X(S, "pool", "memset", [], [f"Fs{d}_0"], Fs[d][0][:], 0.0)

        def p1_load(i_):
            for d in range(2):
                c = i_ if d == 0 else 31 - i_
                b = (i_ % 2) * 2 + d
                DM(S, xsB[b][:], xsB_d[c * 128:(c + 1) * 128, :], writes=[f"xsB{b}"])
        p1_load(0)
        for i_ in range(32):
            if i_ + 1 < 32:
                p1_load(i_ + 1)
            for d in range(2):
                c = i_ if d == 0 else 31 - i_
                b = (i_ % 2) * 2 + d
                cur, nxt = Fs[d][i_ % 2], Fs[d][(i_ + 1) % 2]
                rc, rn = f"Fs{d}_{i_ % 2}", f"Fs{d}_{(i_ + 1) % 2}"
                wv = g3("wdt")[:, c, d * 8:d * 8 + 8].unsqueeze(2).to_broadcast([128, 8, 64])
                X(S, "pool" if d == 0 else "dve", "tensor_tensor", [f"xsB{b}", "wdt"], [f"xsd{b}"],
                  out=xsd[b][:].rearrange("p (h d) -> p h d", h=8), in0=xsB[b][:, 0:512].rearrange("p (h d) -> p h d", h=8),
                  in1=wv, op=ALU.mult)
                pb = 1 + d
                for gq in range(2):
                    X(S, "pe", "matmul", [f"xsB{b}", f"xsd{b}"], [f"ps{pb}"], psum[pb][:, gq * 256:(gq + 1) * 256],
                      lhsT=xsB[b][:, 512 + gq * 128:512 + (gq + 1) * 128], rhs=xsd[b][:, gq * 256:(gq + 1) * 256],
                      start=True, stop=True)
                X(S, "act", "copy", [rc], [f"Fst{d}_{c}"], out=Fstore[d][:, c, :], in_=cur[:])
                cv = g3("cd")[:, c, d * 8:d * 8 + 8].unsqueeze(2).to_broadcast([128, 8, 64])
                X(S, "dve", "tensor_tensor", [rc, "cd"], [rn], out=nxt[:].rearrange("p (h d) -> p h d", h=8),
                  in0=cur[:].rearrange("p (h d) -> p h d", h=8), in1=cv, op=ALU.mult)
                X(S, "dve", "tensor_tensor", [rn, f"ps{pb}"], [rn], out=nxt[:], in0=nxt[:], in1=psum[pb][:, :], op=ALU.add)

        bc = [sba(f"bc{i}", [128, 4, 128], BF16) for i in range(2)]
        zt = [sba(f"zt{i}", [128, 512], F32) for i in range(2)]
        xdt = [sba(f"xdt{i}", [128, 512], BF16) for i in range(2)]
        Gm = [sba(f"Gm{i}", [128, 2, 128], BF16) for i in range(2)]
        lhs4 = [sba(f"lhs4_{i}", [128, 4, 128], F32) for i in range(2)]
        E4 = [sba(f"E4_{i}", [128, 4, 128], BF16) for i in range(2)]
        M4 = [sba(f"M4_{i}", [128, 4, 128], BF16) for i in range(2)]
        tA = sba("tA", [128, 512], F32)
        tB = sba("tB", [128, 512], F32)
        tU = sba("tU", [128, 512], F32)
        ez = sba("ez", [128, 512], F32)
        junk = sba("junk", [128, 256], F32)
        ss = sba("ss", [128, 2], F32)
        yb = sba("yb", [128, 512], BF16)
        yT = [sba(f"yT{i}", [128, 4, 128], BF16) for i in range(2)]
        bcv = bcT_d.rearrange("(q p) t -> p q t", p=128)
        ssdv = ssdT_d.rearrange("(q p) t -> p q t", p=128)
        qi = 0
        def p2_loads(c):
            b = c % 2
            DM(S, xsB[b][:], xsB_d[c * 128:(c + 1) * 128, :], writes=[f"xsB{b}"])
            DM(S, bc[b][:], bcv[:, :, c * 128:(c + 1) * 128], writes=[f"bc{b}"])
            DM(S, zt[b][:], z_d[c * 128:(c + 1) * 128, :], writes=[f"zt{b}"])
        p2_loads(0)
        for c in range(32):
            b = c % 2
            if c + 1 < 32:
                p2_loads(c + 1)
            xs3 = xsB[b][:, 0:512].rearrange("p (h d) -> p h d", h=8)
            for d in range(2):
                dv = g3("dt")[:, c, d * 8:d * 8 + 8].unsqueeze(2).to_broadcast([128, 8, 64])
                X(S, "dve", "tensor_tensor", [f"xsB{b}", "dt"], [f"xdt{d}"], out=xdt[d][:].rearrange("p (h d) -> p h d", h=8),
                  in0=xs3, in1=dv, op=ALU.mult)
            for gq in range(2):
                X(S, "pe", "matmul", [f"bc{b}"], ["ps0"], psum[0][:, gq * 128:(gq + 1) * 128], lhsT=bc[b][:, gq, :],
                  rhs=bc[b][:, 2 + gq, :], start=True, stop=True)
            p0v = psum[0][:, 0:256].rearrange("p (g l) -> p g l", g=2)
            X(S, "dve", "tensor_tensor", ["ps0", "c_UI"], ["Gm0"], out=Gm[0][:], in0=p0v,
              in1=K["c_UI"][:].unsqueeze(1).to_broadcast([128, 2, 128]), op=ALU.mult)
            X(S, "dve", "tensor_tensor", ["ps0", "c_LI"], ["Gm1"], out=Gm[1][:], in0=p0v,
              in1=K["c_LI"][:].unsqueeze(1).to_broadcast([128, 2, 128]), op=ALU.mult)
            for d in range(2):
                for gq in range(2):
                    q = qi % 2
                    qi += 1
                    sb_ = 1 + q
                    h0 = c * 16 + d * 8 + gq * 4
                    av = G["a"][:, h0:h0 + 4].unsqueeze(2).to_broadcast([128, 4, 128])
                    tri = K["c_SL" if d == 0 else "c_SU"][:].unsqueeze(1).to_broadcast([128, 4, 128])
                    X(S, "dve", "tensor_tensor", ["a", "c_SL", "c_SU"], [f"lhs4_{q}"], out=lhs4[q][:], in0=tri, in1=av, op=ALU.mult)
                    rk = K["c_UI" if d == 0 else "c_LI"]
                    for i in range(4):
                        X(S, "pe", "matmul", [f"lhs4_{q}", "c_UI", "c_LI"], [f"ps{sb_}"], psum[sb_][:, i * 128:(i + 1) * 128],
                          lhsT=lhs4[q][:, i, :], rhs=rk[:], start=True, stop=True)
                    X(S, "act", "activation", [f"ps{sb_}"], [f"E4_{q}"], out=E4[q][:].rearrange("p a b -> p (a b)"),
                      in_=psum[sb_][:, :], func=AF.Exp)
                    X(S, "pool", "tensor_tensor", [f"E4_{q}", f"Gm{d}"], [f"M4_{q}"], out=M4[q][:], in0=E4[q][:],
                      in1=Gm[d][:, gq, :].unsqueeze(1).to_broadcast([128, 4, 128]), op=ALU.mult)
                    yb_ = 3 + d
                    for i in range(4):
                        h = gq * 4 + i
                        X(S, "pe", "matmul", [f"M4_{q}", f"xdt{d}"], [f"ps{yb_}"], psum[yb_][:, h * 64:(h + 1) * 64],
                          lhsT=M4[q][:, i, :], rhs=xdt[d][:, h * 64:(h + 1) * 64], start=True, stop=True)
                    X(S, "pe", "matmul", [f"bc{b}", f"Fst{d}_{c}"], [f"ps{5 + d}"], psum[5 + d][:, gq * 256:(gq + 1) * 256],
                      lhsT=bc[b][:, 2 + gq, :], rhs=Fstore[d][:, c, gq * 256:(gq + 1) * 256], start=True, stop=True)
            t3 = lambda t: t[:].rearrange("p (h d) -> p h d", h=8)
            for d, tt in ((0, tA), (1, tB)):
                ov = g3("odec")[:, c, d * 8:d * 8 + 8].unsqueeze(2).to_broadcast([128, 8, 64])
                X(S, "dve", "tensor_tensor", [f"ps{5 + d}", "odec"], [tt.name], out=t3(tt),
                  in0=psum[5 + d][:, :].rearrange("p (h d) -> p h d", h=8), in1=ov, op=ALU.mult)
                X(S, "dve", "tensor_tensor", [f"ps{3 + d}", tt.name], [tt.name], out=tt[:], in0=tt[:], in1=psum[3 + d][:, :], op=ALU.add)
            X(S, "pool", "tensor_tensor", ["tA", "tB"], ["tA"], out=tA[:], in0=tA[:], in1=tB[:], op=ALU.add)
            X(S, "pool", "tensor_tensor", [f"xsB{b}", "dskip"], ["tU"], out=tU[:], in0=xsB[b][:, 0:512], in1=g("dskip"), op=ALU.mult)
            X(S, "pool", "tensor_tensor", ["tA", "tU"], ["tA"], out=tA[:], in0=tA[:], in1=tU[:], op=ALU.add)
            X(S, "dve", "tensor_tensor", ["tA", f"zt{b}"], ["tA"], out=tA[:], in0=tA[:], in1=zt[b][:], op=ALU.mult)
            X(S, "dve", "tensor_tensor", ["tA"], ["tU"], out=tU[:], in0=tA[:], in1=tA[:], op=ALU.mult)
            X(S, "dve", "tensor_reduce", ["tU"], ["ss"], out=ss[:], in_=tU[:].rearrange("p (g f) -> p g f", g=2),
              axis=mybir.AxisListType.X, op=ALU.add)
            X(S, "dve", "tensor_scalar", ["ss"], ["ss"], out=ss[:], in0=ss[:], scalar1=1.0 / 256.0, scalar2=1e-5, op0=ALU.mult, op1=ALU.add)
            X(S, "act", "activation", ["ss"], ["ss"], out=ss[:], in_=ss[:], func=AF.Ln)
            X(S, "act", "activation", ["ss"], ["ss"], out=ss[:], in_=ss[:], func=AF.Exp, scale=-0.5)
            for gq in range(2):
                X(S, "dve", "scalar_tensor_tensor", ["tA", "ss", "nw"], ["yb"], out=yb[:, gq * 256:(gq + 1) * 256],
                  in0=tA[:, gq * 256:(gq + 1) * 256], scalar=ss[:, gq:gq + 1], in1=G["nw"][:, gq * 256:(gq + 1) * 256],
                  op0=ALU.mult, op1=ALU.mult)
            for q4 in range(4):
                X(S, "pe", "transpose", ["yb", "ident_s"], ["psT"], psT[:, q4 * 128:(q4 + 1) * 128], in_=yb[:, q4 * 128:(q4 + 1) * 128],
                  identity=ident[:])
            X(S, "act", "copy", ["psT"], [f"yT{b}"], out=yT[b][:].rearrange("p a b -> p (a b)"), in_=psT[:, 0:512])
            DM(S, ssdv[:, :, c * 128:(c + 1) * 128], yT[b][:], reads=[f"yT{b}"])
        S.end_phase()


ALPHA = float((2.0 * 1) ** 0.25)
NROWS = N_EXP * CAP


def phase_mix(C):
    nc, S, psum, psT = C["nc"], C["S"], C["psum"], C["psT"]
    din, dscr = C["din"], C["dscr"]
    wout_d = din("w_out", [D, D])
    anw_d = din("anwT", [128, 4])
    l1w_d = din("ln1w_rep", [128, D])
    l1b_d = din("ln1b_rep", [128, D])
    rw_d = din("router_w", [D, N_EXP])
    rb_d = din("rb_rep", [128, N_EXP])
    ecap_d = din("ecap_rep", [128, N_EXP])
    su_d = din("c_SU2", [128, 128])
    ones_d = din("c_ones2", [128, 128])
    h1_d = dscr("h1_d", [S_TOK, D], F32)
    Xg_d = dscr("Xg_d", [NROWS, D], BF16)
    C["h1_d"], C["Xg_d"] = h1_d, Xg_d
    gates_all, dest_all = C["gates_all"], C["dest_all"]
    attnT_d, ssdT_d, x = C["attnT_d"], C["ssdT_d"], C["x"]
    with contextlib.ExitStack() as ph:
        def sba(name, shape, dt):
            return ph.enter_context(nc.sbuf_tensor(name, list(shape), dt))
        wo = sba("wo", [128, 8, D], BF16)
        wst = [sba(f"wost{i}", [128, 2, D], F32) for i in range(2)]
        anw = sba("anw", [128, 4], F32)
        l1w = sba("l1w", [128, D], F32)
        l1b = sba("l1b", [128, D], F32)
        rwf = sba("rwf", [128, 8, N_EXP], F32)
        rwb = sba("rwb", [128, 8, N_EXP], BF16)
        rb = sba("rb", [128, N_EXP], F32)
        base = sba("base", [128, N_EXP], F32)
        SU = sba("SU2", [128, 128], F32)
        ones = sba("ones2", [128, 128], F32)
        ident = sba("ident_m", [128, 128], BF16)
        DM(S, ident[:], C["ident_d"], writes=["ident_m"])
        DM(S, anw[:], anw_d, writes=["anw"])
        DM(S, l1w[:], l1w_d, writes=["l1w"])
        DM(S, l1b[:], l1b_d, writes=["l1b"])
        DM(S, rwf[:], rw_d.rearrange("(c p) e -> p c e", p=128), writes=["rwf"])
        DM(S, rb[:], rb_d, writes=["rb"])
        DM(S, base[:], ecap_d, writes=["base"])
        DM(S, SU[:], su_d, writes=["SU2"])
        DM(S, ones[:], ones_d, writes=["ones2"])
        X(S, "dve", "tensor_copy", ["rwf"], ["rwb"], out=rwb[:], in_=rwf[:])
        X(S, "pool", "memset", [], [f"dest{t}" for t in range(32)], dest_all[:], 0)
        wov = wout_d.rearrange("(c p) f -> p c f", p=128)
        for i in range(4):
            b = i % 2
            DM(S, wst[b][:], wov[:, 2 * i:2 * i + 2, :], writes=[f"wost{b}"])
            if i < 2:
                X(S, "dve", "tensor_tensor", [f"wost{b}", "anw"], ["wo"], out=wo[:, 2 * i:2 * i + 2, :], in0=wst[b][:],
                  in1=anw[:, 2 * i:2 * i + 2].unsqueeze(2).to_broadcast([128, 2, D]), op=ALU.mult)
            else:
                X(S, "dve", "tensor_copy", [f"wost{b}"], ["wo"], out=wo[:, 2 * i:2 * i + 2, :], in_=wst[b][:])
        aT = [sba(f"aT{i}", [128, 4, 128], BF16) for i in range(2)]
        sT = [sba(f"sT{i}", [128, 4, 128], BF16) for i in range(2)]
        xt = [sba(f"xt{i}", [128, D], F32) for i in range(2)]
        gjunk = sba("gjunk", [128, 128], F32)
        identf = sba("identf", [128, 128], F32)
        X(S, "dve", "tensor_copy", ["ident_m"], ["identf"], out=identf[:], in_=ident[:])
        sm = sba("sm", [128, 8], F32)
        sm2 = sba("sm2", [128, 8], F32)
        tt = sba("tmix", [128, D], F32)
        stats = sba("stats", [128, 12], F32)
        mv = sba("mv", [128, 2], F32)
        h1f = [sba(f"h1f{i}", [128, D], F32) for i in range(2)]
        h1b = [sba(f"h1b{i}", [128, D], BF16) for i in range(3)]
        h1T = sba("h1T", [128, D], BF16)
        lg = sba("lg", [128, N_EXP], F32)
        top8 = sba("top8", [128, 8], F32)
        msk = sba("msk", [128, N_EXP], F32)
        ex4 = sba("ex4", [128, 4], F32)
        oh = sba("oh", [128, 4, N_EXP], F32)
        posd = sba("posd", [128, N_EXP], F32)
        destf = sba("destf", [128, 4], F32)
        av = attnT_d.rearrange("(q p) t -> p q t", p=128)
        sv = ssdT_d.rearrange("(q p) t -> p q t", p=128)
        def mix_loads(tg):
            b = tg % 2
            tsl = slice(tg * 128, (tg + 1) * 128)
            DM(S, aT[b][:], av[:, :, tsl], writes=[f"aT{b}"])
            DM(S, sT[b][:], sv[:, :, tsl], writes=[f"sT{b}"])
            DM(S, xt[b][:], x[tsl, :], writes=[f"xt{b}"])

        def mix_s1(tg):
            b = tg % 2
            tsl = slice(tg * 128, (tg + 1) * 128)
            for q in range(4):
                X(S, "pe", "matmul", [f"aT{b}"], ["ps6"], psum[6][:, 0:128], lhsT=aT[b][:, q, :], rhs=aT[b][:, q, :],
                  start=(q == 0), stop=(q == 3))
            X(S, "dve", "tensor_tensor", ["ps6", "identf"], ["gjunk"], out=gjunk[:], in0=psum[6][:, 0:128], in1=identf[:], op=ALU.mult)
            X(S, "dve", "tensor_reduce", ["gjunk"], ["sm"], out=sm[:, 7:8], in_=gjunk[:], axis=mybir.AxisListType.X, op=ALU.add)
            X(S, "dve", "tensor_scalar", ["sm"], ["sm"], out=sm[:, 0:1], in0=sm[:, 7:8], scalar1=1.0 / 512.0, scalar2=1e-5,
              op0=ALU.mult, op1=ALU.add)
            X(S, "act", "activation", ["sm"], ["sm"], out=sm[:, 1:2], in_=sm[:, 0:1], func=AF.Ln)
            X(S, "act", "activation", ["sm"], ["sm"], out=sm[:, 2:3], in_=sm[:, 1:2], func=AF.Exp, scale=-0.5)
            for hf in range(2):
                for q in range(4):
                    X(S, "pe", "matmul", [f"aT{b}", "wo"], [f"ps{hf}"], psum[hf][:, :], lhsT=aT[b][:, q, :],
                      rhs=wo[:, q, hf * 512:(hf + 1) * 512], start=(q == 0), stop=(q == 3))
                for q in range(4):
                    X(S, "pe", "matmul", [f"sT{b}", "wo"], [f"ps{2 + hf}"], psum[2 + hf][:, :], lhsT=sT[b][:, q, :],
                      rhs=wo[:, 4 + q, hf * 512:(hf + 1) * 512], start=(q == 0), stop=(q == 3))
            for hf in range(2):
                hs = slice(hf * 512, (hf + 1) * 512)
                X(S, "dve", "scalar_tensor_tensor", [f"xt{b}", f"ps{2 + hf}"], ["tmix"], out=tt[:, hs], in0=xt[b][:, hs], scalar=ALPHA,
                  in1=psum[2 + hf][:, :], op0=ALU.mult, op1=ALU.add)
                X(S, "dve", "scalar_tensor_tensor", ["tmix", f"ps{hf}", "sm"], ["tmix"], out=tt[:, hs], in0=psum[hf][:, :],
                  scalar=sm[:, 2:3], in1=tt[:, hs], op0=ALU.mult, op1=ALU.add)
                X(S, "dve", "bn_stats", ["tmix"], ["stats"], out=stats[:, hf * 6:(hf + 1) * 6], in_=tt[:, hs])
            X(S, "dve", "bn_aggr", ["stats"], ["mv"], out=mv[:], in_=stats[:])
            X(S, "act", "activation", ["mv"], ["sm"], out=sm[:, 3:4], in_=mv[:, 1:2], func=AF.Ln, bias=1e-5)
            X(S, "act", "activation", ["sm"], ["sm"], out=sm[:, 4:5], in_=sm[:, 3:4], func=AF.Exp, scale=-0.5)
            X(S, "dve", "tensor_scalar", ["tmix", "mv", "sm"], ["tmix"], out=tt[:], in0=tt[:], scalar1=mv[:, 0:1], scalar2=sm[:, 4:5],
              op0=ALU.subtract, op1=ALU.mult)
            X(S, "dve", "tensor_tensor", ["tmix", "l1w"], ["tmix"], out=tt[:], in0=tt[:], in1=l1w[:], op=ALU.mult)
            X(S, "pool", "tensor_tensor", ["tmix", "l1b"], [f"h1f{b}"], out=h1f[b][:], in0=tt[:], in1=l1b[:], op=ALU.add)
            X(S, "act", "copy", [f"h1f{b}"], [f"h1b{tg % 3}"], out=h1b[tg % 3][:], in_=h1f[b][:])
            DM(S, h1_d[tsl, :], h1f[b][:], reads=[f"h1f{b}"])

        def mix_s2(tg):
            b = tg % 2
            tsl = slice(tg * 128, (tg + 1) * 128)
            for kc in range(8):
                X(S, "pe", "transpose", [f"h1b{tg % 3}", "ident_m"], ["psT"], psT[:, kc * 128:(kc + 1) * 128],
                  in_=h1b[tg % 3][:, kc * 128:(kc + 1) * 128], identity=ident[:])
            X(S, "act", "copy", ["psT"], ["h1T"], out=h1T[:], in_=psT[:, :])
            for kc in range(8):
                X(S, "pe", "matmul", ["h1T", "rwb"], ["ps4"], psum[4][:, 0:N_EXP], lhsT=h1T[:, kc * 128:(kc + 1) * 128],
                  rhs=rwb[:, kc, :], start=(kc == 0), stop=(kc == 7))
            X(S, "dve", "tensor_tensor", ["ps4", "rb"], ["lg"], out=lg[:], in0=psum[4][:, 0:N_EXP], in1=rb[:], op=ALU.add)
            X(S, "dve", "max", ["lg"], ["top8"], out=top8[:], in_=lg[:])
            X(S, "dve", "tensor_scalar", ["lg", "top8"], ["msk"], out=msk[:], in0=lg[:], scalar1=top8[:, 3:4], scalar2=None, op0=ALU.is_ge)
            X(S, "dve", "tensor_scalar", ["top8"], ["sm2"], out=sm2[:, 5:6], in0=top8[:, 0:1], scalar1=-1.0, scalar2=None, op0=ALU.mult)
            X(S, "act", "activation", ["top8", "sm2"], ["ex4"], out=ex4[:], in_=top8[:, 0:4], func=AF.Exp, bias=sm2[:, 5:6])
            X(S, "dve", "tensor_reduce", ["ex4"], ["sm2"], out=sm2[:, 6:7], in_=ex4[:], axis=mybir.AxisListType.X, op=ALU.add)
            X(S, "dve", "reciprocal", ["sm2"], ["sm2"], out=sm2[:, 7:8], in_=sm2[:, 6:7])
            X(S, "dve", "tensor_scalar", ["ex4", "sm2"], ["gates"], out=gates_all[:, tg, :], in0=ex4[:], scalar1=sm2[:, 7:8], scalar2=None,
              op0=ALU.mult)
            X(S, "pe", "matmul", ["SU2", "msk"], ["ps5"], psum[5][:, 0:N_EXP], lhsT=SU[:], rhs=msk[:], start=True, stop=True)
            X(S, "pe", "matmul", ["ones2", "msk"], ["ps5"], psum[5][:, 64:64 + N_EXP], lhsT=ones[:], rhs=msk[:], start=True, stop=True)
            X(S, "dve", "tensor_tensor", ["ps5", "base"], ["posd"], out=posd[:], in0=psum[5][:, 0:N_EXP], in1=base[:], op=ALU.add)
            X(S, "dve", "tensor_tensor", ["ps5", "base"], ["base"], out=base[:], in0=psum[5][:, 64:64 + N_EXP], in1=base[:], op=ALU.add)
            X(S, "dve", "tensor_tensor", ["lg", "top8"], ["oh"], out=oh[:], in0=lg[:].unsqueeze(1).to_broadcast([128, 4, N_EXP]),
              in1=top8[:, 0:4].unsqueeze(2).to_broadcast([128, 4, N_EXP]), op=ALU.is_equal)
            X(S, "dve", "tensor_tensor", ["oh", "posd"], ["oh"], out=oh[:], in0=oh[:],
              in1=posd[:].unsqueeze(1).to_broadcast([128, 4, N_EXP]), op=ALU.mult)
            X(S, "dve", "tensor_reduce", ["oh"], ["destf"], out=destf[:], in_=oh[:], axis=mybir.AxisListType.X, op=ALU.add)
            X(S, "dve", "tensor_copy", ["destf"], [f"dest{tg}"], out=dest_all[:, tg, :], in_=destf[:])

        def mix_scatter(tg):
            b = tg % 3
            for j in range(4):
                idx = dest_all[:, tg, j:j + 1]
                src = h1b[b][:]
                S.dma(lambda e, idx=idx, src=src: e.indirect_dma_start(
                    out=Xg_d, out_offset=bass.IndirectOffsetOnAxis(ap=idx, axis=0), in_=src, in_offset=None,
                    bounds_check=S.bc(e), oob_is_err=False),
                    reads=[f"dest{tg}", f"h1b{b}"], writes=[f"Xg{tg}_{j}"], q="pool")

        mix_loads(0)
        mix_loads(1)
        mix_s1(0)
        for tg in range(32):
            if tg + 2 < 32:
                mix_loads(tg + 2)
            if tg + 1 < 32:
                mix_s1(tg + 1)
            mix_s2(tg)
            if tg >= 1:
                mix_scatter(tg - 1)
        mix_scatter(31)
        ecs = sba("ecs", [128, N_EXP], F32)
        cntf = sba("cntf", [128, N_EXP], F32)
        DM(S, ecs[:], ecap_d, writes=["ecs"])
        X(S, "dve", "tensor_tensor", ["base", "ecs"], ["cntf"], out=cntf[:], in0=base[:], in1=ecs[:], op=ALU.subtract)
        X(S, "dve", "tensor_scalar", ["cntf"], ["cntf"], out=cntf[:], in0=cntf[:], scalar1=512.0, scalar2=None, op0=ALU.is_gt)
        X(S, "dve", "tensor_copy", ["cntf"], ["flags"], out=C["flags_all"][:], in_=cntf[:])
        if C["debug"]:
            dd = dscr("dest_dbg", [128, 128], I32)
            gd = dscr("gates_dbg", [128, 128], F32)
            DM(S, dd, dest_all[:].rearrange("p a b -> p (a b)"), reads=[f"dest{t}" for t in range(32)])
            DM(S, gd, gates_all[:].rearrange("p a b -> p (a b)"), reads=["gates"])
        S.end_phase()


def phase_experts(C):
    nc, S, psum, psT = C["nc"], C["S"], C["psum"], C["psT"]
    din, dscr = C["din"], C["dscr"]
    wg_d = din("w_gate", [N_EXP, D, D])
    wu_d = din("w_up", [N_EXP, D, D])
    wd_d = din("w_down", [N_EXP, D, D])
    bg_d = din("bgT", [128, N_EXP * 8])
    bu_d = din("buT", [128, N_EXP * 8])
    bd_d = din("b_down", [N_EXP, D])
    Yg_d = dscr("Yg_d", [NROWS, D], F32)
    C["Yg_d"] = Yg_d
    Xg_d = C["Xg_d"]
    NT = CAP // 128
    psTs = [psT, psum[6][:, :].bitcast(BF16)]
    psTn = ["psT", "ps6"]
    with contextlib.ExitStack() as ph:
        def sba(name, shape, dt):
            return ph.enter_context(nc.sbuf_tensor(name, list(shape), dt))
        NS = 4
        wsl = [sba(f"wsl{i}", [128, 8, D], BF16) for i in range(NS)]
        wst = [sba(f"west{i}", [128, 2, D], F32) for i in range(2)]
        bg = sba("bg", [128, N_EXP * 8], F32)
        bu = sba("bu", [128, N_EXP * 8], F32)
        bd = [sba(f"bd{i}", [128, D], F32) for i in range(2)]
        ident = sba("ident_e", [128, 128], BF16)
        NXG = 8
        xg = [sba(f"xg{i}", [128, D], BF16) for i in range(NXG)]
        XT = [sba(f"XT{i}", [128, 8, CAP], BF16) for i in range(2)]
        actT = sba("actT", [128, 8, CAP], BF16)
        g1 = [sba(f"g1_{i}", [128, 512], F32) for i in range(2)]
        u1 = [sba(f"u1_{i}", [128, 512], F32) for i in range(2)]
        sg = [sba(f"sg_{i}", [128, 512], F32) for i in range(2)]
        yo = [sba(f"yo{i}", [128, D], F32) for i in range(2)]
        DM(S, ident[:], C["ident_d"], writes=["ident_e"])
        DM(S, bg[:], bg_d, writes=["bg"])
        DM(S, bu[:], bu_d, writes=["bu"])
        st = dict(slot=0, stg=0, xg=0, tp=0)

        def load_w_pieces(src, e):
            sl = st["slot"] % NS
            st["slot"] += 1
            v = src[e].rearrange("(c p) f -> p c f", p=128)

            def piece(i):
                b = st["stg"] % 2
                st["stg"] += 1
                DM(S, wst[b][:], v[:, 2 * i:2 * i + 2, :], writes=[f"west{b}"])
                X(S, "act", "copy", [f"west{b}"], [f"wsl{sl}"], out=wsl[sl][:, 2 * i:2 * i + 2, :], in_=wst[b][:])
            return sl, [lambda i=i: piece(i) for i in range(4)]

        def load_w(src, e):
            sl, ps = load_w_pieces(src, e)
            for p in ps:
                p()
            return sl

        def xg_loads(e):
            for t in range(NT):
                r0 = e * CAP + t * 128
                DM(S, xg[t][:], Xg_d[r0:r0 + 128, :], writes=[f"xg{t}"])

        flags = C["flags_all"]

        def xt_transposes(e, t0, t1):
            xb = e % 2
            for t in range(t0, t1):
                tp = st["tp"] % 2
                st["tp"] += 1
                for kc in range(8):
                    X(S, "pe", "transpose", [f"xg{t}", "ident_e"], [psTn[tp]], psTs[tp][:, kc * 128:(kc + 1) * 128],
                      in_=xg[t][:, kc * 128:(kc + 1) * 128], identity=ident[:])
                pv_ = psTs[tp][:, :].rearrange("p (c t) -> p c t", c=8)
                X(S, "dve", "tensor_copy", [psTn[tp]], [f"XT{xb}"], out=XT[xb][:, :, t * 128:(t + 1) * 128], in_=pv_)

        def xt_all(e):
            xt_transposes(e, 0, 4)
            S.cond_begin(flags[0:1, e:e + 1])
            xt_transposes(e, 4, 8)
            S.cond_end()

        def gu_unit(e, fc, hf, sg_, su_, xb):
            q = st["ei"] % 2
            st["ei"] += 1
            ns = slice(hf * 512, (hf + 1) * 512)
            pg, pu = 2 * q, 2 * q + 1
            for kc in range(8):
                X(S, "pe", "matmul", [f"wsl{sg_}", f"XT{xb}"], [f"ps{pg}"], psum[pg][:, :], lhsT=wsl[sg_][:, kc, fc * 128:(fc + 1) * 128],
                  rhs=XT[xb][:, kc, ns], start=(kc == 0), stop=(kc == 7))
            for kc in range(8):
                X(S, "pe", "matmul", [f"wsl{su_}", f"XT{xb}"], [f"ps{pu}"], psum[pu][:, :], lhsT=wsl[su_][:, kc, fc * 128:(fc + 1) * 128],
                  rhs=XT[xb][:, kc, ns], start=(kc == 0), stop=(kc == 7))
            bcol = e * 8 + fc
            X(S, "dve", "tensor_scalar", [f"ps{pg}", "bg"], [f"g1_{q}"], out=g1[q][:], in0=psum[pg][:, :], scalar1=bg[:, bcol:bcol + 1],
              scalar2=7.0, op0=ALU.add, op1=ALU.min)
            X(S, "dve", "tensor_scalar", [f"ps{pu}", "bu"], [f"u1_{q}"], out=u1[q][:], in0=psum[pu][:, :], scalar1=bu[:, bcol:bcol + 1],
              scalar2=7.0, op0=ALU.add, op1=ALU.min)
            X(S, "dve", "tensor_scalar", [f"u1_{q}"], [f"u1_{q}"], out=u1[q][:], in0=u1[q][:], scalar1=-7.0, scalar2=1.0,
              op0=ALU.max, op1=ALU.add)
            X(S, "act", "activation", [f"g1_{q}"], [f"sg_{q}"], out=sg[q][:], in_=g1[q][:], func=AF.Sigmoid, scale=1.702)
            X(S, "pool", "tensor_tensor", [f"g1_{q}", f"sg_{q}"], [f"sg_{q}"], out=sg[q][:], in0=g1[q][:], in1=sg[q][:], op=ALU.mult)
            X(S, "pool", "tensor_tensor", [f"u1_{q}", f"sg_{q}"], [f"actT{hf}"], out=actT[:, fc, ns], in0=sg[q][:], in1=u1[q][:], op=ALU.mult)

        def down_tile(e, t, sd_, bb):
            yb_ = t % 2
            for hf in range(2):
                pb = 4 + hf
                for fc in range(8):
                    X(S, "pe", "matmul", [f"wsl{sd_}", f"actT{t // 4}"], [f"ps{pb}"], psum[pb][:, :], lhsT=actT[:, fc, t * 128:(t + 1) * 128],
                      rhs=wsl[sd_][:, fc, hf * 512:(hf + 1) * 512], start=(fc == 0), stop=(fc == 7))
                X(S, "dve", "tensor_tensor", [f"ps{pb}", f"bd{bb}"], [f"yo{yb_}"], out=yo[yb_][:, hf * 512:(hf + 1) * 512], in0=psum[pb][:, :],
                  in1=bd[bb][:, hf * 512:(hf + 1) * 512], op=ALU.add)
            r0 = e * CAP + t * 128
            DM(S, Yg_d[r0:r0 + 128, :], yo[yb_][:], reads=[f"yo{yb_}"], writes=[], q="act")

        st["ei"] = 0
        xg_loads(0)
        sg_, su_ = load_w(wg_d, 0), load_w(wu_d, 0)
        xt_all(0)
        for e in range(N_EXP):
            xb = e % 2
            fl = flags[0:1, e:e + 1]
            if e + 1 < N_EXP:
                xg_loads(e + 1)
            sd_, todo = load_w_pieces(wd_d, e)
            if e + 1 < N_EXP:
                sg_n, todo2 = load_w_pieces(wg_d, e + 1)
                todo = todo + todo2
            bb = e % 2
            DM(S, bd[bb][:], bd_d[e:e + 1, :].partition_broadcast(128), writes=[f"bd{bb}"])
            for fc in range(8):
                gu_unit(e, fc, 0, sg_, su_, xb)
                if todo:
                    todo.pop(0)()
            while todo:
                todo.pop(0)()
            S.cond_begin(fl)
            for fc in range(8):
                gu_unit(e, fc, 1, sg_, su_, xb)
            S.cond_end()
            todo3 = []
            if e + 1 < N_EXP:
                su_n, todo3 = load_w_pieces(wu_d, e + 1)
                xt_all(e + 1)
            for t in range(4):
                if todo3:
                    todo3.pop(0)()
                down_tile(e, t, sd_, bb)
            while todo3:
                todo3.pop(0)()
            S.cond_begin(fl)
            for t in range(4, 8):
                down_tile(e, t, sd_, bb)
            S.cond_end()
            if e + 1 < N_EXP:
                sg_, su_ = sg_n, su_n
        S.end_phase()


def phase_combine(C):
    nc, S = C["nc"], C["S"]
    din, dscr = C["din"], C["dscr"]
    l2w_d = din("ln2w_rep", [128, D])
    l2b_d = din("ln2b_rep", [128, D])
    out_d = nc.dram_tensor("out", [S_TOK, D], F32, kind="ExternalOutput").ap()
    Yg_d, h1_d = C["Yg_d"], C["h1_d"]
    gates_all, dest_all = C["gates_all"], C["dest_all"]
    with contextlib.ExitStack() as ph:
        def sba(name, shape, dt):
            return ph.enter_context(nc.sbuf_tensor(name, list(shape), dt))
        l2w = sba("l2w", [128, D], F32)
        l2b = sba("l2b", [128, D], F32)
        DM(S, l2w[:], l2w_d, writes=["l2w"])
        DM(S, l2b[:], l2b_d, writes=["l2b"])
        yg = [[sba(f"yg{i}_{j}", [128, D], F32) for j in range(4)] for i in range(2)]
        h1 = [sba(f"h1c{i}", [128, D], F32) for i in range(2)]
        acc = sba("cacc", [128, D], F32)
        ot = [sba(f"cot{i}", [128, D], F32) for i in range(2)]
        stats = sba("cstats", [128, 12], F32)
        mv = sba("cmv", [128, 2], F32)
        sm = sba("csm", [128, 4], F32)
        def comb_gather(tg):
            b = tg % 2
            tsl = slice(tg * 128, (tg + 1) * 128)
            DM(S, h1[b][:], h1_d[tsl, :], writes=[f"h1c{b}"])
            for j in range(4):
                idx = dest_all[:, tg, j:j + 1]
                dst = yg[b][j][:]
                S.dma(lambda e, idx=idx, dst=dst: e.indirect_dma_start(
                    out=dst, out_offset=None, in_=Yg_d, in_offset=bass.IndirectOffsetOnAxis(ap=idx, axis=0),
                    bounds_check=S.bc(e), oob_is_err=False),
                    reads=[], writes=[f"yg{b}_{j}"], q="pool")
        comb_gather(0)
        for tg in range(32):
            b = tg % 2
            tsl = slice(tg * 128, (tg + 1) * 128)
            if tg + 1 < 32:
                comb_gather(tg + 1)
            X(S, "dve", "tensor_scalar", [f"h1c{b}"], ["cacc"], out=acc[:], in0=h1[b][:], scalar1=ALPHA, scalar2=None, op0=ALU.mult)
            for j in range(4):
                X(S, "dve", "scalar_tensor_tensor", [f"yg{b}_{j}", "cacc"], ["cacc"], out=acc[:], in0=yg[b][j][:],
                  scalar=gates_all[:, tg, j:j + 1], in1=acc[:], op0=ALU.mult, op1=ALU.add)
            for hf in range(2):
                X(S, "dve", "bn_stats", ["cacc"], ["cstats"], out=stats[:, hf * 6:(hf + 1) * 6], in_=acc[:, hf * 512:(hf + 1) * 512])
            X(S, "dve", "bn_aggr", ["cstats"], ["cmv"], out=mv[:], in_=stats[:])
            X(S, "act", "activation", ["cmv"], ["csm"], out=sm[:, 0:1], in_=mv[:, 1:2], func=AF.Ln, bias=1e-5)
            X(S, "act", "activation", ["csm"], ["csm"], out=sm[:, 1:2], in_=sm[:, 0:1], func=AF.Exp, scale=-0.5)
            X(S, "dve", "tensor_scalar", ["cacc", "cmv", "csm"], ["cacc"], out=acc[:], in0=acc[:], scalar1=mv[:, 0:1], scalar2=sm[:, 1:2],
              op0=ALU.subtract, op1=ALU.mult)
            X(S, "dve", "tensor_tensor", ["cacc", "l2w"], ["cacc"], out=acc[:], in0=acc[:], in1=l2w[:], op=ALU.mult)
            X(S, "pool", "tensor_tensor", ["cacc", "l2b"], [f"cot{b}"], out=ot[b][:], in0=acc[:], in1=l2b[:], op=ALU.add)
            DM(S, out_d[tsl, :], ot[b][:], reads=[f"cot{b}"])
        S.end_phase()


def build(debug=False):
    nc = bass.Bass("TRN2", target_bir_lowering=False)
    es = contextlib.ExitStack()
    with es:
        def din(name, shape, dt=F32):
            return nc.dram_tensor(name, list(shape), dt, kind="ExternalInput").ap()

        def dscr(name, shape, dt, out=False):
            kind = "ExternalOutput" if (out or debug) else "Internal"
            return nc.dram_tensor(name, list(shape), dt, kind=kind).ap()

        xT = din("xT", [D, S_TOK])
        x = din("x", [S_TOK, D])
        w_in = din("w_in_ext", [D, WCOLS])
        cosT = din("cosT", [128, S_TOK])
        sinT = din("sinT", [128, S_TOK])

        qT_d = dscr("qT_d", [512, S_TOK], BF16)
        kT_d = dscr("kT_d", [512, S_TOK], BF16)
        v_d = dscr("v_d", [S_TOK, 512], BF16)
        z_d = dscr("z_d", [S_TOK, 512], F32)
        dt_d = dscr("dt_d", [128, 512], F32)
        xbcT_d = dscr("xbcT_d", [1024, S_TOK + 4], BF16)

        S = Sched(nc, es)
        psum = [es.enter_context(nc.psum_tensor(f"ps{i}", [128, 512], F32)) for i in range(7)]
        psT = es.enter_context(nc.psum_tensor("psT", [128, 1024], BF16))

        def sb(name, shape, dt):
            return es.enter_context(nc.sbuf_tensor(name, list(shape), dt))

        with contextlib.ExitStack() as pa:
            def sba(name, shape, dt):
                return pa.enter_context(nc.sbuf_tensor(name, list(shape), dt))
            wbf = sba("wbf", [128, 8, WCOLS], BF16)
            wst = [sba(f"wst{i}", [128, WCOLS // 2], F32) for i in range(2)]
            cos_sb = sba("cos_sb", [128, S_TOK], F32)
            sin_sb = sba("sin_sb", [128, S_TOK], F32)
            xst = [sba(f"xst{i}", [128, 8, 512], F32) for i in range(2)]
            xb = [sba(f"xb{i}", [128, 8, 512], BF16) for i in range(2)]
            t1 = [sba(f"t1_{i}", [128, 512], F32) for i in range(2)]
            t2 = [sba(f"t2_{i}", [128, 512], F32) for i in range(2)]
            ob = [sba(f"ob{i}", [128, 512], BF16) for i in range(4)]
            of = [sba(f"of{i}", [128, 512], F32) for i in range(2)]
            dts = sba("dts", [128, 512], F32)
            zpad = sba("zpad", [128, 8, 2], BF16)

            S.dma(lambda e: e.dma_start(out=cos_sb[:], in_=cosT), writes=["cos_sb"])
            S.dma(lambda e: e.dma_start(out=sin_sb[:], in_=sinT), writes=["sin_sb"])
            S.op("pool", lambda e: e.memset(zpad[:], 0.0), writes=["zpad"])
            xbc_rows = xbcT_d.rearrange("(c p) t -> p c t", p=128)
            S.dma(lambda e: e.dma_start(out=xbc_rows[:, :, 0:2], in_=zpad[:]), reads=["zpad"], writes=["xbcpadL"])
            S.dma(lambda e: e.dma_start(out=xbc_rows[:, :, S_TOK + 2:S_TOK + 4], in_=zpad[:]), reads=["zpad"], writes=["xbcpadR"])
            H = WCOLS // 2
            n = 0
            for hf in range(2):
                for kc in range(8):
                    st = wst[n % 2]
                    S.dma(lambda e, st=st, kc=kc, hf=hf: e.dma_start(out=st[:], in_=w_in[kc * 128:(kc + 1) * 128, hf * H:(hf + 1) * H]),
                          writes=[f"wst{n % 2}"])
                    if n % 2 == 0:
                        S.op("dve", lambda e, st=st, kc=kc, hf=hf: e.tensor_copy(out=wbf[:, kc, hf * H:(hf + 1) * H], in_=st[:]),
                             reads=[f"wst{n % 2}"], writes=[f"wbf{kc}_{hf}"])
                    else:
                        S.op("act", lambda e, st=st, kc=kc, hf=hf: e.copy(out=wbf[:, kc, hf * H:(hf + 1) * H], in_=st[:]),
                             reads=[f"wst{n % 2}"], writes=[f"wbf{kc}_{hf}"])
                    n += 1
            def wres_for(kc, c0, c1):
                return [f"wbf{kc}_{hf}" for hf in range(2) if c0 < (hf + 1) * H and c1 > hf * H]
            xT_r = xT.rearrange("(c p) t -> p c t", p=128)
            pb = 0
            obi = 0
            for ch in range(8):
                t0 = ch * 512
                bi = ch % 2
                S.dma(lambda e, bi=bi, t0=t0: e.dma_start(out=xst[bi][:], in_=xT_r[:, :, t0:t0 + 512]), writes=[f"xst{bi}"])
                S.op("dve", lambda e, bi=bi: e.tensor_copy(out=xb[bi][:], in_=xst[bi][:]), reads=[f"xst{bi}"], writes=[f"xb{bi}"])
                xres = f"xb{bi}"

                def fm_tile(j, bank, bi=bi):
                    for kc in range(8):
                        S.op("pe", lambda e, j=j, kc=kc, bank=bank: e.matmul(
                            psum[bank][:, :], lhsT=wbf[:, kc, j * 128:(j + 1) * 128], rhs=xb[bi][:, kc, :],
                            start=(kc == 0), stop=(kc == 7)), reads=[xres] + wres_for(kc, j * 128, (j + 1) * 128), writes=[f"ps{bank}"])
                for which, dst in ((0, qT_d), (8, kT_d)):
                    for j in range(4):
                        ba, bb = pb % 6, (pb + 1) % 6
                        pb += 2
                        fm_tile(which + j, ba)
                        fm_tile(which + 4 + j, bb)
                        ti = j % 2
                        S.op("dve", lambda e, ba=ba, ti=ti, t0=t0: e.tensor_tensor(
                            out=t1[ti][:], in0=psum[ba][:, :], in1=cos_sb[:, t0:t0 + 512], op=ALU.mult),
                            reads=[f"ps{ba}", "cos_sb"], writes=[f"t1_{ti}"])
                        S.op("dve", lambda e, bb=bb, ti=ti, t0=t0: e.tensor_tensor(
                            out=t2[ti][:], in0=psum[bb][:, :], in1=sin_sb[:, t0:t0 + 512], op=ALU.mult),
                            reads=[f"ps{bb}", "sin_sb"], writes=[f"t2_{ti}"])
                        o = obi % 4
                        obi += 1
                        S.op("pool", lambda e, ti=ti, o=o: e.tensor_tensor(
                            out=ob[o][:], in0=t1[ti][:], in1=t2[ti][:], op=ALU.add),
                            reads=[f"t1_{ti}", f"t2_{ti}"], writes=[f"ob{o}"])
                        S.dma(lambda e, o=o, j=j, dst=dst, t0=t0: e.dma_start(
                            out=dst[j * 128:(j + 1) * 128, t0:t0 + 512], in_=ob[o][:]), reads=[f"ob{o}"], writes=[])
                for j in range(8):
                    ba = pb % 6
                    pb += 1
                    fm_tile(16 + j, ba)
                    o = obi % 4
                    obi += 1
                    S.op("act", lambda e, ba=ba, o=o: e.copy(out=ob[o][:], in_=psum[ba][:, :]),
                         reads=[f"ps{ba}"], writes=[f"ob{o}"])
                    S.dma(lambda e, o=o, j=j, t0=t0: e.dma_start(
                        out=xbcT_d[j * 128:(j + 1) * 128, 2 + t0:2 + t0 + 512], in_=ob[o][:]), reads=[f"ob{o}"], writes=[])
                for tt in range(4):
                    tg = ch * 4 + tt
                    for which in range(2):
                        ba = pb % 6
                        pb += 1
                        c0 = 3072 + which * 512
                        for kc in range(8):
                            S.op("pe", lambda e, kc=kc, ba=ba, tt=tt, c0=c0, bi=bi: e.matmul(
                                psum[ba][:, :], lhsT=xb[bi][:, kc, tt * 128:(tt + 1) * 128], rhs=wbf[:, kc, c0:c0 + 512],
                                start=(kc == 0), stop=(kc == 7)), reads=[xres] + wres_for(kc, c0, c0 + 512), writes=[f"ps{ba}"])
                        if which == 0:
                            o = obi % 4
                            obi += 1
                            S.op("act", lambda e, ba=ba, o=o: e.copy(out=ob[o][:], in_=psum[ba][:, :]),
                                 reads=[f"ps{ba}"], writes=[f"ob{o}"])
                            S.dma(lambda e, o=o, tg=tg: e.dma_start(out=v_d[tg * 128:(tg + 1) * 128, :], in_=ob[o][:]),
                                  reads=[f"ob{o}"], writes=[])
                        else:
                            o = tg % 2
                            S.op("act", lambda e, ba=ba, o=o: e.copy(out=of[o][:], in_=psum[ba][:, :]),
                                 reads=[f"ps{ba}"], writes=[f"of{o}"])
                            S.dma(lambda e, o=o, tg=tg: e.dma_start(out=z_d[tg * 128:(tg + 1) * 128, :], in_=of[o][:]),
                                  reads=[f"of{o}"], writes=[])
                    for kc in range(8):
                        S.op("pe", lambda e, kc=kc, tt=tt, tg=tg, bi=bi: e.matmul(
                            psum[6][:, tg * 16:(tg + 1) * 16], lhsT=xb[bi][:, kc, tt * 128:(tt + 1) * 128],
                            rhs=wbf[:, kc, 4096:4112], start=(kc == 0), stop=(kc == 7)),
                            reads=[xres] + wres_for(kc, 4096, 4112), writes=["ps6"])
            S.op("act", lambda e: e.copy(out=dts[:], in_=psum[6][:, :]), reads=["ps6"], writes=["dts"])
            S.dma(lambda e: e.dma_start(out=dt_d, in_=dts[:]), reads=["dts"], writes=[])

            S.end_phase()

        C = dict(nc=nc, S=S, psum=psum, debug=debug, din=din, dscr=dscr)
        C.update(qT_d=qT_d, kT_d=kT_d, v_d=v_d, z_d=z_d, dt_d=dt_d, xbcT_d=xbcT_d, x=x)
        C["psT"] = psT
        phase_attn(C)
        phase_conv(C)
        phase_ssd(C)
        C["gates_all"] = es.enter_context(nc.sbuf_tensor("gates_all", [128, 32, 4], F32))
        C["dest_all"] = es.enter_context(nc.sbuf_tensor("dest_all", [128, 32, 4], I32))
        C["flags_all"] = es.enter_context(nc.sbuf_tensor("flags_all", [128, N_EXP], I32))
        phase_mix(C)
        phase_experts(C)
        phase_combine(C)
        S.streams["sp"].append(("wait", "c_pe", S.cnt["pe"])) if False else None
        S.finish()
        S.emit()
    return nc


_CACHE = {}


def kernel(**inputs):
    debug = bool(inputs.pop("_debug", False))
    x = np.asarray(inputs["x"], dtype=np.float32)
    w_in = np.asarray(inputs["w_in"], dtype=np.float32)[0]
    perm = np.concatenate([np.concatenate([np.arange(32, 64), np.arange(0, 32)]) + 64 * h for h in range(8)])
    wq, wk, wv = w_in[:, 0:512], w_in[:, 512:1024], w_in[:, 1024:1536]
    wz, wxbc, wdt = w_in[:, 1536:2048], w_in[:, 2048:3072], w_in[:, 3072:3088]
    w_ext = np.ascontiguousarray(np.concatenate([wq, wq[:, perm], wk, wk[:, perm], wxbc, wv, wz, wdt], axis=1))
    consts = host_consts()
    g = lambda k: np.asarray(inputs[k], dtype=np.float32)[0]
    rep = lambda v: np.ascontiguousarray(np.broadcast_to(v[None, :], (128, v.shape[0])))
    consts["conv_wT"] = np.ascontiguousarray(g("conv_w").reshape(5, 8, 128).transpose(2, 1, 0).reshape(128, 40))
    consts["conv_bT"] = np.ascontiguousarray(g("conv_b").reshape(8, 128).T)
    consts["conv_b_row"] = np.ascontiguousarray(g("conv_b").reshape(1, 1024))
    consts["dtb_rep"] = rep(np.tile(np.concatenate([g("dt_bias_fwd"), g("dt_bias_bwd")]), 32))
    consts["alog_rep"] = rep(np.tile(np.concatenate([g("a_log_fwd"), g("a_log_bwd")]), 32))
    consts["dskip_rep"] = rep(np.repeat(g("d_skip"), 64))
    consts["ssdnw_rep"] = rep(g("ssd_norm_w"))
    consts["w_out"] = g("w_out")
    consts["anwT"] = np.ascontiguousarray(g("attn_norm_w").reshape(4, 128).T)
    consts["ln1w_rep"] = rep(g("ln1_w"))
    consts["ln1b_rep"] = rep(g("ln1_b"))
    consts["router_w"] = g("router_w")
    consts["rb_rep"] = rep(g("router_b"))
    consts["ecap_rep"] = rep((np.arange(N_EXP) * CAP).astype(np.float32))
    consts["w_gate"] = g("w_gate")
    consts["w_up"] = g("w_up")
    consts["w_down"] = g("w_down")
    consts["bgT"] = np.ascontiguousarray(g("b_gate").reshape(N_EXP, 8, 128).transpose(2, 0, 1).reshape(128, N_EXP * 8))
    consts["buT"] = np.ascontiguousarray(g("b_up").reshape(N_EXP, 8, 128).transpose(2, 0, 1).reshape(128, N_EXP * 8))
    consts["b_down"] = g("b_down")
    consts["ln2w_rep"] = rep(g("ln2_w"))
    consts["ln2b_rep"] = rep(g("ln2_b"))
    consts["c_SU2"] = consts["c_SU"]
    consts["c_ones2"] = consts["c_ones"]
    nc = build(debug=debug)
    in_maps = []
    for b in range(8):
        m = {"xT": np.ascontiguousarray(x[b].T), "x": np.ascontiguousarray(x[b]), "w_in_ext": w_ext}
        m.update(consts)
        in_maps.append(m)
    ncores = int(inputs.pop("_ncores", 8)) if "_ncores" in inputs else 8
    res = run_bass_kernel_spmd(nc, in_maps[:ncores], core_ids=list(range(ncores)))
    if debug:
        return res.results
    return np.stack([r["out"] for r in res.results], axis=0)
```

```python
import contextlib
import numpy as np
import ml_dtypes
import concourse.bass as bass
import concourse.mybir as mybir
from concourse.bass_utils import run_bass_kernel_spmd

F32 = mybir.dt.float32
BF16 = mybir.dt.bfloat16
U32 = mybir.dt.uint32
I32 = mybir.dt.int32
AF = mybir.ActivationFunctionType
ALU = mybir.AluOpType

S_TOK = 4096
D = 1024
NCH = 32
WCOLS = 3072 + 1040
N_EXP = 32
CAP = 1024


class Sched:
    COMPUTE = ("pe", "act", "dve", "pool")

    def __init__(self, nc, es, n_ring=12):
        self.nc = nc
        self.streams = {e: [] for e in ("pe", "act", "dve", "pool", "sp")}
        self.sem = {}
        for e in self.COMPUTE:
            self.sem[e] = es.enter_context(nc.semaphore("sem_" + e))
        self.cnt = {e: 0 for e in self.COMPUTE}
        self.ring = {}
        self.ring_i = {}
        self.ring_tot = {}
        for q, n in (("sp", n_ring), ("pool", 8), ("act", 6), ("dve", 6)):
            self.ring[q] = [es.enter_context(nc.semaphore(f"dq_{q}_{i}")) for i in range(n)]
            self.ring_i[q] = 0
        self.last_writer = {}
        self.readers = {}
        self.waited = {e: {} for e in self.streams}
        self.n_ins = 0

    def _deps(self, reads, writes):
        deps = {}

        def add(tok):
            k, v, e = tok
            if k not in deps or deps[k][0] < v:
                deps[k] = (v, e)
        for r in reads:
            if r in self.last_writer:
                add(self.last_writer[r])
        for w in writes:
            if w in self.last_writer:
                add(self.last_writer[w])
            for k, (v, e) in self.readers.get(w, {}).items():
                add((k, v, e))
        return deps

    def _emit_waits(self, eng, deps):
        for k, (v, src) in deps.items():
            if src == eng and eng == "pe":
                continue
            if self.waited[eng].get(k, 0) >= v:
                continue
            self.waited[eng][k] = v
            self.streams[eng].append(("wait", k, v))

    def _commit(self, tok, reads, writes):
        k, v, e = tok
        for w in writes:
            self.last_writer[w] = tok
            self.readers[w] = {}
        for r in reads:
            d = self.readers.setdefault(r, {})
            if k not in d or d[k][0] < v:
                d[k] = (v, e)

    def cond_begin(self, flag_ap):
        import copy
        self._cond = dict(n={e: 0 for e in self.streams}, rings={e: {} for e in self.streams},
                          snap=copy.deepcopy(self.waited))
        for e in self.streams:
            self.streams[e].append(("cond_begin", flag_ap))

    def cond_end(self):
        c = self._cond
        for e in self.streams:
            self.streams[e].append(("cond_end", c["n"][e], c["rings"][e]))
        self.waited = c["snap"]
        self._cond = None

    def op(self, eng, fn, reads=(), writes=()):
        deps = self._deps(reads, writes)
        self._emit_waits(eng, deps)
        if getattr(self, "_cond", None):
            self._cond["n"][eng] += 1
        self.cnt[eng] += 1
        key = "c_" + eng
        tok = (key, self.cnt[eng], eng)
        self.streams[eng].append(("ins", fn, self.sem[eng], 1))
        self._commit(tok, reads, writes)
        self.n_ins += 1

    def dma(self, fn, reads=(), writes=(), q="sp"):
        deps = self._deps(reads, writes)
        self._emit_waits(q, deps)
        i = self.ring_i[q]
        self.ring_i[q] = (i + 1) % len(self.ring[q])
        key = f"d_{q}_{i}"
        tot = self.ring_tot.get(key, 0)
        if tot > 0 and self.waited[q].get(key, 0) < tot:
            self.waited[q][key] = tot
            self.streams[q].append(("wait", key, tot))
        tot += 16
        self.ring_tot[key] = tot
        if getattr(self, "_cond", None):
            ent = self._cond["rings"][q].setdefault(key, [0, tot - 16])
            ent[0] += 1
        tok = (key, tot, "dma")
        self.streams[q].append(("ins", fn, self.ring[q][i], 16))
        self._commit(tok, reads, writes)
        self.n_ins += 1

    def _semh(self, key):
        if key.startswith("c_"):
            return self.sem[key[2:]]
        _, q, i = key.split("_")
        return self.ring[q][int(i)]

    def finish(self):
        for key, tot in self.ring_tot.items():
            if self.waited["sp"].get(key, 0) < tot:
                self.streams["sp"].append(("wait", key, tot))
                self.waited["sp"][key] = tot
        for e in self.COMPUTE:
            if self.cnt[e] and self.waited["sp"].get("c_" + e, 0) < self.cnt[e]:
                self.streams["sp"].append(("wait", "c_" + e, self.cnt[e]))

    def barrier(self):
        for eng in self.streams:
            for key, tot in self.ring_tot.items():
                if self.waited[eng].get(key, 0) < tot:
                    self.streams[eng].append(("wait", key, tot))
                    self.waited[eng][key] = tot
            for e in self.COMPUTE:
                if e != eng and self.cnt[e] and self.waited[eng].get("c_" + e, 0) < self.cnt[e]:
                    self.streams[eng].append(("wait", "c_" + e, self.cnt[e]))
                    self.waited[eng]["c_" + e] = self.cnt[e]

    def bc(self, e):
        if getattr(self, "_bc", None) is None:
            self._bc = e.to_reg(NROWS - 1)
        return self._bc

    def end_phase(self):
        self._bc_prev = getattr(self, "_bc", None)
        print("phase end n_ins", self.n_ins, {k: len(v) for k, v in self.streams.items()}, flush=True)
        self.barrier()
        self.emit()
        self._bc = None
        self.streams = {e: [] for e in self.streams}
        self.last_writer = {}
        self.readers = {}

    def emit(self):
        nc = self.nc
        with nc.Block() as block:
            def mk(name):
                def body(eng):
                    items = self.streams[name]
                    reg = None
                    guard = None
                    i = 0
                    while i < len(items):
                        it = items[i]
                        if it[0] == "wait":
                            eng.wait_ge(self._semh(it[1]), it[2])
                        elif it[0] == "ins":
                            it[1](eng).then_inc(it[2], it[3])
                        elif it[0] == "cond_begin":
                            j = i + 1
                            while items[j][0] != "cond_end":
                                j += 1
                            if j == i + 1:
                                i = j + 1
                                continue
                            if reg is None:
                                self._uid = getattr(self, "_uid", 0) + 1
                                reg = eng.alloc_register(f"cr_{name}_{self._uid}")
                            eng.reg_load(reg, it[1])
                            guard = eng.If_ne(reg, 0)
                            guard.__enter__()
                        elif it[0] == "cond_end":
                            guard.__exit__(None, None, None)
                            g2 = eng.Else()
                            g2.__enter__()
                            if it[1] and name in self.sem:
                                left = it[1]
                                while left > 0:
                                    k_ = min(16, left)
                                    eng.drain().then_inc(self.sem[name], k_)
                                    left -= k_
                            for key, (cnt_, before_) in it[2].items():
                                if before_ > 0:
                                    eng.wait_ge(self._semh(key), before_)
                                for _ in range(cnt_):
                                    eng.drain().then_inc(self._semh(key), 16)
                            g2.__exit__(None, None, None)
                            guard = None
                        i += 1
                return body
            if self.streams["sp"]:
                block.sync(mk("sp"))
            if self.streams["pe"]:
                block.tensor(mk("pe"))
            if self.streams["act"]:
                block.scalar(mk("act"))
            if self.streams["dve"]:
                block.vector(mk("dve"))
            if self.streams["pool"]:
                block.gpsimd(mk("pool"))


def host_consts():
    c = {}
    pos = np.arange(S_TOK, dtype=np.float32)
    inv = (np.float32(10000.0) ** (-np.arange(0, 64, 2, dtype=np.float32) / np.float32(64))).astype(np.float32)
    ang = (pos[:, None] * inv[None, :]).astype(np.float32)
    cos = np.cos(ang).astype(np.float32).T
    sin = np.sin(ang).astype(np.float32).T
    cosT = np.concatenate([cos, cos, cos, cos], 0)
    sinT = np.concatenate([-sin, sin, -sin, sin], 0)
    kk = np.arange(128)[:, None]
    nn = np.arange(256)[None, :]
    c["negmask"] = np.where((kk <= nn) & (nn <= kk + 128), 0.0, -30000.0).astype(ml_dtypes.bfloat16)
    c["ident_bf"] = np.eye(128, dtype=np.float32).astype(ml_dtypes.bfloat16)
    sel = np.zeros((65, 64), np.float32)
    sel[64, :] = 1.0
    c["sel65"] = sel
    t_ = np.arange(128)
    c["c_SL"] = (t_[:, None] > t_[None, :]).astype(np.float32)
    c["c_UI"] = (t_[:, None] <= t_[None, :]).astype(np.float32)
    c["c_SU"] = (t_[:, None] < t_[None, :]).astype(np.float32)
    c["c_LI"] = (t_[:, None] >= t_[None, :]).astype(np.float32)
    c["c_ones"] = np.ones((128, 128), np.float32)
    c["cosT"] = np.ascontiguousarray(cosT)
    c["sinT"] = np.ascontiguousarray(sinT)
    return c


def phase_attn(C):
    nc, S, psum = C["nc"], C["S"], C["psum"]
    qT_d, kT_d, v_d = C["qT_d"], C["kT_d"], C["v_d"]
    negmask_d = C["din"]("negmask", [128, 256], BF16)
    ident_d = C["din"]("ident_bf", [128, 128], BF16)
    sel_d = C["din"]("sel65", [65, 64], F32)
    attnT_d = C["dscr"]("attnT_d", [512, S_TOK], BF16)
    C["attnT_d"] = attnT_d
    C["ident_d"] = ident_d
    with contextlib.ExitStack() as ph:
        def sba(name, shape, dt):
            return ph.enter_context(nc.sbuf_tensor(name, list(shape), dt))
        negmask = sba("negmask_sb", [128, 256], BF16)
        ident = sba("ident_sb", [128, 128], BF16)
        sel = sba("sel_sb", [65, 64], F32)
        V = [sba(f"V{i}", [128, 32, 4, 65], BF16) for i in range(3)]
        vst = [sba(f"vst{i}", [128, 8, 256], BF16) for i in range(2)]
        acc = [sba(f"acc{i}", [128, S_TOK], F32) for i in range(2)]
        qh = [sba(f"qh{i}", [128, S_TOK], BF16) for i in range(2)]
        kh = [sba(f"kh{i}", [128, S_TOK], BF16) for i in range(2)]
        pT = [sba(f"pT{i}", [128, 256], BF16) for i in range(4)]
        rec = [sba(f"rec{i}", [64, 512], F32) for i in range(2)]
        outb = [sba(f"aob{i}", [64, 512], BF16) for i in range(2)]
        S.dma(lambda e: e.dma_start(out=negmask[:], in_=negmask_d), writes=["negmask"])
        S.dma(lambda e: e.dma_start(out=ident[:], in_=ident_d), writes=["ident"])
        S.dma(lambda e: e.dma_start(out=sel[:], in_=sel_d), writes=["sel"])
        for i in range(3):
            S.op("pool", lambda e, i=i: e.memset(V[i][:], 1.0), writes=[f"V{i}"])
        PAT = (1, 4, 16)
        scb = [psum[0], psum[1], C["psT"][:, :].bitcast(F32)]
        out_banks = (2, 3, 4)
        cnt = dict(vs=0, sc=0, pt=0, ob=0, nb=0)

        for i in range(2):
            S.op("pool", lambda e, i=i: e.memset(kh[i][:], 0.0), writes=[f"kh{i}"])

        def load_qk(h):
            b = h % 2
            if h % 2 == 0:
                p = h // 2
                S.dma(lambda e: e.dma_start(out=qh[p % 2][:], in_=qT_d[p * 128:(p + 1) * 128, :]), writes=[f"qh{p % 2}"])
            S.dma(lambda e: e.dma_start(out=kh[b][b * 64:(b + 1) * 64, :], in_=kT_d[h * 64:(h + 1) * 64, :]), writes=[f"kh{b}"])

        def load_V(hh):
            for pi, r in enumerate(PAT):
                n_t = 32 // r
                view4 = v_d.rearrange("(u k r) c -> k r u c", r=r, k=128)
                for gi in range(4):
                    b = cnt["vs"] % 2
                    cnt["vs"] += 1
                    cs = slice(hh * 256, (hh + 1) * 256)
                    if r == 1:
                        src = view4[:, 0, 8 * gi:8 * gi + 8, cs]
                        dst = vst[b][:]
                    elif r == 4:
                        src = view4[:, gi, :, cs]
                        dst = vst[b][:]
                    else:
                        src = None
                        for j in range(4):
                            srcj = view4[:, 4 * gi + j, :, cs]
                            dstj = vst[b][:, 2 * j:2 * j + 2, :]
                            S.dma(lambda e, src=srcj, dst=dstj: e.dma_start(out=dst, in_=src), writes=[f"vst{b}"])
                    if src is not None:
                        S.dma(lambda e, src=src, dst=dst: e.dma_start(out=dst, in_=src), writes=[f"vst{b}"])
                    eng = ("pool", "dve")[gi % 2]
                    S.op(eng, lambda e, pi=pi, gi=gi, b=b: e.tensor_copy(
                        out=V[pi][:, 8 * gi:8 * gi + 8, :, 0:64],
                        in_=vst[b][:].rearrange("p t (h d) -> p t h d", h=4)),
                        reads=[f"vst{b}"], writes=[f"V{pi}"])

        def head(h):
            hb = h % 2
            hl = h % 4
            ab = h % 2
            accr = f"acc{ab}"
            tiles = []
            for pi, r in enumerate(PAT):
                L = S_TOK // r
                n_t = L // 128
                for rho in range(r):
                    nbanks = (L + 64 + 511) // 512
                    base = cnt["nb"]
                    cnt["nb"] += nbanks
                    for u in range(n_t):
                        tiles.append((pi, r, rho, u, n_t, L, base, nbanks))

            def score(tl):
                pi, r, rho, u, n_t, L, base, nbanks = tl
                m_lo = max(0, 128 * u - 64)
                m_hi = min(L, 128 * u + 192)
                N = m_hi - m_lo
                c0 = m_lo - (128 * u - 64)
                sl_ = cnt["sc"] % 3
                cnt["sc"] += 1
                sbk, so = sl_, 0
                pb_ = cnt["pt"] % 4
                cnt["pt"] += 1
                k0 = rho + r * 128 * u
                kc = kh[hb][:, k0:k0 + r * 127 + 1:r]
                qb_ = (h // 2) % 2
                qc = qh[qb_][:, rho + r * m_lo:rho + r * (m_hi - 1) + 1:r]
                S.op("pe", lambda e: e.matmul(scb[sbk][:, so:so + N], lhsT=kc, rhs=qc, start=True, stop=False),
                     reads=[f"qh{qb_}", f"kh{hb}"], writes=[f"sc{sl_}"])
                S.op("pe", lambda e: e.matmul(scb[sbk][:, so:so + N], lhsT=ident[:], rhs=negmask[:, c0:c0 + N],
                                               start=False, stop=True),
                     reads=["ident", "negmask"], writes=[f"sc{sl_}"])
                S.op("act", lambda e: e.activation(out=pT[pb_][:, 0:N], in_=scb[sbk][:, so:so + N], func=AF.Exp, scale=0.125),
                     reads=[f"sc{sl_}"], writes=[f"pT{pb_}"])
                return (pb_, m_lo, m_hi)

            def pv(tl, info):
                pi, r, rho, u, n_t, L, base, nbanks = tl
                pb_, m_lo, m_hi = info

                def phys(gb):
                    return out_banks[(base + gb) % 3]
                ti = rho * n_t + u
                vl = V[pi][:, ti, hl, :]
                nA = (128 * u + 64) - m_lo
                cA = m_lo + 64
                gA, colA = cA // 512, cA % 512
                pA = phys(gA)
                S.op("pe", lambda e: e.matmul(psum[pA][0:65, colA:colA + nA], lhsT=vl, rhs=pT[pb_][:, 0:nA],
                                               start=(u == 0), stop=True),
                     reads=[f"V{pi}", f"pT{pb_}"], writes=[f"ps{pA}"])
                nB = m_hi - (128 * u + 64)
                cB = 128 * (u + 1)
                gB, colB = cB // 512, cB % 512
                pB = phys(gB)
                lastu = (u == n_t - 1)
                S.op("pe", lambda e: e.matmul(psum[pB][0:65, colB:colB + nB], lhsT=vl, rhs=pT[pb_][:, nA:nA + nB],
                                               start=True, stop=lastu),
                     reads=[f"V{pi}", f"pT{pb_}"], writes=[f"ps{pB}"])
                for gb in range(nbanks):
                    if min(4 * gb + 3, n_t - 1) == u:
                        ma = max(0, 512 * gb - 64)
                        mb = min(L, 512 * gb + 448)
                        ca = ma + 64 - 512 * gb
                        n = mb - ma
                        asl = acc[ab][0:65, rho + r * ma:rho + r * (mb - 1) + 1:r]
                        pg_ = phys(gb)
                        psl = psum[pg_][0:65, ca:ca + n]
                        if pi == 0:
                            S.op("dve", lambda e, asl=asl, psl=psl: e.tensor_copy(out=asl, in_=psl),
                                 reads=[f"ps{pg_}"], writes=[accr])
                        else:
                            S.op("dve", lambda e, asl=asl, psl=psl: e.tensor_tensor(out=asl, in0=asl, in1=psl, op=ALU.add),
                                 reads=[f"ps{pg_}", accr], writes=[accr])

            LA = 2
            infos = []
            for k_ in range(min(LA, len(tiles))):
                infos.append(score(tiles[k_]))
            for k_ in range(len(tiles)):
                if k_ + LA < len(tiles):
                    infos.append(score(tiles[k_ + LA]))
                pv(tiles[k_], infos[k_])
            for c in range(8):
                db = 5 + (c % 2)
                rb = c % 2
                S.op("pe", lambda e, c=c, db=db: e.matmul(psum[db][0:64, :], lhsT=sel[:], rhs=acc[ab][0:65, c * 512:(c + 1) * 512],
                                                           start=True, stop=True),
                     reads=[accr, "sel"], writes=[f"ps{db}"])
                S.op("dve", lambda e, db=db, rb=rb: e.reciprocal(out=rec[rb][:], in_=psum[db][0:64, :]),
                     reads=[f"ps{db}"], writes=[f"rec{rb}"])
                S.op("pool", lambda e, c=c, rb=rb: e.tensor_tensor(out=outb[rb][:], in0=acc[ab][0:64, c * 512:(c + 1) * 512],
                                                                     in1=rec[rb][:], op=ALU.mult),
                     reads=[accr, f"rec{rb}"], writes=[f"aob{rb}"])
                S.dma(lambda e, c=c, rb=rb: e.dma_start(out=attnT_d[h * 64:(h + 1) * 64, c * 512:(c + 1) * 512], in_=outb[rb][:]),
                      reads=[f"aob{rb}"], writes=[])

        load_qk(0)
        for hh in range(2):
            load_V(hh)
            for hl_ in range(4):
                h = hh * 4 + hl_
                if h + 1 < 8:
                    load_qk(h + 1)
                head(h)
        S.end_phase()


def X(S, eng, meth, reads, writes, *a, **kw):
    S.op(eng, lambda e: getattr(e, meth)(*a, **kw), reads, writes)


def DM(S, out, in_, reads=(), writes=(), q="sp"):
    S.dma(lambda e: e.dma_start(out=out, in_=in_), reads, writes, q)


def phase_conv(C):
    nc, S, psum = C["nc"], C["S"], C["psum"]
    cwT_d = C["din"]("conv_wT", [128, 40])
    cbT_d = C["din"]("conv_bT", [128, 8])
    cbrow_d = C["din"]("conv_b_row", [1, 1024])
    bcT_d = C["dscr"]("bcT_d", [512, S_TOK], BF16)
    xsB_d = C["dscr"]("xsB_d", [S_TOK, 768], BF16)
    C["bcT_d"], C["xsB_d"] = bcT_d, xsB_d
    with contextlib.ExitStack() as ph:
        def sba(name, shape, dt):
            return ph.enter_context(nc.sbuf_tensor(name, list(shape), dt))
        xpre = sba("xpre", [128, 8, S_TOK + 4], BF16)
        dg = sba("dg", [128, 40, 128], BF16)
        ident = sba("ident_c", [128, 128], BF16)
        cwT = sba("cwT", [128, 40], F32)
        cbT = sba("cbT", [128, 8], F32)
        cbrow = sba("cbrow", [1, 1024], F32)
        cbrow_bf = sba("cbrow_bf", [1, 1024], BF16)
        ones1 = sba("ones1", [1, 128], BF16)
        ob = [sba(f"cob{i}", [128, 512], BF16) for i in range(3)]
        DM(S, ident[:], C["ident_d"], writes=["ident_c"])
        DM(S, cwT[:], cwT_d, writes=["cwT"])
        DM(S, cbT[:], cbT_d, writes=["cbT"])
        DM(S, cbrow[:], cbrow_d, writes=["cbrow"])
        xv = C["xbcT_d"].rearrange("(c p) t -> p c t", p=128)
        for j in range(8):
            DM(S, xpre[:, j, :], xv[:, j, :], writes=[f"xpre{j}"])
        X(S, "dve", "tensor_copy", ["cbrow"], ["cbrow_bf"], out=cbrow_bf[:], in_=cbrow[:])
        X(S, "pool", "memset", [], ["ones1"], ones1[:], 1.0)
        for jk in range(40):
            X(S, "dve", "tensor_scalar", ["ident_c", "cwT"], ["dg"], out=dg[:, jk, :], in0=ident[:],
              scalar1=cwT[:, jk:jk + 1], scalar2=None, op0=ALU.mult)
        bi = 0
        oi = 0
        for j in range(4, 8):
            for tc in range(8):
                b = bi % 2
                bi += 1
                for k in range(5):
                    X(S, "pe", "matmul", [f"xpre{j}", "dg"], [f"ps{b}"], psum[b][:, :], lhsT=dg[:, j * 5 + k, :],
                      rhs=xpre[:, j, tc * 512 + k:tc * 512 + k + 512], start=(k == 0), stop=(k == 4))
                o = oi % 3
                oi += 1
                X(S, "act", "activation", [f"ps{b}", "cbT"], [f"cob{o}"], out=ob[o][:], in_=psum[b][:, :], func=AF.Silu,
                  bias=cbT[:, j:j + 1])
                DM(S, bcT_d[(j - 4) * 128:(j - 3) * 128, tc * 512:(tc + 1) * 512], ob[o][:], reads=[f"cob{o}"])
        sz_d = C["dscr"]("sz_d", [S_TOK, 512], F32)
        C["sz_d"] = sz_d
        zin = [sba(f"zin{i}", [128, 512], F32) for i in range(2)]
        zou = [sba(f"zou{i}", [128, 512], F32) for i in range(2)]
        for tg in range(32):
            zb_ = tg % 2
            DM(S, zin[zb_][:], C["z_d"][tg * 128:(tg + 1) * 128, :], writes=[f"zin{zb_}"])
            X(S, "act", "activation", [f"zin{zb_}"], [f"zou{zb_}"], out=zou[zb_][:], in_=zin[zb_][:], func=AF.Silu)
            DM(S, sz_d[tg * 128:(tg + 1) * 128, :], zou[zb_][:], reads=[f"zou{zb_}"])
            bA = 2 + (tg % 2) * 2
            bB = bA + 1
            for j in range(6):
                bank = bA if j < 4 else bB
                col = (j % 4) * 128
                for k in range(5):
                    X(S, "pe", "matmul", [f"xpre{j}", "dg"], [f"ps{bank}"], psum[bank][:, col:col + 128],
                      lhsT=xpre[:, j, tg * 128 + k:tg * 128 + k + 128], rhs=dg[:, j * 5 + k, :], start=(k == 0), stop=False)
                X(S, "pe", "matmul", ["ones1", "cbrow_bf"], [f"ps{bank}"], psum[bank][:, col:col + 128],
                  lhsT=ones1[0:1, :], rhs=cbrow_bf[0:1, j * 128:(j + 1) * 128], start=False, stop=True)
            o = oi % 3
            oi += 1
            X(S, "act", "activation", [f"ps{bA}"], [f"cob{o}"], out=ob[o][:], in_=psum[bA][:, :], func=AF.Silu)
            DM(S, xsB_d[tg * 128:(tg + 1) * 128, 0:512], ob[o][:], reads=[f"cob{o}"])
            o = oi % 3
            oi += 1
            X(S, "act", "activation", [f"ps{bB}"], [f"cob{o}"], out=ob[o][:, 0:256], in_=psum[bB][:, 0:256], func=AF.Silu)
            DM(S, xsB_d[tg * 128:(tg + 1) * 128, 512:768], ob[o][:, 0:256], reads=[f"cob{o}"])
        S.end_phase()


def phase_ssd(C):
    nc, S, psum = C["nc"], C["S"], C["psum"]
    psT = C["psT"]
    din = C["din"]
    cst = {k: din(k, [128, 128]) for k in ("c_SL", "c_UI", "c_SU", "c_LI", "c_ones")}
    dtb_d = din("dtb_rep", [128, 512])
    alog_d = din("alog_rep", [128, 512])
    dskip_d = din("dskip_rep", [128, 512])
    nw_d = din("ssdnw_rep", [128, 512])
    ssdT_d = C["dscr"]("ssdT_d", [512, S_TOK], BF16)
    C["ssdT_d"] = ssdT_d
    xsB_d, bcT_d, z_d, dt_d = C["xsB_d"], C["bcT_d"], C["sz_d"], C["dt_d"]
    with contextlib.ExitStack() as ph:
        def sba(name, shape, dt):
            return ph.enter_context(nc.sbuf_tensor(name, list(shape), dt))
        K = {k: sba("k_" + k, [128, 128], F32) for k in cst}
        for k in cst:
            DM(S, K[k][:], cst[k], writes=[k])
        ident = sba("ident_s", [128, 128], BF16)
        DM(S, ident[:], C["ident_d"], writes=["ident_s"])
        G = {}
        for nm in ("dtr", "dtb", "alog", "dskip", "nw", "u", "au", "dt", "eA", "a", "tot", "acs", "cd", "d1", "d2",
                   "sarg", "oarg", "sdec", "odec", "wdt"):
            G[nm] = sba("g_" + nm, [128, 512], F32)
        DM(S, G["dtr"][:], dt_d, writes=["dtr"])
        DM(S, G["dtb"][:], dtb_d, writes=["dtb"])
        DM(S, G["alog"][:], alog_d, writes=["alog"])
        DM(S, G["dskip"][:], dskip_d, writes=["dskip"])
        DM(S, G["nw"][:], nw_d, writes=["nw"])

        def g(nm):
            return G[nm][:]

        def g3(nm):
            return G[nm][:].rearrange("p (c h) -> p c h", h=16)
        X(S, "dve", "tensor_tensor", ["dtr", "dtb"], ["u"], out=g("u"), in0=g("dtr"), in1=g("dtb"), op=ALU.add)
        X(S, "act", "activation", ["u"], ["au"], out=g("au"), in_=g("u"), func=AF.Exp)
        X(S, "act", "activation", ["au"], ["dt"], out=g("dt"), in_=g("au"), func=AF.Ln, bias=1.0)
        X(S, "act", "activation", ["alog"], ["eA"], out=g("eA"), in_=g("alog"), func=AF.Exp)
        X(S, "dve", "scalar_tensor_tensor", ["dt", "eA"], ["a"], out=g("a"), in0=g("dt"), scalar=-1.0, in1=g("eA"),
          op0=ALU.mult, op1=ALU.mult)
        X(S, "pe", "matmul", ["c_ones", "a"], ["ps0"], psum[0][:, :], lhsT=K["c_ones"][:], rhs=g("a"), start=True, stop=True)
        X(S, "pe", "matmul", ["c_UI", "a"], ["ps1"], psum[1][:, :], lhsT=K["c_UI"][:], rhs=g("a"), start=True, stop=True)
        X(S, "act", "copy", ["ps0"], ["tot"], out=g("tot"), in_=psum[0][:, :])
        X(S, "act", "copy", ["ps1"], ["acs"], out=g("acs"), in_=psum[1][:, :])
        X(S, "act", "activation", ["tot"], ["cd"], out=g("cd"), in_=g("tot"), func=AF.Exp)
        X(S, "dve", "tensor_tensor", ["tot", "acs"], ["d1"], out=g("d1"), in0=g("tot"), in1=g("acs"), op=ALU.subtract)
        X(S, "dve", "tensor_tensor", ["acs", "a"], ["d2"], out=g("d2"), in0=g("acs"), in1=g("a"), op=ALU.subtract)
        X(S, "pool", "tensor_copy", ["d1"], ["sarg"], out=g3("sarg")[:, :, 0:8], in_=g3("d1")[:, :, 0:8])
        X(S, "pool", "tensor_copy", ["d2"], ["sarg"], out=g3("sarg")[:, :, 8:16], in_=g3("d2")[:, :, 8:16])
        X(S, "pool", "tensor_copy", ["acs"], ["oarg"], out=g3("oarg")[:, :, 0:8], in_=g3("acs")[:, :, 0:8])
        X(S, "dve", "tensor_tensor", ["d1", "a"], ["oarg"], out=g3("oarg")[:, :, 8:16], in0=g3("d1")[:, :, 8:16],
          in1=g3("a")[:, :, 8:16], op=ALU.add)
        X(S, "act", "activation", ["sarg"], ["sdec"], out=g("sdec"), in_=g("sarg"), func=AF.Exp)
        X(S, "act", "activation", ["oarg"], ["odec"], out=g("odec"), in_=g("oarg"), func=AF.Exp)
        X(S, "dve", "tensor_tensor", ["dt", "sdec"], ["wdt"], out=g("wdt"), in0=g("dt"), in1=g("sdec"), op=ALU.mult)

        Fs = [[sba(f"Fs{d}_{k}", [128, 512], F32) for k in range(2)] for d in range(2)]
        Fstore = [sba(f"Fstore{d}", [128, 32, 512], BF16) for d in range(2)]
        xsB = [sba(f"xsB{i}", [128, 768], BF16) for i in range(4)]
        xsd = [sba(f"xsd{i}", [128, 512], BF16) for i in range(4)]
        for d in range(2):
            X(S, "pool", "memset", [], [f"Fs{d}_0"], Fs[d][0][:], 0.0)

        def p1_load(i_):
            for d in range(2):
                c = i_ if d == 0 else 31 - i_
                b = (i_ % 2) * 2 + d
                DM(S, xsB[b][:], xsB_d[c * 128:(c + 1) * 128, :], writes=[f"xsB{b}"])
        p1_load(0)
        for i_ in range(32):
            if i_ + 1 < 32:
                p1_load(i_ + 1)
            for d in range(2):
                c = i_ if d == 0 else 31 - i_
                b = (i_ % 2) * 2 + d
                cur, nxt = Fs[d][i_ % 2], Fs[d][(i_ + 1) % 2]
                rc, rn = f"Fs{d}_{i_ % 2}", f"Fs{d}_{(i_ + 1) % 2}"
                wv = g3("wdt")[:, c, d * 8:d * 8 + 8].unsqueeze(2).to_broadcast([128, 8, 64])
                X(S, "pool" if d == 0 else "dve", "tensor_tensor", [f"xsB{b}", "wdt"], [f"xsd{b}"],
                  out=xsd[b][:].rearrange("p (h d) -> p h d", h=8), in0=xsB[b][:, 0:512].rearrange("p (h d) -> p h d", h=8),
                  in1=wv, op=ALU.mult)
                pb = 1 + d
                for gq in range(2):
                    X(S, "pe", "matmul", [f"xsB{b}", f"xsd{b}"], [f"ps{pb}"], psum[pb][:, gq * 256:(gq + 1) * 256],
                      lhsT=xsB[b][:, 512 + gq * 128:512 + (gq + 1) * 128], rhs=xsd[b][:, gq * 256:(gq + 1) * 256],
                      start=True, stop=True)
                X(S, "act", "copy", [rc], [f"Fst{d}_{c}"], out=Fstore[d][:, c, :], in_=cur[:])
                cv = g3("cd")[:, c, d * 8:d * 8 + 8].unsqueeze(2).to_broadcast([128, 8, 64])
                X(S, "dve", "tensor_tensor", [rc, "cd"], [rn], out=nxt[:].rearrange("p (h d) -> p h d", h=8),
                  in0=cur[:].rearrange("p (h d) -> p h d", h=8), in1=cv, op=ALU.mult)
                X(S, "dve", "tensor_tensor", [rn, f"ps{pb}"], [rn], out=nxt[:], in0=nxt[:], in1=psum[pb][:, :], op=ALU.add)

        bc = [sba(f"bc{i}", [128, 4, 128], BF16) for i in range(3)]
        zt = [sba(f"zt{i}", [128, 512], F32) for i in range(3)]
        xdt = [sba(f"xdt{i}", [128, 512], BF16) for i in range(2)]
        Gm = [sba(f"Gm{i}", [128, 2, 128], BF16) for i in range(2)]
        lhs4 = [sba(f"lhs4_{i}", [128, 4, 128], F32) for i in range(2)]
        E4 = [sba(f"E4_{i}", [128, 4, 128], BF16) for i in range(2)]
        M4 = [sba(f"M4_{i}", [128, 4, 128], BF16) for i in range(2)]
        tA = sba("tA", [128, 512], F32)
        tB = sba("tB", [128, 512], F32)
        tU = sba("tU", [128, 512], F32)
        ez = sba("ez", [128, 512], F32)
        junk = sba("junk", [128, 256], F32)
        ss = sba("ss", [128, 2], F32)
        yb = sba("yb", [128, 512], BF16)
        yT = [sba(f"yT{i}", [128, 4, 128], BF16) for i in range(3)]
        bcv = bcT_d.rearrange("(q p) t -> p q t", p=128)
        ssdv = ssdT_d.rearrange("(q p) t -> p q t", p=128)
        qs = dict(qi=0)
        def p2_loads(c):
            b = c % 3
            DM(S, xsB[b][:], xsB_d[c * 128:(c + 1) * 128, :], writes=[f"xsB{b}"])
            DM(S, bc[b][:], bcv[:, :, c * 128:(c + 1) * 128], writes=[f"bc{b}"])
            DM(S, zt[b][:], z_d[c * 128:(c + 1) * 128, :], writes=[f"zt{b}"])
        def p2_front(c):
            b = c % 3
            xs3 = xsB[b][:, 0:512].rearrange("p (h d) -> p h d", h=8)
            for d in range(2):
                dv = g3("dt")[:, c, d * 8:d * 8 + 8].unsqueeze(2).to_broadcast([128, 8, 64])
                X(S, "dve", "tensor_tensor", [f"xsB{b}", "dt"], [f"xdt{d}"], out=xdt[d][:].rearrange("p (h d) -> p h d", h=8),
                  in0=xs3, in1=dv, op=ALU.mult)
            for gq in range(2):
                X(S, "pe", "matmul", [f"bc{b}"], ["ps0"], psum[0][:, gq * 128:(gq + 1) * 128], lhsT=bc[b][:, gq, :],
                  rhs=bc[b][:, 2 + gq, :], start=True, stop=True)
            p0v = psum[0][:, 0:256].rearrange("p (g l) -> p g l", g=2)
            X(S, "dve", "tensor_tensor", ["ps0", "c_UI"], ["Gm0"], out=Gm[0][:], in0=p0v,
              in1=K["c_UI"][:].unsqueeze(1).to_broadcast([128, 2, 128]), op=ALU.mult)
            X(S, "dve", "tensor_tensor", ["ps0", "c_LI"], ["Gm1"], out=Gm[1][:], in0=p0v,
              in1=K["c_LI"][:].unsqueeze(1).to_broadcast([128, 2, 128]), op=ALU.mult)
            for d in range(2):
                for gq in range(2):
                    q = qs["qi"] % 2
                    qs["qi"] += 1
                    sb_ = 1 + q
                    h0 = c * 16 + d * 8 + gq * 4
                    av = G["a"][:, h0:h0 + 4].unsqueeze(2).to_broadcast([128, 4, 128])
                    tri = K["c_SL" if d == 0 else "c_SU"][:].unsqueeze(1).to_broadcast([128, 4, 128])
                    X(S, "dve", "tensor_tensor", ["a", "c_SL", "c_SU"], [f"lhs4_{q}"], out=lhs4[q][:], in0=tri, in1=av, op=ALU.mult)
                    rk = K["c_UI" if d == 0 else "c_LI"]
                    for i in range(4):
                        X(S, "pe", "matmul", [f"lhs4_{q}", "c_UI", "c_LI"], [f"ps{sb_}"], psum[sb_][:, i * 128:(i + 1) * 128],
                          lhsT=lhs4[q][:, i, :], rhs=rk[:], start=True, stop=True)
                    X(S, "act", "activation", [f"ps{sb_}"], [f"E4_{q}"], out=E4[q][:].rearrange("p a b -> p (a b)"),
                      in_=psum[sb_][:, :], func=AF.Exp)
                    X(S, "pool", "tensor_tensor", [f"E4_{q}", f"Gm{d}"], [f"M4_{q}"], out=M4[q][:], in0=E4[q][:],
                      in1=Gm[d][:, gq, :].unsqueeze(1).to_broadcast([128, 4, 128]), op=ALU.mult)
                    yb_ = 3 + d
                    for i in range(4):
                        h = gq * 4 + i
                        X(S, "pe", "matmul", [f"M4_{q}", f"xdt{d}"], [f"ps{yb_}"], psum[yb_][:, h * 64:(h + 1) * 64],
                          lhsT=M4[q][:, i, :], rhs=xdt[d][:, h * 64:(h + 1) * 64], start=True, stop=True)
                    X(S, "pe", "matmul", [f"bc{b}", f"Fst{d}_{c}"], [f"ps{5 + d}"], psum[5 + d][:, gq * 256:(gq + 1) * 256],
                      lhsT=bc[b][:, 2 + gq, :], rhs=Fstore[d][:, c, gq * 256:(gq + 1) * 256], start=True, stop=True)

        def p2_epi1(c):
            b = c % 3
            t3 = lambda t: t[:].rearrange("p (h d) -> p h d", h=8)
            for d, tt in ((0, tA), (1, tB)):
                ov = g3("odec")[:, c, d * 8:d * 8 + 8].unsqueeze(2).to_broadcast([128, 8, 64])
                X(S, "dve", "tensor_tensor", [f"ps{5 + d}", "odec"], [tt.name], out=t3(tt),
                  in0=psum[5 + d][:, :].rearrange("p (h d) -> p h d", h=8), in1=ov, op=ALU.mult)
                X(S, "dve", "tensor_tensor", [f"ps{3 + d}", tt.name], [tt.name], out=tt[:], in0=tt[:], in1=psum[3 + d][:, :], op=ALU.add)

        def p2_epi2(c):
            b = c % 3
            X(S, "pool", "tensor_tensor", ["tA", "tB"], ["tA"], out=tA[:], in0=tA[:], in1=tB[:], op=ALU.add)
            X(S, "pool", "tensor_tensor", [f"xsB{b}", "dskip"], ["tU"], out=tU[:], in0=xsB[b][:, 0:512], in1=g("dskip"), op=ALU.mult)
            X(S, "pool", "tensor_tensor", ["tA", "tU"], ["tA"], out=tA[:], in0=tA[:], in1=tU[:], op=ALU.add)
            X(S, "dve", "tensor_tensor", ["tA", f"zt{b}"], ["tA"], out=tA[:], in0=tA[:], in1=zt[b][:], op=ALU.mult)
            X(S, "dve", "tensor_tensor", ["tA"], ["tU"], out=tU[:], in0=tA[:], in1=tA[:], op=ALU.mult)
            X(S, "dve", "tensor_reduce", ["tU"], ["ss"], out=ss[:], in_=tU[:].rearrange("p (g f) -> p g f", g=2),
              axis=mybir.AxisListType.X, op=ALU.add)
            X(S, "dve", "tensor_scalar", ["ss"], ["ss"], out=ss[:], in0=ss[:], scalar1=1.0 / 256.0, scalar2=1e-5, op0=ALU.mult, op1=ALU.add)
            X(S, "act", "activation", ["ss"], ["ss"], out=ss[:], in_=ss[:], func=AF.Ln)
            X(S, "act", "activation", ["ss"], ["ss"], out=ss[:], in_=ss[:], func=AF.Exp, scale=-0.5)
            for gq in range(2):
                X(S, "dve", "scalar_tensor_tensor", ["tA", "ss", "nw"], ["yb"], out=yb[:, gq * 256:(gq + 1) * 256],
                  in0=tA[:, gq * 256:(gq + 1) * 256], scalar=ss[:, gq:gq + 1], in1=G["nw"][:, gq * 256:(gq + 1) * 256],
                  op0=ALU.mult, op1=ALU.mult)
            for q4 in range(4):
                X(S, "pe", "transpose", ["yb", "ident_s"], ["psT"], psT[:, q4 * 128:(q4 + 1) * 128], in_=yb[:, q4 * 128:(q4 + 1) * 128],
                  identity=ident[:])
            X(S, "act", "copy", ["psT"], [f"yT{b}"], out=yT[b][:].rearrange("p a b -> p (a b)"), in_=psT[:, 0:512])
            DM(S, ssdv[:, :, c * 128:(c + 1) * 128], yT[b][:], reads=[f"yT{b}"])

        p2_loads(0)
        p2_loads(1)
        p2_front(0)
        for c in range(32):
            if c + 2 < 32:
                p2_loads(c + 2)
            p2_epi1(c)
            if c + 1 < 32:
                p2_front(c + 1)
            p2_epi2(c)
        S.end_phase()


ALPHA = float((2.0 * 1) ** 0.25)
NROWS = N_EXP * CAP


def phase_mix(C):
    nc, S, psum, psT = C["nc"], C["S"], C["psum"], C["psT"]
    din, dscr = C["din"], C["dscr"]
    wout_d = din("w_out", [D, D])
    anw_d = din("anwT", [128, 4])
    l1w_d = din("ln1w_rep", [128, D])
    l1b_d = din("ln1b_rep", [128, D])
    rw_d = din("router_w", [D, N_EXP])
    rb_d = din("rb_rep", [128, N_EXP])
    ecap_d = din("ecap_rep", [128, N_EXP])
    su_d = din("c_SU2", [128, 128])
    ones_d = din("c_ones2", [128, 128])
    h1_d = dscr("h1_d", [S_TOK, D], F32)
    Xg_d = dscr("Xg_d", [NROWS, D], BF16)
    C["h1_d"], C["Xg_d"] = h1_d, Xg_d
    gates_all, dest_all = C["gates_all"], C["dest_all"]
    attnT_d, ssdT_d, x = C["attnT_d"], C["ssdT_d"], C["x"]
    with contextlib.ExitStack() as ph:
        def sba(name, shape, dt):
            return ph.enter_context(nc.sbuf_tensor(name, list(shape), dt))
        wo = sba("wo", [128, 8, D], BF16)
        wst = [sba(f"wost{i}", [128, 2, D], F32) for i in range(2)]
        anw = sba("anw", [128, 4], F32)
        l1w = sba("l1w", [128, D], F32)
        l1b = sba("l1b", [128, D], F32)
        rwf = sba("rwf", [128, 8, N_EXP], F32)
        rwb = sba("rwb", [128, 8, N_EXP], BF16)
        rb = sba("rb", [128, N_EXP], F32)
        base = sba("base", [128, N_EXP], F32)
        SU = sba("SU2", [128, 128], F32)
        ones = sba("ones2", [128, 128], F32)
        ident = sba("ident_m", [128, 128], BF16)
        DM(S, ident[:], C["ident_d"], writes=["ident_m"])
        DM(S, anw[:], anw_d, writes=["anw"])
        DM(S, l1w[:], l1w_d, writes=["l1w"])
        DM(S, l1b[:], l1b_d, writes=["l1b"])
        DM(S, rwf[:], rw_d.rearrange("(c p) e -> p c e", p=128), writes=["rwf"])
        DM(S, rb[:], rb_d, writes=["rb"])
        DM(S, base[:], ecap_d, writes=["base"])
        DM(S, SU[:], su_d, writes=["SU2"])
        DM(S, ones[:], ones_d, writes=["ones2"])
        X(S, "dve", "tensor_copy", ["rwf"], ["rwb"], out=rwb[:], in_=rwf[:])
        X(S, "pool", "memset", [], [f"dest{t}" for t in range(32)], dest_all[:], 0)
        wov = wout_d.rearrange("(c p) f -> p c f", p=128)
        for i in range(4):
            b = i % 2
            DM(S, wst[b][:], wov[:, 2 * i:2 * i + 2, :], writes=[f"wost{b}"])
            if i < 2:
                X(S, "dve", "tensor_tensor", [f"wost{b}", "anw"], ["wo"], out=wo[:, 2 * i:2 * i + 2, :], in0=wst[b][:],
                  in1=anw[:, 2 * i:2 * i + 2].unsqueeze(2).to_broadcast([128, 2, D]), op=ALU.mult)
            else:
                X(S, "dve", "tensor_copy", [f"wost{b}"], ["wo"], out=wo[:, 2 * i:2 * i + 2, :], in_=wst[b][:])
        aT = [sba(f"aT{i}", [128, 4, 128], BF16) for i in range(2)]
        sT = [sba(f"sT{i}", [128, 4, 128], BF16) for i in range(2)]
        xt = [sba(f"xt{i}", [128, D], F32) for i in range(2)]
        gjunk = sba("gjunk", [128, 128], F32)
        identf = sba("identf", [128, 128], F32)
        X(S, "dve", "tensor_copy", ["ident_m"], ["identf"], out=identf[:], in_=ident[:])
        sm = sba("sm", [128, 8], F32)
        sm2 = sba("sm2", [128, 8], F32)
        tt = sba("tmix", [128, D], F32)
        stats = sba("stats", [128, 12], F32)
        mv = sba("mv", [128, 2], F32)
        h1f = [sba(f"h1f{i}", [128, D], F32) for i in range(2)]
        h1b = [sba(f"h1b{i}", [128, D], BF16) for i in range(3)]
        h1T = sba("h1T", [128, D], BF16)
        lg = sba("lg", [128, N_EXP], F32)
        top8 = sba("top8", [128, 8], F32)
        msk = sba("msk", [128, N_EXP], F32)
        ex4 = sba("ex4", [128, 4], F32)
        oh = sba("oh", [128, 4, N_EXP], F32)
        posd = sba("posd", [128, N_EXP], F32)
        destf = sba("destf", [128, 4], F32)
        av = attnT_d.rearrange("(q p) t -> p q t", p=128)
        sv = ssdT_d.rearrange("(q p) t -> p q t", p=128)
        def mix_loads(tg):
            b = tg % 2
            tsl = slice(tg * 128, (tg + 1) * 128)
            DM(S, aT[b][:], av[:, :, tsl], writes=[f"aT{b}"])
            DM(S, sT[b][:], sv[:, :, tsl], writes=[f"sT{b}"])
            DM(S, xt[b][:], x[tsl, :], writes=[f"xt{b}"])

        def mix_s1(tg):
            b = tg % 2
            tsl = slice(tg * 128, (tg + 1) * 128)
            for q in range(4):
                X(S, "pe", "matmul", [f"aT{b}"], ["ps6"], psum[6][:, 0:128], lhsT=aT[b][:, q, :], rhs=aT[b][:, q, :],
                  start=(q == 0), stop=(q == 3))
            X(S, "dve", "tensor_tensor", ["ps6", "identf"], ["gjunk"], out=gjunk[:], in0=psum[6][:, 0:128], in1=identf[:], op=ALU.mult)
            X(S, "dve", "tensor_reduce", ["gjunk"], ["sm"], out=sm[:, 7:8], in_=gjunk[:], axis=mybir.AxisListType.X, op=ALU.add)
            X(S, "dve", "tensor_scalar", ["sm"], ["sm"], out=sm[:, 0:1], in0=sm[:, 7:8], scalar1=1.0 / 512.0, scalar2=1e-5,
              op0=ALU.mult, op1=ALU.add)
            X(S, "act", "activation", ["sm"], ["sm"], out=sm[:, 1:2], in_=sm[:, 0:1], func=AF.Ln)
            X(S, "act", "activation", ["sm"], ["sm"], out=sm[:, 2:3], in_=sm[:, 1:2], func=AF.Exp, scale=-0.5)
            for hf in range(2):
                for q in range(4):
                    X(S, "pe", "matmul", [f"aT{b}", "wo"], [f"ps{hf}"], psum[hf][:, :], lhsT=aT[b][:, q, :],
                      rhs=wo[:, q, hf * 512:(hf + 1) * 512], start=(q == 0), stop=(q == 3))
                for q in range(4):
                    X(S, "pe", "matmul", [f"sT{b}", "wo"], [f"ps{2 + hf}"], psum[2 + hf][:, :], lhsT=sT[b][:, q, :],
                      rhs=wo[:, 4 + q, hf * 512:(hf + 1) * 512], start=(q == 0), stop=(q == 3))
            for hf in range(2):
                hs = slice(hf * 512, (hf + 1) * 512)
                X(S, "dve", "scalar_tensor_tensor", [f"xt{b}", f"ps{2 + hf}"], ["tmix"], out=tt[:, hs], in0=xt[b][:, hs], scalar=ALPHA,
                  in1=psum[2 + hf][:, :], op0=ALU.mult, op1=ALU.add)
                X(S, "dve", "scalar_tensor_tensor", ["tmix", f"ps{hf}", "sm"], ["tmix"], out=tt[:, hs], in0=psum[hf][:, :],
                  scalar=sm[:, 2:3], in1=tt[:, hs], op0=ALU.mult, op1=ALU.add)
                X(S, "dve", "bn_stats", ["tmix"], ["stats"], out=stats[:, hf * 6:(hf + 1) * 6], in_=tt[:, hs])
            X(S, "dve", "bn_aggr", ["stats"], ["mv"], out=mv[:], in_=stats[:])
            X(S, "act", "activation", ["mv"], ["sm"], out=sm[:, 3:4], in_=mv[:, 1:2], func=AF.Ln, bias=1e-5)
            X(S, "act", "activation", ["sm"], ["sm"], out=sm[:, 4:5], in_=sm[:, 3:4], func=AF.Exp, scale=-0.5)
            X(S, "dve", "tensor_scalar", ["tmix", "mv", "sm"], ["tmix"], out=tt[:], in0=tt[:], scalar1=mv[:, 0:1], scalar2=sm[:, 4:5],
              op0=ALU.subtract, op1=ALU.mult)
            X(S, "dve", "tensor_tensor", ["tmix", "l1w"], ["tmix"], out=tt[:], in0=tt[:], in1=l1w[:], op=ALU.mult)
            X(S, "pool", "tensor_tensor", ["tmix", "l1b"], [f"h1f{b}"], out=h1f[b][:], in0=tt[:], in1=l1b[:], op=ALU.add)
            X(S, "act", "copy", [f"h1f{b}"], [f"h1b{tg % 3}"], out=h1b[tg % 3][:], in_=h1f[b][:])
            DM(S, h1_d[tsl, :], h1f[b][:], reads=[f"h1f{b}"])

        def mix_s2(tg):
            b = tg % 2
            tsl = slice(tg * 128, (tg + 1) * 128)
            for kc in range(8):
                X(S, "pe", "transpose", [f"h1b{tg % 3}", "ident_m"], ["psT"], psT[:, kc * 128:(kc + 1) * 128],
                  in_=h1b[tg % 3][:, kc * 128:(kc + 1) * 128], identity=ident[:])
            X(S, "act", "copy", ["psT"], ["h1T"], out=h1T[:], in_=psT[:, :])
            for kc in range(8):
                X(S, "pe", "matmul", ["h1T", "rwb"], ["ps4"], psum[4][:, 0:N_EXP], lhsT=h1T[:, kc * 128:(kc + 1) * 128],
                  rhs=rwb[:, kc, :], start=(kc == 0), stop=(kc == 7))
            X(S, "dve", "tensor_tensor", ["ps4", "rb"], ["lg"], out=lg[:], in0=psum[4][:, 0:N_EXP], in1=rb[:], op=ALU.add)
            X(S, "dve", "max", ["lg"], ["top8"], out=top8[:], in_=lg[:])
            X(S, "dve", "tensor_scalar", ["lg", "top8"], ["msk"], out=msk[:], in0=lg[:], scalar1=top8[:, 3:4], scalar2=None, op0=ALU.is_ge)
            X(S, "dve", "tensor_scalar", ["top8"], ["sm2"], out=sm2[:, 5:6], in0=top8[:, 0:1], scalar1=-1.0, scalar2=None, op0=ALU.mult)
            X(S, "act", "activation", ["top8", "sm2"], ["ex4"], out=ex4[:], in_=top8[:, 0:4], func=AF.Exp, bias=sm2[:, 5:6])
            X(S, "dve", "tensor_reduce", ["ex4"], ["sm2"], out=sm2[:, 6:7], in_=ex4[:], axis=mybir.AxisListType.X, op=ALU.add)
            X(S, "dve", "reciprocal", ["sm2"], ["sm2"], out=sm2[:, 7:8], in_=sm2[:, 6:7])
            X(S, "dve", "tensor_scalar", ["ex4", "sm2"], ["gates"], out=gates_all[:, tg, :], in0=ex4[:], scalar1=sm2[:, 7:8], scalar2=None,
              op0=ALU.mult)
            X(S, "pe", "matmul", ["SU2", "msk"], ["ps5"], psum[5][:, 0:N_EXP], lhsT=SU[:], rhs=msk[:], start=True, stop=True)
            X(S, "pe", "matmul", ["ones2", "msk"], ["ps5"], psum[5][:, 64:64 + N_EXP], lhsT=ones[:], rhs=msk[:], start=True, stop=True)
            X(S, "dve", "tensor_tensor", ["ps5", "base"], ["posd"], out=posd[:], in0=psum[5][:, 0:N_EXP], in1=base[:], op=ALU.add)
            X(S, "dve", "tensor_tensor", ["ps5", "base"], ["base"], out=base[:], in0=psum[5][:, 64:64 + N_EXP], in1=base[:], op=ALU.add)
            X(S, "dve", "tensor_tensor", ["lg", "top8"], ["oh"], out=oh[:], in0=lg[:].unsqueeze(1).to_broadcast([128, 4, N_EXP]),
              in1=top8[:, 0:4].unsqueeze(2).to_broadcast([128, 4, N_EXP]), op=ALU.is_equal)
            X(S, "dve", "tensor_tensor", ["oh", "posd"], ["oh"], out=oh[:], in0=oh[:],
              in1=posd[:].unsqueeze(1).to_broadcast([128, 4, N_EXP]), op=ALU.mult)
            X(S, "dve", "tensor_reduce", ["oh"], ["destf"], out=destf[:], in_=oh[:], axis=mybir.AxisListType.X, op=ALU.add)
            X(S, "dve", "tensor_copy", ["destf"], [f"dest{tg}"], out=dest_all[:, tg, :], in_=destf[:])

        def mix_scatter(tg):
            b = tg % 3
            for j in range(4):
                idx = dest_all[:, tg, j:j + 1]
                src = h1b[b][:]
                S.dma(lambda e, idx=idx, src=src: e.indirect_dma_start(
                    out=Xg_d, out_offset=bass.IndirectOffsetOnAxis(ap=idx, axis=0), in_=src, in_offset=None,
                    bounds_check=S.bc(e), oob_is_err=False),
                    reads=[f"dest{tg}", f"h1b{b}"], writes=[f"Xg{tg}_{j}"], q="pool")

        mix_loads(0)
        mix_loads(1)
        mix_s1(0)
        for tg in range(32):
            if tg + 2 < 32:
                mix_loads(tg + 2)
            if tg + 1 < 32:
                mix_s1(tg + 1)
            mix_s2(tg)
            if tg >= 1:
                mix_scatter(tg - 1)
        mix_scatter(31)
        ecs = sba("ecs", [128, N_EXP], F32)
        cntf = sba("cntf", [128, N_EXP], F32)
        DM(S, ecs[:], ecap_d, writes=["ecs"])
        X(S, "dve", "tensor_tensor", ["base", "ecs"], ["cntf"], out=cntf[:], in0=base[:], in1=ecs[:], op=ALU.subtract)
        flf = sba("flf", [128, N_EXP, 8], F32)
        for t in range(8):
            X(S, "dve", "tensor_scalar", ["cntf"], ["flf"], out=flf[:, :, t], in0=cntf[:], scalar1=float(128 * t), scalar2=None, op0=ALU.is_gt)
        X(S, "dve", "tensor_copy", ["flf"], ["flags"], out=C["flags_all"][:], in_=flf[:].rearrange("p e t -> p (e t)"))
        if C["debug"]:
            dd = dscr("dest_dbg", [128, 128], I32)
            gd = dscr("gates_dbg", [128, 128], F32)
            DM(S, dd, dest_all[:].rearrange("p a b -> p (a b)"), reads=[f"dest{t}" for t in range(32)])
            DM(S, gd, gates_all[:].rearrange("p a b -> p (a b)"), reads=["gates"])
        S.end_phase()


def phase_experts(C):
    nc, S, psum, psT = C["nc"], C["S"], C["psum"], C["psT"]
    din, dscr = C["din"], C["dscr"]
    wg_d = din("w_gate", [N_EXP, D, D])
    wu_d = din("w_up", [N_EXP, D, D])
    wd_d = din("w_down", [N_EXP, D, D])
    bg_d = din("bgT", [128, N_EXP * 8])
    bu_d = din("buT", [128, N_EXP * 8])
    bd_d = din("b_down", [N_EXP, D])
    Yg_d = dscr("Yg_d", [NROWS, D], F32)
    C["Yg_d"] = Yg_d
    Xg_d = C["Xg_d"]
    NT = CAP // 128
    psTs = [psT, psum[6][:, :].bitcast(BF16)]
    psTn = ["psT", "ps6"]
    with contextlib.ExitStack() as ph:
        def sba(name, shape, dt):
            return ph.enter_context(nc.sbuf_tensor(name, list(shape), dt))
        NS = 4
        wsl = [sba(f"wsl{i}", [128, 8, D], BF16) for i in range(NS)]
        wst = [sba(f"west{i}", [128, 2, D], F32) for i in range(2)]
        bg = sba("bg", [128, N_EXP * 8], F32)
        bu = sba("bu", [128, N_EXP * 8], F32)
        bd = [sba(f"bd{i}", [128, D], F32) for i in range(2)]
        ident = sba("ident_e", [128, 128], BF16)
        NXG = 8
        xg = [sba(f"xg{i}", [128, D], BF16) for i in range(NXG)]
        XT = [sba(f"XT{i}", [128, 8, CAP], BF16) for i in range(2)]
        actT = sba("actT", [128, 8, CAP], BF16)
        g1 = [sba(f"g1_{i}", [128, 512], F32) for i in range(2)]
        u1 = [sba(f"u1_{i}", [128, 512], F32) for i in range(2)]
        sg = [sba(f"sg_{i}", [128, 512], F32) for i in range(2)]
        yo = [sba(f"yo{i}", [128, D], F32) for i in range(2)]
        DM(S, ident[:], C["ident_d"], writes=["ident_e"])
        DM(S, bg[:], bg_d, writes=["bg"])
        DM(S, bu[:], bu_d, writes=["bu"])
        st = dict(slot=0, stg=0, xg=0, tp=0)

        def load_w_pieces(src, e):
            sl = st["slot"] % NS
            st["slot"] += 1
            v = src[e].rearrange("(c p) f -> p c f", p=128)

            def piece(i):
                b = st["stg"] % 2
                st["stg"] += 1
                DM(S, wst[b][:], v[:, 2 * i:2 * i + 2, :], writes=[f"west{b}"])
                X(S, "act", "copy", [f"west{b}"], [f"wsl{sl}"], out=wsl[sl][:, 2 * i:2 * i + 2, :], in_=wst[b][:])
            return sl, [lambda i=i: piece(i) for i in range(4)]

        def load_w(src, e):
            sl, ps = load_w_pieces(src, e)
            for p in ps:
                p()
            return sl

        def xg_loads(e):
            for t in range(NT):
                r0 = e * CAP + t * 128
                DM(S, xg[t][:], Xg_d[r0:r0 + 128, :], writes=[f"xg{t}"])

        flags = C["flags_all"]

        def xt_transposes(e, t0, t1):
            xb = e % 2
            for t in range(t0, t1):
                tp = st["tp"] % 2
                st["tp"] += 1
                for kc in range(8):
                    X(S, "pe", "transpose", [f"xg{t}", "ident_e"], [psTn[tp]], psTs[tp][:, kc * 128:(kc + 1) * 128],
                      in_=xg[t][:, kc * 128:(kc + 1) * 128], identity=ident[:])
                pv_ = psTs[tp][:, :].rearrange("p (c t) -> p c t", c=8)
                X(S, "dve", "tensor_copy", [psTn[tp]], [f"XT{xb}"], out=XT[xb][:, :, t * 128:(t + 1) * 128], in_=pv_)

        def fl_(e, t):
            return flags[0:1, e * 8 + t:e * 8 + t + 1]

        def xt_all(e):
            xt_transposes(e, 0, 3)
            for t in range(3, 8):
                S.cond_begin(fl_(e, t))
                xt_transposes(e, t, t + 1)
                S.cond_end()

        def gu_unit_small(e, fc, sg_, su_, xb, n0, nn, q):
            ns = slice(n0, n0 + nn)
            pg, pu = 2 * q, 2 * q + 1
            ar = f"actT{n0 // 256}"
            for kc in range(8):
                X(S, "pe", "matmul", [f"wsl{sg_}", f"XT{xb}"], [f"ps{pg}"], psum[pg][:, 0:nn], lhsT=wsl[sg_][:, kc, fc * 128:(fc + 1) * 128],
                  rhs=XT[xb][:, kc, ns], start=(kc == 0), stop=(kc == 7))
            for kc in range(8):
                X(S, "pe", "matmul", [f"wsl{su_}", f"XT{xb}"], [f"ps{pu}"], psum[pu][:, 0:nn], lhsT=wsl[su_][:, kc, fc * 128:(fc + 1) * 128],
                  rhs=XT[xb][:, kc, ns], start=(kc == 0), stop=(kc == 7))
            bcol = e * 8 + fc
            X(S, "dve", "tensor_scalar", [f"ps{pg}", "bg"], [f"g1_{q}"], out=g1[q][:, 0:nn], in0=psum[pg][:, 0:nn], scalar1=bg[:, bcol:bcol + 1],
              scalar2=7.0, op0=ALU.add, op1=ALU.min)
            X(S, "dve", "tensor_scalar", [f"ps{pu}", "bu"], [f"u1_{q}"], out=u1[q][:, 0:nn], in0=psum[pu][:, 0:nn], scalar1=bu[:, bcol:bcol + 1],
              scalar2=7.0, op0=ALU.add, op1=ALU.min)
            X(S, "dve", "tensor_scalar", [f"u1_{q}"], [f"u1_{q}"], out=u1[q][:, 0:nn], in0=u1[q][:, 0:nn], scalar1=-7.0, scalar2=1.0,
              op0=ALU.max, op1=ALU.add)
            X(S, "act", "activation", [f"g1_{q}"], [f"sg_{q}"], out=sg[q][:, 0:nn], in_=g1[q][:, 0:nn], func=AF.Sigmoid, scale=1.702)
            X(S, "pool", "tensor_tensor", [f"g1_{q}", f"sg_{q}"], [f"sg_{q}"], out=sg[q][:, 0:nn], in0=g1[q][:, 0:nn], in1=sg[q][:, 0:nn], op=ALU.mult)
            X(S, "pool", "tensor_tensor", [f"u1_{q}", f"sg_{q}"], [ar], out=actT[:, fc, ns], in0=sg[q][:, 0:nn], in1=u1[q][:, 0:nn], op=ALU.mult)

        def gu_unit(e, fc, hf, sg_, su_, xb, n0=None, nn=512):
            q = st["ei"] % 2
            st["ei"] += 1
            if n0 is None:
                n0 = hf * 512
            ns = slice(n0, n0 + nn)
            pg, pu = 2 * q, 2 * q + 1
            if nn != 512:
                return gu_unit_small(e, fc, sg_, su_, xb, n0, nn, q)
            for kc in range(8):
                X(S, "pe", "matmul", [f"wsl{sg_}", f"XT{xb}"], [f"ps{pg}"], psum[pg][:, :], lhsT=wsl[sg_][:, kc, fc * 128:(fc + 1) * 128],
                  rhs=XT[xb][:, kc, ns], start=(kc == 0), stop=(kc == 7))
            for kc in range(8):
                X(S, "pe", "matmul", [f"wsl{su_}", f"XT{xb}"], [f"ps{pu}"], psum[pu][:, :], lhsT=wsl[su_][:, kc, fc * 128:(fc + 1) * 128],
                  rhs=XT[xb][:, kc, ns], start=(kc == 0), stop=(kc == 7))
            bcol = e * 8 + fc
            X(S, "dve", "tensor_scalar", [f"ps{pg}", "bg"], [f"g1_{q}"], out=g1[q][:], in0=psum[pg][:, :], scalar1=bg[:, bcol:bcol + 1],
              scalar2=7.0, op0=ALU.add, op1=ALU.min)
            X(S, "dve", "tensor_scalar", [f"ps{pu}", "bu"], [f"u1_{q}"], out=u1[q][:], in0=psum[pu][:, :], scalar1=bu[:, bcol:bcol + 1],
              scalar2=7.0, op0=ALU.add, op1=ALU.min)
            X(S, "dve", "tensor_scalar", [f"u1_{q}"], [f"u1_{q}"], out=u1[q][:], in0=u1[q][:], scalar1=-7.0, scalar2=1.0,
              op0=ALU.max, op1=ALU.add)
            X(S, "act", "activation", [f"g1_{q}"], [f"sg_{q}"], out=sg[q][:], in_=g1[q][:], func=AF.Sigmoid, scale=1.702)
            X(S, "pool", "tensor_tensor", [f"g1_{q}", f"sg_{q}"], [f"sg_{q}"], out=sg[q][:], in0=g1[q][:], in1=sg[q][:], op=ALU.mult)
            X(S, "pool", "tensor_tensor", [f"u1_{q}", f"sg_{q}"], ["actT0", "actT1"], out=actT[:, fc, ns], in0=sg[q][:], in1=u1[q][:], op=ALU.mult)

        def down_tile(e, t, sd_, bb):
            yb_ = t % 2
            for hf in range(2):
                pb = 4 + hf
                for fc in range(8):
                    X(S, "pe", "matmul", [f"wsl{sd_}", f"actT{t // 2}"], [f"ps{pb}"], psum[pb][:, :], lhsT=actT[:, fc, t * 128:(t + 1) * 128],
                      rhs=wsl[sd_][:, fc, hf * 512:(hf + 1) * 512], start=(fc == 0), stop=(fc == 7))
                X(S, "dve", "tensor_tensor", [f"ps{pb}", f"bd{bb}"], [f"yo{yb_}"], out=yo[yb_][:, hf * 512:(hf + 1) * 512], in0=psum[pb][:, :],
                  in1=bd[bb][:, hf * 512:(hf + 1) * 512], op=ALU.add)
            r0 = e * CAP + t * 128
            DM(S, Yg_d[r0:r0 + 128, :], yo[yb_][:], reads=[f"yo{yb_}"], writes=[], q="act")

        st["ei"] = 0
        xg_loads(0)
        sg_, su_ = load_w(wg_d, 0), load_w(wu_d, 0)
        xt_all(0)
        for e in range(N_EXP):
            xb = e % 2
            fl = flags[0:1, e:e + 1]
            if e + 1 < N_EXP:
                xg_loads(e + 1)
            sd_, todo = load_w_pieces(wd_d, e)
            if e + 1 < N_EXP:
                sg_n, todo2 = load_w_pieces(wg_d, e + 1)
                todo = todo + todo2
            bb = e % 2
            DM(S, bd[bb][:], bd_d[e:e + 1, :].partition_broadcast(128), writes=[f"bd{bb}"])
            for fc in range(8):
                gu_unit(e, fc, 0, sg_, su_, xb)
                if todo:
                    todo.pop(0)()
            while todo:
                todo.pop(0)()
            for qq in range(2):
                S.cond_begin(fl_(e, 4 + 2 * qq))
                for fc in range(8):
                    gu_unit(e, fc, 1, sg_, su_, xb, n0=512 + 256 * qq, nn=256)
                S.cond_end()
            todo3 = []
            if e + 1 < N_EXP:
                su_n, todo3 = load_w_pieces(wu_d, e + 1)
                xt_all(e + 1)
            for t in range(3):
                if todo3:
                    todo3.pop(0)()
                down_tile(e, t, sd_, bb)
            while todo3:
                todo3.pop(0)()
            for t in range(3, 8):
                S.cond_begin(fl_(e, t))
                down_tile(e, t, sd_, bb)
                S.cond_end()
            if e + 1 < N_EXP:
                sg_, su_ = sg_n, su_n
        S.end_phase()


def phase_combine(C):
    nc, S = C["nc"], C["S"]
    din, dscr = C["din"], C["dscr"]
    l2w_d = din("ln2w_rep", [128, D])
    l2b_d = din("ln2b_rep", [128, D])
    out_d = nc.dram_tensor("out", [S_TOK, D], F32, kind="ExternalOutput").ap()
    Yg_d, h1_d = C["Yg_d"], C["h1_d"]
    gates_all, dest_all = C["gates_all"], C["dest_all"]
    with contextlib.ExitStack() as ph:
        def sba(name, shape, dt):
            return ph.enter_context(nc.sbuf_tensor(name, list(shape), dt))
        l2w = sba("l2w", [128, D], F32)
        l2b = sba("l2b", [128, D], F32)
        DM(S, l2w[:], l2w_d, writes=["l2w"])
        DM(S, l2b[:], l2b_d, writes=["l2b"])
        yg = [[sba(f"yg{i}_{j}", [128, D], F32) for j in range(4)] for i in range(2)]
        h1 = [sba(f"h1c{i}", [128, D], F32) for i in range(2)]
        acc = sba("cacc", [128, D], F32)
        ot = [sba(f"cot{i}", [128, D], F32) for i in range(2)]
        stats = sba("cstats", [128, 12], F32)
        mv = sba("cmv", [128, 2], F32)
        sm = sba("csm", [128, 4], F32)
        def comb_gather(tg):
            b = tg % 2
            tsl = slice(tg * 128, (tg + 1) * 128)
            DM(S, h1[b][:], h1_d[tsl, :], writes=[f"h1c{b}"])
            for j in range(4):
                idx = dest_all[:, tg, j:j + 1]
                dst = yg[b][j][:]
                S.dma(lambda e, idx=idx, dst=dst: e.indirect_dma_start(
                    out=dst, out_offset=None, in_=Yg_d, in_offset=bass.IndirectOffsetOnAxis(ap=idx, axis=0),
                    bounds_check=S.bc(e), oob_is_err=False),
                    reads=[], writes=[f"yg{b}_{j}"], q="pool")
        comb_gather(0)
        for tg in range(32):
            b = tg % 2
            tsl = slice(tg * 128, (tg + 1) * 128)
            if tg + 1 < 32:
                comb_gather(tg + 1)
            X(S, "dve", "tensor_scalar", [f"h1c{b}"], ["cacc"], out=acc[:], in0=h1[b][:], scalar1=ALPHA, scalar2=None, op0=ALU.mult)
            for j in range(4):
                X(S, "dve", "scalar_tensor_tensor", [f"yg{b}_{j}", "cacc"], ["cacc"], out=acc[:], in0=yg[b][j][:],
                  scalar=gates_all[:, tg, j:j + 1], in1=acc[:], op0=ALU.mult, op1=ALU.add)
            for hf in range(2):
                X(S, "dve", "bn_stats", ["cacc"], ["cstats"], out=stats[:, hf * 6:(hf + 1) * 6], in_=acc[:, hf * 512:(hf + 1) * 512])
            X(S, "dve", "bn_aggr", ["cstats"], ["cmv"], out=mv[:], in_=stats[:])
            X(S, "act", "activation", ["cmv"], ["csm"], out=sm[:, 0:1], in_=mv[:, 1:2], func=AF.Ln, bias=1e-5)
            X(S, "act", "activation", ["csm"], ["csm"], out=sm[:, 1:2], in_=sm[:, 0:1], func=AF.Exp, scale=-0.5)
            X(S, "dve", "tensor_scalar", ["cacc", "cmv", "csm"], ["cacc"], out=acc[:], in0=acc[:], scalar1=mv[:, 0:1], scalar2=sm[:, 1:2],
              op0=ALU.subtract, op1=ALU.mult)
            X(S, "dve", "tensor_tensor", ["cacc", "l2w"], ["cacc"], out=acc[:], in0=acc[:], in1=l2w[:], op=ALU.mult)
            X(S, "pool", "tensor_tensor", ["cacc", "l2b"], [f"cot{b}"], out=ot[b][:], in0=acc[:], in1=l2b[:], op=ALU.add)
            DM(S, out_d[tsl, :], ot[b][:], reads=[f"cot{b}"])
        S.end_phase()


def build(debug=False):
    nc = bass.Bass("TRN2", target_bir_lowering=False)
    es = contextlib.ExitStack()
    with es:
        def din(name, shape, dt=F32):
            return nc.dram_tensor(name, list(shape), dt, kind="ExternalInput").ap()

        def dscr(name, shape, dt, out=False):
            kind = "ExternalOutput" if (out or debug) else "Internal"
            return nc.dram_tensor(name, list(shape), dt, kind=kind).ap()

        xT = din("xT", [D, S_TOK])
        x = din("x", [S_TOK, D])
        w_in = din("w_in_ext", [D, WCOLS])
        cosT = din("cosT", [128, S_TOK])
        sinT = din("sinT", [128, S_TOK])

        qT_d = dscr("qT_d", [512, S_TOK], BF16)
        kT_d = dscr("kT_d", [512, S_TOK], BF16)
        v_d = dscr("v_d", [S_TOK, 512], BF16)
        z_d = dscr("z_d", [S_TOK, 512], F32)
        dt_d = dscr("dt_d", [128, 512], F32)
        xbcT_d = dscr("xbcT_d", [1024, S_TOK + 4], BF16)

        S = Sched(nc, es)
        psum = [es.enter_context(nc.psum_tensor(f"ps{i}", [128, 512], F32)) for i in range(7)]
        psT = es.enter_context(nc.psum_tensor("psT", [128, 1024], BF16))

        def sb(name, shape, dt):
            return es.enter_context(nc.sbuf_tensor(name, list(shape), dt))

        with contextlib.ExitStack() as pa:
            def sba(name, shape, dt):
                return pa.enter_context(nc.sbuf_tensor(name, list(shape), dt))
            wbf = sba("wbf", [128, 8, WCOLS], BF16)
            wst = [sba(f"wst{i}", [128, WCOLS // 2], F32) for i in range(2)]
            cos_sb = sba("cos_sb", [128, S_TOK], F32)
            sin_sb = sba("sin_sb", [128, S_TOK], F32)
            xst = [sba(f"xst{i}", [128, 8, 512], F32) for i in range(2)]
            xb = [sba(f"xb{i}", [128, 8, 512], BF16) for i in range(2)]
            t1 = [sba(f"t1_{i}", [128, 512], F32) for i in range(2)]
            t2 = [sba(f"t2_{i}", [128, 512], F32) for i in range(2)]
            ob = [sba(f"ob{i}", [128, 512], BF16) for i in range(4)]
            of = [sba(f"of{i}", [128, 512], F32) for i in range(2)]
            dts = sba("dts", [128, 512], F32)
            zpad = sba("zpad", [128, 8, 2], BF16)

            S.dma(lambda e: e.dma_start(out=cos_sb[:], in_=cosT), writes=["cos_sb"])
            S.dma(lambda e: e.dma_start(out=sin_sb[:], in_=sinT), writes=["sin_sb"])
            S.op("pool", lambda e: e.memset(zpad[:], 0.0), writes=["zpad"])
            xbc_rows = xbcT_d.rearrange("(c p) t -> p c t", p=128)
            S.dma(lambda e: e.dma_start(out=xbc_rows[:, :, 0:2], in_=zpad[:]), reads=["zpad"], writes=["xbcpadL"])
            S.dma(lambda e: e.dma_start(out=xbc_rows[:, :, S_TOK + 2:S_TOK + 4], in_=zpad[:]), reads=["zpad"], writes=["xbcpadR"])
            H = WCOLS // 2
            n = 0
            for hf in range(2):
                for kc in range(8):
                    st = wst[n % 2]
                    S.dma(lambda e, st=st, kc=kc, hf=hf: e.dma_start(out=st[:], in_=w_in[kc * 128:(kc + 1) * 128, hf * H:(hf + 1) * H]),
                          writes=[f"wst{n % 2}"])
                    if n % 2 == 0:
                        S.op("dve", lambda e, st=st, kc=kc, hf=hf: e.tensor_copy(out=wbf[:, kc, hf * H:(hf + 1) * H], in_=st[:]),
                             reads=[f"wst{n % 2}"], writes=[f"wbf{kc}_{hf}"])
                    else:
                        S.op("act", lambda e, st=st, kc=kc, hf=hf: e.copy(out=wbf[:, kc, hf * H:(hf + 1) * H], in_=st[:]),
                             reads=[f"wst{n % 2}"], writes=[f"wbf{kc}_{hf}"])
                    n += 1
            def wres_for(kc, c0, c1):
                return [f"wbf{kc}_{hf}" for hf in range(2) if c0 < (hf + 1) * H and c1 > hf * H]
            xT_r = xT.rearrange("(c p) t -> p c t", p=128)
            pb = 0
            obi = 0
            for ch in range(8):
                t0 = ch * 512
                bi = ch % 2
                S.dma(lambda e, bi=bi, t0=t0: e.dma_start(out=xst[bi][:], in_=xT_r[:, :, t0:t0 + 512]), writes=[f"xst{bi}"])
                S.op("dve", lambda e, bi=bi: e.tensor_copy(out=xb[bi][:], in_=xst[bi][:]), reads=[f"xst{bi}"], writes=[f"xb{bi}"])
                xres = f"xb{bi}"

                def fm_tile(j, bank, bi=bi):
                    for kc in range(8):
                        S.op("pe", lambda e, j=j, kc=kc, bank=bank: e.matmul(
                            psum[bank][:, :], lhsT=wbf[:, kc, j * 128:(j + 1) * 128], rhs=xb[bi][:, kc, :],
                            start=(kc == 0), stop=(kc == 7)), reads=[xres] + wres_for(kc, j * 128, (j + 1) * 128), writes=[f"ps{bank}"])
                for which, dst in ((0, qT_d), (8, kT_d)):
                    for j in range(4):
                        ba, bb = pb % 6, (pb + 1) % 6
                        pb += 2
                        fm_tile(which + j, ba)
                        fm_tile(which + 4 + j, bb)
                        ti = j % 2
                        S.op("dve", lambda e, ba=ba, ti=ti, t0=t0: e.tensor_tensor(
                            out=t1[ti][:], in0=psum[ba][:, :], in1=cos_sb[:, t0:t0 + 512], op=ALU.mult),
                            reads=[f"ps{ba}", "cos_sb"], writes=[f"t1_{ti}"])
                        S.op("dve", lambda e, bb=bb, ti=ti, t0=t0: e.tensor_tensor(
                            out=t2[ti][:], in0=psum[bb][:, :], in1=sin_sb[:, t0:t0 + 512], op=ALU.mult),
                            reads=[f"ps{bb}", "sin_sb"], writes=[f"t2_{ti}"])
                        o = obi % 4
                        obi += 1
                        S.op("pool", lambda e, ti=ti, o=o: e.tensor_tensor(
                            out=ob[o][:], in0=t1[ti][:], in1=t2[ti][:], op=ALU.add),
                            reads=[f"t1_{ti}", f"t2_{ti}"], writes=[f"ob{o}"])
                        S.dma(lambda e, o=o, j=j, dst=dst, t0=t0: e.dma_start(
                            out=dst[j * 128:(j + 1) * 128, t0:t0 + 512], in_=ob[o][:]), reads=[f"ob{o}"], writes=[])
                for j in range(8):
                    ba = pb % 6
                    pb += 1
                    fm_tile(16 + j, ba)
                    o = obi % 4
                    obi += 1
                    S.op("act", lambda e, ba=ba, o=o: e.copy(out=ob[o][:], in_=psum[ba][:, :]),
                         reads=[f"ps{ba}"], writes=[f"ob{o}"])
                    S.dma(lambda e, o=o, j=j, t0=t0: e.dma_start(
                        out=xbcT_d[j * 128:(j + 1) * 128, 2 + t0:2 + t0 + 512], in_=ob[o][:]), reads=[f"ob{o}"], writes=[])
                for tt in range(4):
                    tg = ch * 4 + tt
                    for which in range(2):
                        ba = pb % 6
                        pb += 1
                        c0 = 3072 + which * 512
                        for kc in range(8):
                            S.op("pe", lambda e, kc=kc, ba=ba, tt=tt, c0=c0, bi=bi: e.matmul(
                                psum[ba][:, :], lhsT=xb[bi][:, kc, tt * 128:(tt + 1) * 128], rhs=wbf[:, kc, c0:c0 + 512],
                                start=(kc == 0), stop=(kc == 7)), reads=[xres] + wres_for(kc, c0, c0 + 512), writes=[f"ps{ba}"])
                        if which == 0:
                            o = obi % 4
                            obi += 1
                            S.op("act", lambda e, ba=ba, o=o: e.copy(out=ob[o][:], in_=psum[ba][:, :]),
                                 reads=[f"ps{ba}"], writes=[f"ob{o}"])
                            S.dma(lambda e, o=o, tg=tg: e.dma_start(out=v_d[tg * 128:(tg + 1) * 128, :], in_=ob[o][:]),
                                  reads=[f"ob{o}"], writes=[])
                        else:
                            o = tg % 2
                            S.op("act", lambda e, ba=ba, o=o: e.copy(out=of[o][:], in_=psum[ba][:, :]),
                                 reads=[f"ps{ba}"], writes=[f"of{o}"])
                            S.dma(lambda e, o=o, tg=tg: e.dma_start(out=z_d[tg * 128:(tg + 1) * 128, :], in_=of[o][:]),
                                  reads=[f"of{o}"], writes=[])
                    for kc in range(8):
                        S.op("pe", lambda e, kc=kc, tt=tt, tg=tg, bi=bi: e.matmul(
                            psum[6][:, tg * 16:(tg + 1) * 16], lhsT=xb[bi][:, kc, tt * 128:(tt + 1) * 128],
                            rhs=wbf[:, kc, 4096:4112], start=(kc == 0), stop=(kc == 7)),
                            reads=[xres] + wres_for(kc, 4096, 4112), writes=["ps6"])
            S.op("act", lambda e: e.copy(out=dts[:], in_=psum[6][:, :]), reads=["ps6"], writes=["dts"])
            S.dma(lambda e: e.dma_start(out=dt_d, in_=dts[:]), reads=["dts"], writes=[])

            S.end_phase()

        C = dict(nc=nc, S=S, psum=psum, debug=debug, din=din, dscr=dscr)
        C.update(qT_d=qT_d, kT_d=kT_d, v_d=v_d, z_d=z_d, dt_d=dt_d, xbcT_d=xbcT_d, x=x)
        C["psT"] = psT
        phase_attn(C)
        phase_conv(C)
        phase_ssd(C)
        C["gates_all"] = es.enter_context(nc.sbuf_tensor("gates_all", [128, 32, 4], F32))
        C["dest_all"] = es.enter_context(nc.sbuf_tensor("dest_all", [128, 32, 4], I32))
        C["flags_all"] = es.enter_context(nc.sbuf_tensor("flags_all", [128, N_EXP * 8], I32))
        phase_mix(C)
        phase_experts(C)
        phase_combine(C)
        S.streams["sp"].append(("wait", "c_pe", S.cnt["pe"])) if False else None
        S.finish()
        S.emit()
    return nc


_CACHE = {}


def kernel(**inputs):
    debug = bool(inputs.pop("_debug", False))
    x = np.asarray(inputs["x"], dtype=np.float32)
    w_in = np.asarray(inputs["w_in"], dtype=np.float32)[0]
    perm = np.concatenate([np.concatenate([np.arange(32, 64), np.arange(0, 32)]) + 64 * h for h in range(8)])
    wq, wk, wv = w_in[:, 0:512], w_in[:, 512:1024], w_in[:, 1024:1536]
    wz, wxbc, wdt = w_in[:, 1536:2048], w_in[:, 2048:3072], w_in[:, 3072:3088]
    w_ext = np.ascontiguousarray(np.concatenate([wq, wq[:, perm], wk, wk[:, perm], wxbc, wv, wz, wdt], axis=1))
    consts = host_consts()
    g = lambda k: np.asarray(inputs[k], dtype=np.float32)[0]
    rep = lambda v: np.ascontiguousarray(np.broadcast_to(v[None, :], (128, v.shape[0])))
    consts["conv_wT"] = np.ascontiguousarray(g("conv_w").reshape(5, 8, 128).transpose(2, 1, 0).reshape(128, 40))
    consts["conv_bT"] = np.ascontiguousarray(g("conv_b").reshape(8, 128).T)
    consts["conv_b_row"] = np.ascontiguousarray(g("conv_b").reshape(1, 1024))
    consts["dtb_rep"] = rep(np.tile(np.concatenate([g("dt_bias_fwd"), g("dt_bias_bwd")]), 32))
    consts["alog_rep"] = rep(np.tile(np.concatenate([g("a_log_fwd"), g("a_log_bwd")]), 32))
    consts["dskip_rep"] = rep(np.repeat(g("d_skip"), 64))
    consts["ssdnw_rep"] = rep(g("ssd_norm_w"))
    consts["w_out"] = g("w_out")
    consts["anwT"] = np.ascontiguousarray(g("attn_norm_w").reshape(4, 128).T)
    consts["ln1w_rep"] = rep(g("ln1_w"))
    consts["ln1b_rep"] = rep(g("ln1_b"))
    consts["router_w"] = g("router_w")
    consts["rb_rep"] = rep(g("router_b"))
    consts["ecap_rep"] = rep((np.arange(N_EXP) * CAP).astype(np.float32))
    consts["w_gate"] = g("w_gate")
    consts["w_up"] = g("w_up")
    consts["w_down"] = g("w_down")
    consts["bgT"] = np.ascontiguousarray(g("b_gate").reshape(N_EXP, 8, 128).transpose(2, 0, 1).reshape(128, N_EXP * 8))
    consts["buT"] = np.ascontiguousarray(g("b_up").reshape(N_EXP, 8, 128).transpose(2, 0, 1).reshape(128, N_EXP * 8))
    consts["b_down"] = g("b_down")
    consts["ln2w_rep"] = rep(g("ln2_w"))
    consts["ln2b_rep"] = rep(g("ln2_b"))
    consts["c_SU2"] = consts["c_SU"]
    consts["c_ones2"] = consts["c_ones"]
    nc = build(debug=debug)
    in_maps = []
    for b in range(8):
        m = {"xT": np.ascontiguousarray(x[b].T), "x": np.ascontiguousarray(x[b]), "w_in_ext": w_ext}
        m.update(consts)
        in_maps.append(m)
    ncores = int(inputs.pop("_ncores", 8)) if "_ncores" in inputs else 8
    res = run_bass_kernel_spmd(nc, in_maps[:ncores], core_ids=list(range(ncores)))
    if debug:
        return res.results
    return np.stack([r["out"] for r in res.results], axis=0)
```

```python
import contextlib
import numpy as np
import ml_dtypes
import concourse.bass as bass
import concourse.mybir as mybir
from concourse.bass_utils import run_bass_kernel_spmd

F32 = mybir.dt.float32
BF16 = mybir.dt.bfloat16
U32 = mybir.dt.uint32
I32 = mybir.dt.int32
AF = mybir.ActivationFunctionType
ALU = mybir.AluOpType

S_TOK = 4096
D = 1024
NCH = 32
WCOLS = 3072 + 1040
N_EXP = 32
CAP = 1024


class Sched:
    COMPUTE = ("pe", "act", "dve", "pool")

    def __init__(self, nc, es, n_ring=12):
        self.nc = nc
        self.streams = {e: [] for e in ("pe", "act", "dve", "pool", "sp")}
        self.sem = {}
        for e in self.COMPUTE:
            self.sem[e] = es.enter_context(nc.semaphore("sem_" + e))
        self.cnt = {e: 0 for e in self.COMPUTE}
        self.ring = {}
        self.ring_i = {}
        self.ring_tot = {}
        for q, n in (("sp", n_ring), ("pool", 8), ("act", 6), ("dve", 6)):
            self.ring[q] = [es.enter_context(nc.semaphore(f"dq_{q}_{i}")) for i in range(n)]
            self.ring_i[q] = 0
        self.last_writer = {}
        self.readers = {}
        self.waited = {e: {} for e in self.streams}
        self.n_ins = 0

    def _deps(self, reads, writes):
        deps = {}

        def add(tok):
            k, v, e = tok
            if k not in deps or deps[k][0] < v:
                deps[k] = (v, e)
        for r in reads:
            if r in self.last_writer:
                add(self.last_writer[r])
        for w in writes:
            if w in self.last_writer:
                add(self.last_writer[w])
            for k, (v, e) in self.readers.get(w, {}).items():
                add((k, v, e))
        return deps

    def _emit_waits(self, eng, deps):
        for k, (v, src) in deps.items():
            if src == eng and eng == "pe":
                continue
            if self.waited[eng].get(k, 0) >= v:
                continue
            self.waited[eng][k] = v
            self.streams[eng].append(("wait", k, v))

    def _commit(self, tok, reads, writes):
        k, v, e = tok
        for w in writes:
            self.last_writer[w] = tok
            self.readers[w] = {}
        for r in reads:
            d = self.readers.setdefault(r, {})
            if k not in d or d[k][0] < v:
                d[k] = (v, e)

    def cond_begin(self, flag_ap):
        import copy
        self._cond = dict(n={e: 0 for e in self.streams}, rings={e: {} for e in self.streams},
                          snap=copy.deepcopy(self.waited))
        for e in self.streams:
            self.streams[e].append(("cond_begin", flag_ap))

    def cond_end(self):
        c = self._cond
        for e in self.streams:
            self.streams[e].append(("cond_end", c["n"][e], c["rings"][e]))
        self.waited = c["snap"]
        self._cond = None

    def op(self, eng, fn, reads=(), writes=()):
        deps = self._deps(reads, writes)
        self._emit_waits(eng, deps)
        if getattr(self, "_cond", None):
            self._cond["n"][eng] += 1
        self.cnt[eng] += 1
        key = "c_" + eng
        tok = (key, self.cnt[eng], eng)
        self.streams[eng].append(("ins", fn, self.sem[eng], 1))
        self._commit(tok, reads, writes)
        self.n_ins += 1

    def dma(self, fn, reads=(), writes=(), q="sp"):
        deps = self._deps(reads, writes)
        self._emit_waits(q, deps)
        i = self.ring_i[q]
        self.ring_i[q] = (i + 1) % len(self.ring[q])
        key = f"d_{q}_{i}"
        tot = self.ring_tot.get(key, 0)
        if tot > 0 and self.waited[q].get(key, 0) < tot:
            self.waited[q][key] = tot
            self.streams[q].append(("wait", key, tot))
        tot += 16
        self.ring_tot[key] = tot
        if getattr(self, "_cond", None):
            ent = self._cond["rings"][q].setdefault(key, [0, tot - 16])
            ent[0] += 1
        tok = (key, tot, "dma")
        self.streams[q].append(("ins", fn, self.ring[q][i], 16))
        self._commit(tok, reads, writes)
        self.n_ins += 1

    def _semh(self, key):
        if key.startswith("c_"):
            return self.sem[key[2:]]
        _, q, i = key.split("_")
        return self.ring[q][int(i)]

    def finish(self):
        for key, tot in self.ring_tot.items():
            if self.waited["sp"].get(key, 0) < tot:
                self.streams["sp"].append(("wait", key, tot))
                self.waited["sp"][key] = tot
        for e in self.COMPUTE:
            if self.cnt[e] and self.waited["sp"].get("c_" + e, 0) < self.cnt[e]:
                self.streams["sp"].append(("wait", "c_" + e, self.cnt[e]))

    def barrier(self):
        for eng in self.streams:
            for key, tot in self.ring_tot.items():
                if self.waited[eng].get(key, 0) < tot:
                    self.streams[eng].append(("wait", key, tot))
                    self.waited[eng][key] = tot
            for e in self.COMPUTE:
                if e != eng and self.cnt[e] and self.waited[eng].get("c_" + e, 0) < self.cnt[e]:
                    self.streams[eng].append(("wait", "c_" + e, self.cnt[e]))
                    self.waited[eng]["c_" + e] = self.cnt[e]

    def bc(self, e):
        if getattr(self, "_bc", None) is None:
            self._bc = e.to_reg(NROWS - 1)
        return self._bc

    def end_phase(self):
        self._bc_prev = getattr(self, "_bc", None)
        print("phase end n_ins", self.n_ins, {k: len(v) for k, v in self.streams.items()}, flush=True)
        self.barrier()
        self.emit()
        self._bc = None
        self.streams = {e: [] for e in self.streams}
        self.last_writer = {}
        self.readers = {}

    def emit(self):
        nc = self.nc
        with nc.Block() as block:
            def mk(name):
                def body(eng):
                    items = self.streams[name]
                    reg = None
                    guard = None
                    i = 0
                    while i < len(items):
                        it = items[i]
                        if it[0] == "wait":
                            eng.wait_ge(self._semh(it[1]), it[2])
                        elif it[0] == "ins":
                            it[1](eng).then_inc(it[2], it[3])
                        elif it[0] == "cond_begin":
                            j = i + 1
                            while items[j][0] != "cond_end":
                                j += 1
                            if j == i + 1:
                                i = j + 1
                                continue
                            if reg is None:
                                self._uid = getattr(self, "_uid", 0) + 1
                                reg = eng.alloc_register(f"cr_{name}_{self._uid}")
                            eng.reg_load(reg, it[1])
                            guard = eng.If_ne(reg, 0)
                            guard.__enter__()
                        elif it[0] == "cond_end":
                            guard.__exit__(None, None, None)
                            g2 = eng.Else()
                            g2.__enter__()
                            if it[1] and name in self.sem:
                                left = it[1]
                                while left > 0:
                                    k_ = min(16, left)
                                    eng.drain().then_inc(self.sem[name], k_)
                                    left -= k_
                            for key, (cnt_, before_) in it[2].items():
                                if before_ > 0:
                                    eng.wait_ge(self._semh(key), before_)
                                for _ in range(cnt_):
                                    eng.drain().then_inc(self._semh(key), 16)
                            g2.__exit__(None, None, None)
                            guard = None
                        i += 1
                return body
            if self.streams["sp"]:
                block.sync(mk("sp"))
            if self.streams["pe"]:
                block.tensor(mk("pe"))
            if self.streams["act"]:
                block.scalar(mk("act"))
            if self.streams["dve"]:
                block.vector(mk("dve"))
            if self.streams["pool"]:
                block.gpsimd(mk("pool"))


def host_consts():
    c = {}
    pos = np.arange(S_TOK, dtype=np.float32)
    inv = (np.float32(10000.0) ** (-np.arange(0, 64, 2, dtype=np.float32) / np.float32(64))).astype(np.float32)
    ang = (pos[:, None] * inv[None, :]).astype(np.float32)
    cos = np.cos(ang).astype(np.float32).T
    sin = np.sin(ang).astype(np.float32).T
    cosT = np.concatenate([cos, cos, cos, cos], 0)
    sinT = np.concatenate([-sin, sin, -sin, sin], 0)
    kk = np.arange(128)[:, None]
    nn = np.arange(256)[None, :]
    c["negmask"] = np.where((kk <= nn) & (nn <= kk + 128), 0.0, -30000.0).astype(ml_dtypes.bfloat16)
    c["ident_bf"] = np.eye(128, dtype=np.float32).astype(ml_dtypes.bfloat16)
    sel = np.zeros((65, 64), np.float32)
    sel[64, :] = 1.0
    c["sel65"] = sel
    t_ = np.arange(128)
    c["c_SL"] = (t_[:, None] > t_[None, :]).astype(np.float32)
    c["c_UI"] = (t_[:, None] <= t_[None, :]).astype(np.float32)
    c["c_SU"] = (t_[:, None] < t_[None, :]).astype(np.float32)
    c["c_LI"] = (t_[:, None] >= t_[None, :]).astype(np.float32)
    c["c_ones"] = np.ones((128, 128), np.float32)
    c["cosT"] = np.ascontiguousarray(cosT)
    c["sinT"] = np.ascontiguousarray(sinT)
    return c


def phase_attn(C):
    nc, S, psum = C["nc"], C["S"], C["psum"]
    qT_d, kT_d, v_d = C["qT_d"], C["kT_d"], C["v_d"]
    negmask_d = C["din"]("negmask", [128, 256], BF16)
    ident_d = C["din"]("ident_bf", [128, 128], BF16)
    sel_d = C["din"]("sel65", [65, 64], F32)
    attnT_d = C["dscr"]("attnT_d", [512, S_TOK], BF16)
    C["attnT_d"] = attnT_d
    C["ident_d"] = ident_d
    with contextlib.ExitStack() as ph:
        def sba(name, shape, dt):
            return ph.enter_context(nc.sbuf_tensor(name, list(shape), dt))
        negmask = sba("negmask_sb", [128, 256], BF16)
        ident = sba("ident_sb", [128, 128], BF16)
        sel = sba("sel_sb", [65, 64], F32)
        V = [sba(f"V{i}", [128, 32, 4, 65], BF16) for i in range(3)]
        vst = [sba(f"vst{i}", [128, 8, 256], BF16) for i in range(2)]
        acc = [sba(f"acc{i}", [128, S_TOK], F32) for i in range(2)]
        qh = [sba(f"qh{i}", [128, S_TOK], BF16) for i in range(2)]
        kh = [sba(f"kh{i}", [128, S_TOK], BF16) for i in range(2)]
        pT = [sba(f"pT{i}", [128, 256], BF16) for i in range(4)]
        rec = [sba(f"rec{i}", [64, 512], F32) for i in range(2)]
        outb = [sba(f"aob{i}", [64, 512], BF16) for i in range(2)]
        S.dma(lambda e: e.dma_start(out=negmask[:], in_=negmask_d), writes=["negmask"])
        S.dma(lambda e: e.dma_start(out=ident[:], in_=ident_d), writes=["ident"])
        S.dma(lambda e: e.dma_start(out=sel[:], in_=sel_d), writes=["sel"])
        for i in range(3):
            S.op("pool", lambda e, i=i: e.memset(V[i][:], 1.0), writes=[f"V{i}"])
        PAT = (1, 4, 16)
        scb = [psum[0], psum[1], C["psT"][:, :].bitcast(F32)]
        out_banks = (2, 3, 4)
        cnt = dict(vs=0, sc=0, pt=0, ob=0, nb=0)

        for i in range(2):
            S.op("pool", lambda e, i=i: e.memset(kh[i][:], 0.0), writes=[f"kh{i}"])

        def load_qk(h):
            b = h % 2
            if h % 2 == 0:
                p = h // 2
                S.dma(lambda e: e.dma_start(out=qh[p % 2][:], in_=qT_d[p * 128:(p + 1) * 128, :]), writes=[f"qh{p % 2}"])
            S.dma(lambda e: e.dma_start(out=kh[b][b * 64:(b + 1) * 64, :], in_=kT_d[h * 64:(h + 1) * 64, :]), writes=[f"kh{b}"])

        def load_V(hh):
            for pi, r in enumerate(PAT):
                n_t = 32 // r
                view4 = v_d.rearrange("(u k r) c -> k r u c", r=r, k=128)
                for gi in range(4):
                    b = cnt["vs"] % 2
                    cnt["vs"] += 1
                    cs = slice(hh * 256, (hh + 1) * 256)
                    if r == 1:
                        src = view4[:, 0, 8 * gi:8 * gi + 8, cs]
                        dst = vst[b][:]
                    elif r == 4:
                        src = view4[:, gi, :, cs]
                        dst = vst[b][:]
                    else:
                        src = None
                        for j in range(4):
                            srcj = view4[:, 4 * gi + j, :, cs]
                            dstj = vst[b][:, 2 * j:2 * j + 2, :]
                            S.dma(lambda e, src=srcj, dst=dstj: e.dma_start(out=dst, in_=src), writes=[f"vst{b}"])
                    if src is not None:
                        S.dma(lambda e, src=src, dst=dst: e.dma_start(out=dst, in_=src), writes=[f"vst{b}"])
                    eng = ("pool", "dve")[gi % 2]
                    S.op(eng, lambda e, pi=pi, gi=gi, b=b: e.tensor_copy(
                        out=V[pi][:, 8 * gi:8 * gi + 8, :, 0:64],
                        in_=vst[b][:].rearrange("p t (h d) -> p t h d", h=4)),
                        reads=[f"vst{b}"], writes=[f"V{pi}"])

        def head(h):
            hb = h % 2
            hl = h % 4
            ab = h % 2
            accr = f"acc{ab}"
            tiles = []
            for pi, r in enumerate(PAT):
                L = S_TOK // r
                n_t = L // 128
                for rho in range(r):
                    nbanks = (L + 64 + 511) // 512
                    base = cnt["nb"]
                    cnt["nb"] += nbanks
                    for u in range(n_t):
                        tiles.append((pi, r, rho, u, n_t, L, base, nbanks))

            def score(tl):
                pi, r, rho, u, n_t, L, base, nbanks = tl
                m_lo = max(0, 128 * u - 64)
                m_hi = min(L, 128 * u + 192)
                N = m_hi - m_lo
                c0 = m_lo - (128 * u - 64)
                sl_ = cnt["sc"] % 3
                cnt["sc"] += 1
                sbk, so = sl_, 0
                pb_ = cnt["pt"] % 4
                cnt["pt"] += 1
                k0 = rho + r * 128 * u
                kc = kh[hb][:, k0:k0 + r * 127 + 1:r]
                qb_ = (h // 2) % 2
                qc = qh[qb_][:, rho + r * m_lo:rho + r * (m_hi - 1) + 1:r]
                S.op("pe", lambda e: e.matmul(scb[sbk][:, so:so + N], lhsT=kc, rhs=qc, start=True, stop=False),
                     reads=[f"qh{qb_}", f"kh{hb}"], writes=[f"sc{sl_}"])
                S.op("pe", lambda e: e.matmul(scb[sbk][:, so:so + N], lhsT=ident[:], rhs=negmask[:, c0:c0 + N],
                                               start=False, stop=True),
                     reads=["ident", "negmask"], writes=[f"sc{sl_}"])
                S.op("act", lambda e: e.activation(out=pT[pb_][:, 0:N], in_=scb[sbk][:, so:so + N], func=AF.Exp, scale=0.125),
                     reads=[f"sc{sl_}"], writes=[f"pT{pb_}"])
                return (pb_, m_lo, m_hi)

            def pv(tl, info):
                pi, r, rho, u, n_t, L, base, nbanks = tl
                pb_, m_lo, m_hi = info

                def phys(gb):
                    return out_banks[(base + gb) % 3]
                ti = rho * n_t + u
                vl = V[pi][:, ti, hl, :]
                nA = (128 * u + 64) - m_lo
                cA = m_lo + 64
                gA, colA = cA // 512, cA % 512
                pA = phys(gA)
                S.op("pe", lambda e: e.matmul(psum[pA][0:65, colA:colA + nA], lhsT=vl, rhs=pT[pb_][:, 0:nA],
                                               start=(u == 0), stop=True),
                     reads=[f"V{pi}", f"pT{pb_}"], writes=[f"ps{pA}"])
                nB = m_hi - (128 * u + 64)
                cB = 128 * (u + 1)
                gB, colB = cB // 512, cB % 512
                pB = phys(gB)
                lastu = (u == n_t - 1)
                S.op("pe", lambda e: e.matmul(psum[pB][0:65, colB:colB + nB], lhsT=vl, rhs=pT[pb_][:, nA:nA + nB],
                                               start=True, stop=lastu),
                     reads=[f"V{pi}", f"pT{pb_}"], writes=[f"ps{pB}"])
                for gb in range(nbanks):
                    if min(4 * gb + 3, n_t - 1) == u:
                        ma = max(0, 512 * gb - 64)
                        mb = min(L, 512 * gb + 448)
                        ca = ma + 64 - 512 * gb
                        n = mb - ma
                        asl = acc[ab][0:65, rho + r * ma:rho + r * (mb - 1) + 1:r]
                        pg_ = phys(gb)
                        psl = psum[pg_][0:65, ca:ca + n]
                        if pi == 0:
                            S.op("dve", lambda e, asl=asl, psl=psl: e.tensor_copy(out=asl, in_=psl),
                                 reads=[f"ps{pg_}"], writes=[accr])
                        else:
                            S.op("dve", lambda e, asl=asl, psl=psl: e.tensor_tensor(out=asl, in0=asl, in1=psl, op=ALU.add),
                                 reads=[f"ps{pg_}", accr], writes=[accr])

            LA = 2
            infos = []
            for k_ in range(min(LA, len(tiles))):
                infos.append(score(tiles[k_]))
            for k_ in range(len(tiles)):
                if k_ + LA < len(tiles):
                    infos.append(score(tiles[k_ + LA]))
                pv(tiles[k_], infos[k_])
            for c in range(8):
                db = 5 + (c % 2)
                rb = c % 2
                S.op("pe", lambda e, c=c, db=db: e.matmul(psum[db][0:64, :], lhsT=sel[:], rhs=acc[ab][0:65, c * 512:(c + 1) * 512],
                                                           start=True, stop=True),
                     reads=[accr, "sel"], writes=[f"ps{db}"])
                S.op("dve", lambda e, db=db, rb=rb: e.reciprocal(out=rec[rb][:], in_=psum[db][0:64, :]),
                     reads=[f"ps{db}"], writes=[f"rec{rb}"])
                S.op("pool", lambda e, c=c, rb=rb: e.tensor_tensor(out=outb[rb][:], in0=acc[ab][0:64, c * 512:(c + 1) * 512],
                                                                     in1=rec[rb][:], op=ALU.mult),
                     reads=[accr, f"rec{rb}"], writes=[f"aob{rb}"])
                S.dma(lambda e, c=c, rb=rb: e.dma_start(out=attnT_d[h * 64:(h + 1) * 64, c * 512:(c + 1) * 512], in_=outb[rb][:]),
                      reads=[f"aob{rb}"], writes=[])

        load_qk(0)
        for hh in range(2):
            load_V(hh)
            for hl_ in range(4):
                h = hh * 4 + hl_
                if h + 1 < 8:
                    load_qk(h + 1)
                head(h)
        S.end_phase()


def X(S, eng, meth, reads, writes, *a, **kw):
    S.op(eng, lambda e: getattr(e, meth)(*a, **kw), reads, writes)


def DM(S, out, in_, reads=(), writes=(), q="sp"):
    S.dma(lambda e: e.dma_start(out=out, in_=in_), reads, writes, q)


def phase_conv(C):
    nc, S, psum = C["nc"], C["S"], C["psum"]
    cwT_d = C["din"]("conv_wT", [128, 40])
    cbT_d = C["din"]("conv_bT", [128, 8])
    cbrow_d = C["din"]("conv_b_row", [1, 1024])
    bcT_d = C["dscr"]("bcT_d", [512, S_TOK], BF16)
    xsB_d = C["dscr"]("xsB_d", [S_TOK, 768], BF16)
    C["bcT_d"], C["xsB_d"] = bcT_d, xsB_d
    with contextlib.ExitStack() as ph:
        def sba(name, shape, dt):
            return ph.enter_context(nc.sbuf_tensor(name, list(shape), dt))
        xpre = sba("xpre", [128, 8, S_TOK + 4], BF16)
        dg = sba("dg", [128, 40, 128], BF16)
        ident = sba("ident_c", [128, 128], BF16)
        cwT = sba("cwT", [128, 40], F32)
        cbT = sba("cbT", [128, 8], F32)
        cbrow = sba("cbrow", [1, 1024], F32)
        cbrow_bf = sba("cbrow_bf", [1, 1024], BF16)
        ones1 = sba("ones1", [1, 128], BF16)
        ob = [sba(f"cob{i}", [128, 512], BF16) for i in range(3)]
        DM(S, ident[:], C["ident_d"], writes=["ident_c"])
        DM(S, cwT[:], cwT_d, writes=["cwT"])
        DM(S, cbT[:], cbT_d, writes=["cbT"])
        DM(S, cbrow[:], cbrow_d, writes=["cbrow"])
        xv = C["xbcT_d"].rearrange("(c p) t -> p c t", p=128)
        for j in range(8):
            DM(S, xpre[:, j, :], xv[:, j, :], writes=[f"xpre{j}"])
        X(S, "dve", "tensor_copy", ["cbrow"], ["cbrow_bf"], out=cbrow_bf[:], in_=cbrow[:])
        X(S, "pool", "memset", [], ["ones1"], ones1[:], 1.0)
        for jk in range(40):
            X(S, "dve", "tensor_scalar", ["ident_c", "cwT"], ["dg"], out=dg[:, jk, :], in0=ident[:],
              scalar1=cwT[:, jk:jk + 1], scalar2=None, op0=ALU.mult)
        bi = 0
        oi = 0
        for j in range(4, 8):
            for tc in range(8):
                b = bi % 2
                bi += 1
                for k in range(5):
                    X(S, "pe", "matmul", [f"xpre{j}", "dg"], [f"ps{b}"], psum[b][:, :], lhsT=dg[:, j * 5 + k, :],
                      rhs=xpre[:, j, tc * 512 + k:tc * 512 + k + 512], start=(k == 0), stop=(k == 4))
                o = oi % 3
                oi += 1
                X(S, "act", "activation", [f"ps{b}", "cbT"], [f"cob{o}"], out=ob[o][:], in_=psum[b][:, :], func=AF.Silu,
                  bias=cbT[:, j:j + 1])
                DM(S, bcT_d[(j - 4) * 128:(j - 3) * 128, tc * 512:(tc + 1) * 512], ob[o][:], reads=[f"cob{o}"])
        sz_d = C["dscr"]("sz_d", [S_TOK, 512], F32)
        C["sz_d"] = sz_d
        zin = [sba(f"zin{i}", [128, 512], F32) for i in range(2)]
        zou = [sba(f"zou{i}", [128, 512], F32) for i in range(2)]
        for tg in range(32):
            zb_ = tg % 2
            DM(S, zin[zb_][:], C["z_d"][tg * 128:(tg + 1) * 128, :], writes=[f"zin{zb_}"])
            X(S, "act", "activation", [f"zin{zb_}"], [f"zou{zb_}"], out=zou[zb_][:], in_=zin[zb_][:], func=AF.Silu)
            DM(S, sz_d[tg * 128:(tg + 1) * 128, :], zou[zb_][:], reads=[f"zou{zb_}"])
            bA = 2 + (tg % 2) * 2
            bB = bA + 1
            for j in range(6):
                bank = bA if j < 4 else bB
                col = (j % 4) * 128
                for k in range(5):
                    X(S, "pe", "matmul", [f"xpre{j}", "dg"], [f"ps{bank}"], psum[bank][:, col:col + 128],
                      lhsT=xpre[:, j, tg * 128 + k:tg * 128 + k + 128], rhs=dg[:, j * 5 + k, :], start=(k == 0), stop=False)
                X(S, "pe", "matmul", ["ones1", "cbrow_bf"], [f"ps{bank}"], psum[bank][:, col:col + 128],
                  lhsT=ones1[0:1, :], rhs=cbrow_bf[0:1, j * 128:(j + 1) * 128], start=False, stop=True)
            o = oi % 3
            oi += 1
            X(S, "act", "activation", [f"ps{bA}"], [f"cob{o}"], out=ob[o][:], in_=psum[bA][:, :], func=AF.Silu)
            DM(S, xsB_d[tg * 128:(tg + 1) * 128, 0:512], ob[o][:], reads=[f"cob{o}"])
            o = oi % 3
            oi += 1
            X(S, "act", "activation", [f"ps{bB}"], [f"cob{o}"], out=ob[o][:, 0:256], in_=psum[bB][:, 0:256], func=AF.Silu)
            DM(S, xsB_d[tg * 128:(tg + 1) * 128, 512:768], ob[o][:, 0:256], reads=[f"cob{o}"])
        S.end_phase()


def phase_ssd(C):
    nc, S, psum = C["nc"], C["S"], C["psum"]
    psT = C["psT"]
    din = C["din"]
    cst = {k: din(k, [128, 128]) for k in ("c_SL", "c_UI", "c_SU", "c_LI", "c_ones")}
    dtb_d = din("dtb_rep", [128, 512])
    alog_d = din("alog_rep", [128, 512])
    dskip_d = din("dskip_rep", [128, 512])
    nw_d = din("ssdnw_rep", [128, 512])
    ssdT_d = C["dscr"]("ssdT_d", [512, S_TOK], BF16)
    C["ssdT_d"] = ssdT_d
    xsB_d, bcT_d, z_d, dt_d = C["xsB_d"], C["bcT_d"], C["sz_d"], C["dt_d"]
    with contextlib.ExitStack() as ph:
        def sba(name, shape, dt):
            return ph.enter_context(nc.sbuf_tensor(name, list(shape), dt))
        K = {k: sba("k_" + k, [128, 128], F32) for k in cst}
        for k in cst:
            DM(S, K[k][:], cst[k], writes=[k])
        ident = sba("ident_s", [128, 128], BF16)
        DM(S, ident[:], C["ident_d"], writes=["ident_s"])
        G = {}
        for nm in ("dtr", "dtb", "alog", "dskip", "nw", "u", "au", "dt", "eA", "a", "tot", "acs", "cd", "d1", "d2",
                   "sarg", "oarg", "sdec", "odec", "wdt"):
            G[nm] = sba("g_" + nm, [128, 512], F32)
        DM(S, G["dtr"][:], dt_d, writes=["dtr"])
        DM(S, G["dtb"][:], dtb_d, writes=["dtb"])
        DM(S, G["alog"][:], alog_d, writes=["alog"])
        DM(S, G["dskip"][:], dskip_d, writes=["dskip"])
        DM(S, G["nw"][:], nw_d, writes=["nw"])

        def g(nm):
            return G[nm][:]

        def g3(nm):
            return G[nm][:].rearrange("p (c h) -> p c h", h=16)
        X(S, "dve", "tensor_tensor", ["dtr", "dtb"], ["u"], out=g("u"), in0=g("dtr"), in1=g("dtb"), op=ALU.add)
        X(S, "act", "activation", ["u"], ["au"], out=g("au"), in_=g("u"), func=AF.Exp)
        X(S, "act", "activation", ["au"], ["dt"], out=g("dt"), in_=g("au"), func=AF.Ln, bias=1.0)
        X(S, "act", "activation", ["alog"], ["eA"], out=g("eA"), in_=g("alog"), func=AF.Exp)
        X(S, "dve", "scalar_tensor_tensor", ["dt", "eA"], ["a"], out=g("a"), in0=g("dt"), scalar=-1.0, in1=g("eA"),
          op0=ALU.mult, op1=ALU.mult)
        X(S, "pe", "matmul", ["c_ones", "a"], ["ps0"], psum[0][:, :], lhsT=K["c_ones"][:], rhs=g("a"), start=True, stop=True)
        X(S, "pe", "matmul", ["c_UI", "a"], ["ps1"], psum[1][:, :], lhsT=K["c_UI"][:], rhs=g("a"), start=True, stop=True)
        X(S, "act", "copy", ["ps0"], ["tot"], out=g("tot"), in_=psum[0][:, :])
        X(S, "act", "copy", ["ps1"], ["acs"], out=g("acs"), in_=psum[1][:, :])
        X(S, "act", "activation", ["tot"], ["cd"], out=g("cd"), in_=g("tot"), func=AF.Exp)
        X(S, "dve", "tensor_tensor", ["tot", "acs"], ["d1"], out=g("d1"), in0=g("tot"), in1=g("acs"), op=ALU.subtract)
        X(S, "dve", "tensor_tensor", ["acs", "a"], ["d2"], out=g("d2"), in0=g("acs"), in1=g("a"), op=ALU.subtract)
        X(S, "pool", "tensor_copy", ["d1"], ["sarg"], out=g3("sarg")[:, :, 0:8], in_=g3("d1")[:, :, 0:8])
        X(S, "pool", "tensor_copy", ["d2"], ["sarg"], out=g3("sarg")[:, :, 8:16], in_=g3("d2")[:, :, 8:16])
        X(S, "pool", "tensor_copy", ["acs"], ["oarg"], out=g3("oarg")[:, :, 0:8], in_=g3("acs")[:, :, 0:8])
        X(S, "dve", "tensor_tensor", ["d1", "a"], ["oarg"], out=g3("oarg")[:, :, 8:16], in0=g3("d1")[:, :, 8:16],
          in1=g3("a")[:, :, 8:16], op=ALU.add)
        X(S, "act", "activation", ["sarg"], ["sdec"], out=g("sdec"), in_=g("sarg"), func=AF.Exp)
        X(S, "act", "activation", ["oarg"], ["odec"], out=g("odec"), in_=g("oarg"), func=AF.Exp)
        X(S, "dve", "tensor_tensor", ["dt", "sdec"], ["wdt"], out=g("wdt"), in0=g("dt"), in1=g("sdec"), op=ALU.mult)

        Fs = [[sba(f"Fs{d}_{k}", [128, 512], F32) for k in range(2)] for d in range(2)]
        Fstore = [sba(f"Fstore{d}", [128, 32, 512], BF16) for d in range(2)]
        xsB = [sba(f"xsB{i}", [128, 768], BF16) for i in range(4)]
        xsd = [sba(f"xsd{i}", [128, 512], BF16) for i in range(4)]
        for d in range(2):
            X(S, "pool", "memset", [], [f"Fs{d}_0"], Fs[d][0][:], 0.0)

        def p1_load(i_):
            for d in range(2):
                c = i_ if d == 0 else 31 - i_
                b = (i_ % 2) * 2 + d
                DM(S, xsB[b][:], xsB_d[c * 128:(c + 1) * 128, :], writes=[f"xsB{b}"])
        p1_load(0)
        for i_ in range(32):
            if i_ + 1 < 32:
                p1_load(i_ + 1)
            for d in range(2):
                c = i_ if d == 0 else 31 - i_
                b = (i_ % 2) * 2 + d
                cur, nxt = Fs[d][i_ % 2], Fs[d][(i_ + 1) % 2]
                rc, rn = f"Fs{d}_{i_ % 2}", f"Fs{d}_{(i_ + 1) % 2}"
                wv = g3("wdt")[:, c, d * 8:d * 8 + 8].unsqueeze(2).to_broadcast([128, 8, 64])
                X(S, "pool" if d == 0 else "dve", "tensor_tensor", [f"xsB{b}", "wdt"], [f"xsd{b}"],
                  out=xsd[b][:].rearrange("p (h d) -> p h d", h=8), in0=xsB[b][:, 0:512].rearrange("p (h d) -> p h d", h=8),
                  in1=wv, op=ALU.mult)
                pb = 1 + d
                for gq in range(2):
                    X(S, "pe", "matmul", [f"xsB{b}", f"xsd{b}"], [f"ps{pb}"], psum[pb][:, gq * 256:(gq + 1) * 256],
                      lhsT=xsB[b][:, 512 + gq * 128:512 + (gq + 1) * 128], rhs=xsd[b][:, gq * 256:(gq + 1) * 256],
                      start=True, stop=True)
                X(S, "act", "copy", [rc], [f"Fst{d}_{c}"], out=Fstore[d][:, c, :], in_=cur[:])
                cv = g3("cd")[:, c, d * 8:d * 8 + 8].unsqueeze(2).to_broadcast([128, 8, 64])
                X(S, "dve", "tensor_tensor", [rc, "cd"], [rn], out=nxt[:].rearrange("p (h d) -> p h d", h=8),
                  in0=cur[:].rearrange("p (h d) -> p h d", h=8), in1=cv, op=ALU.mult)
                X(S, "dve", "tensor_tensor", [rn, f"ps{pb}"], [rn], out=nxt[:], in0=nxt[:], in1=psum[pb][:, :], op=ALU.add)

        bc = [sba(f"bc{i}", [128, 4, 128], BF16) for i in range(3)]
        zt = [sba(f"zt{i}", [128, 512], F32) for i in range(3)]
        xdt = [sba(f"xdt{i}", [128, 512], BF16) for i in range(2)]
        Gm = [sba(f"Gm{i}", [128, 2, 128], BF16) for i in range(2)]
        lhs4 = [sba(f"lhs4_{i}", [128, 4, 128], F32) for i in range(2)]
        E4 = [sba(f"E4_{i}", [128, 4, 128], BF16) for i in range(2)]
        M4 = [sba(f"M4_{i}", [128, 4, 128], BF16) for i in range(2)]
        tA = sba("tA", [128, 512], F32)
        tB = sba("tB", [128, 512], F32)
        tU = sba("tU", [128, 512], F32)
        ez = sba("ez", [128, 512], F32)
        junk = sba("junk", [128, 256], F32)
        ss = sba("ss", [128, 2], F32)
        yb = sba("yb", [128, 512], BF16)
        yT = [sba(f"yT{i}", [128, 4, 128], BF16) for i in range(3)]
        bcv = bcT_d.rearrange("(q p) t -> p q t", p=128)
        ssdv = ssdT_d.rearrange("(q p) t -> p q t", p=128)
        qs = dict(qi=0)
        def p2_loads(c):
            b = c % 3
            DM(S, xsB[b][:], xsB_d[c * 128:(c + 1) * 128, :], writes=[f"xsB{b}"])
            DM(S, bc[b][:], bcv[:, :, c * 128:(c + 1) * 128], writes=[f"bc{b}"])
            DM(S, zt[b][:], z_d[c * 128:(c + 1) * 128, :], writes=[f"zt{b}"])
        def p2_front(c):
            b = c % 3
            xs3 = xsB[b][:, 0:512].rearrange("p (h d) -> p h d", h=8)
            for d in range(2):
                dv = g3("dt")[:, c, d * 8:d * 8 + 8].unsqueeze(2).to_broadcast([128, 8, 64])
                X(S, "dve", "tensor_tensor", [f"xsB{b}", "dt"], [f"xdt{d}"], out=xdt[d][:].rearrange("p (h d) -> p h d", h=8),
                  in0=xs3, in1=dv, op=ALU.mult)
            for gq in range(2):
                X(S, "pe", "matmul", [f"bc{b}"], ["ps0"], psum[0][:, gq * 128:(gq + 1) * 128], lhsT=bc[b][:, gq, :],
                  rhs=bc[b][:, 2 + gq, :], start=True, stop=True)
            p0v = psum[0][:, 0:256].rearrange("p (g l) -> p g l", g=2)
            X(S, "dve", "tensor_tensor", ["ps0", "c_UI"], ["Gm0"], out=Gm[0][:], in0=p0v,
              in1=K["c_UI"][:].unsqueeze(1).to_broadcast([128, 2, 128]), op=ALU.mult)
            X(S, "dve", "tensor_tensor", ["ps0", "c_LI"], ["Gm1"], out=Gm[1][:], in0=p0v,
              in1=K["c_LI"][:].unsqueeze(1).to_broadcast([128, 2, 128]), op=ALU.mult)
            quads = []
            for d in range(2):
                for gq in range(2):
                    q = qs["qi"] % 2
                    qs["qi"] += 1
                    quads.append((d, gq, q))

            def stA(d, gq, q):
                sb_ = 1 + q
                h0 = c * 16 + d * 8 + gq * 4
                av = G["a"][:, h0:h0 + 4].unsqueeze(2).to_broadcast([128, 4, 128])
                tri = K["c_SL" if d == 0 else "c_SU"][:].unsqueeze(1).to_broadcast([128, 4, 128])
                X(S, "dve", "tensor_tensor", ["a", "c_SL", "c_SU"], [f"lhs4_{q}"], out=lhs4[q][:], in0=tri, in1=av, op=ALU.mult)
                rk = K["c_UI" if d == 0 else "c_LI"]
                for i in range(4):
                    X(S, "pe", "matmul", [f"lhs4_{q}", "c_UI", "c_LI"], [f"ps{sb_}"], psum[sb_][:, i * 128:(i + 1) * 128],
                      lhsT=lhs4[q][:, i, :], rhs=rk[:], start=True, stop=True)
                X(S, "act", "activation", [f"ps{sb_}"], [f"E4_{q}"], out=E4[q][:].rearrange("p a b -> p (a b)"),
                  in_=psum[sb_][:, :], func=AF.Exp)
                X(S, "pool", "tensor_tensor", [f"E4_{q}", f"Gm{d}"], [f"M4_{q}"], out=M4[q][:], in0=E4[q][:],
                  in1=Gm[d][:, gq, :].unsqueeze(1).to_broadcast([128, 4, 128]), op=ALU.mult)

            def stB(d, gq, q):
                yb_ = 3 + d
                for i in range(4):
                    h = gq * 4 + i
                    X(S, "pe", "matmul", [f"M4_{q}", f"xdt{d}"], [f"ps{yb_}"], psum[yb_][:, h * 64:(h + 1) * 64],
                      lhsT=M4[q][:, i, :], rhs=xdt[d][:, h * 64:(h + 1) * 64], start=True, stop=True)
                X(S, "pe", "matmul", [f"bc{b}", f"Fst{d}_{c}"], [f"ps{5 + d}"], psum[5 + d][:, gq * 256:(gq + 1) * 256],
                  lhsT=bc[b][:, 2 + gq, :], rhs=Fstore[d][:, c, gq * 256:(gq + 1) * 256], start=True, stop=True)

            stA(*quads[0])
            stA(*quads[1])
            stB(*quads[0])
            stA(*quads[2])
            stB(*quads[1])
            stA(*quads[3])
            stB(*quads[2])
            stB(*quads[3])

        def p2_epi1(c):
            b = c % 3
            t3 = lambda t: t[:].rearrange("p (h d) -> p h d", h=8)
            for d, tt in ((0, tA), (1, tB)):
                ov = g3("odec")[:, c, d * 8:d * 8 + 8].unsqueeze(2).to_broadcast([128, 8, 64])
                X(S, "dve", "tensor_tensor", [f"ps{5 + d}", "odec"], [tt.name], out=t3(tt),
                  in0=psum[5 + d][:, :].rearrange("p (h d) -> p h d", h=8), in1=ov, op=ALU.mult)
                X(S, "dve", "tensor_tensor", [f"ps{3 + d}", tt.name], [tt.name], out=tt[:], in0=tt[:], in1=psum[3 + d][:, :], op=ALU.add)

        def p2_epi2(c):
            b = c % 3
            X(S, "pool", "tensor_tensor", ["tA", "tB"], ["tA"], out=tA[:], in0=tA[:], in1=tB[:], op=ALU.add)
            X(S, "pool", "tensor_tensor", [f"xsB{b}", "dskip"], ["tU"], out=tU[:], in0=xsB[b][:, 0:512], in1=g("dskip"), op=ALU.mult)
            X(S, "pool", "tensor_tensor", ["tA", "tU"], ["tA"], out=tA[:], in0=tA[:], in1=tU[:], op=ALU.add)
            X(S, "dve", "tensor_tensor", ["tA", f"zt{b}"], ["tA"], out=tA[:], in0=tA[:], in1=zt[b][:], op=ALU.mult)
            X(S, "dve", "tensor_tensor", ["tA"], ["tU"], out=tU[:], in0=tA[:], in1=tA[:], op=ALU.mult)
            X(S, "dve", "tensor_reduce", ["tU"], ["ss"], out=ss[:], in_=tU[:].rearrange("p (g f) -> p g f", g=2),
              axis=mybir.AxisListType.X, op=ALU.add)
            X(S, "dve", "tensor_scalar", ["ss"], ["ss"], out=ss[:], in0=ss[:], scalar1=1.0 / 256.0, scalar2=1e-5, op0=ALU.mult, op1=ALU.add)
            X(S, "act", "activation", ["ss"], ["ss"], out=ss[:], in_=ss[:], func=AF.Ln)
            X(S, "act", "activation", ["ss"], ["ss"], out=ss[:], in_=ss[:], func=AF.Exp, scale=-0.5)
            for gq in range(2):
                X(S, "dve", "scalar_tensor_tensor", ["tA", "ss", "nw"], ["yb"], out=yb[:, gq * 256:(gq + 1) * 256],
                  in0=tA[:, gq * 256:(gq + 1) * 256], scalar=ss[:, gq:gq + 1], in1=G["nw"][:, gq * 256:(gq + 1) * 256],
                  op0=ALU.mult, op1=ALU.mult)
            for q4 in range(4):
                X(S, "pe", "transpose", ["yb", "ident_s"], ["psT"], psT[:, q4 * 128:(q4 + 1) * 128], in_=yb[:, q4 * 128:(q4 + 1) * 128],
                  identity=ident[:])
            X(S, "act", "copy", ["psT"], [f"yT{b}"], out=yT[b][:].rearrange("p a b -> p (a b)"), in_=psT[:, 0:512])
            DM(S, ssdv[:, :, c * 128:(c + 1) * 128], yT[b][:], reads=[f"yT{b}"])

        p2_loads(0)
        p2_loads(1)
        p2_front(0)
        for c in range(32):
            if c + 2 < 32:
                p2_loads(c + 2)
            p2_epi1(c)
            if c + 1 < 32:
                p2_front(c + 1)
            p2_epi2(c)
        S.end_phase()


ALPHA = float((2.0 * 1) ** 0.25)
NROWS = N_EXP * CAP


def phase_mix(C):
    nc, S, psum, psT = C["nc"], C["S"], C["psum"], C["psT"]
    din, dscr = C["din"], C["dscr"]
    wout_d = din("w_out", [D, D])
    anw_d = din("anwT", [128, 4])
    l1w_d = din("ln1w_rep", [128, D])
    l1b_d = din("ln1b_rep", [128, D])
    rw_d = din("router_w", [D, N_EXP])
    rb_d = din("rb_rep", [128, N_EXP])
    ecap_d = din("ecap_rep", [128, N_EXP])
    su_d = din("c_SU2", [128, 128])
    ones_d = din("c_ones2", [128, 128])
    h1_d = dscr("h1_d", [S_TOK, D], F32)
    Xg_d = dscr("Xg_d", [NROWS, D], BF16)
    C["h1_d"], C["Xg_d"] = h1_d, Xg_d
    gates_all, dest_all = C["gates_all"], C["dest_all"]
    attnT_d, ssdT_d, x = C["attnT_d"], C["ssdT_d"], C["x"]
    with contextlib.ExitStack() as ph:
        def sba(name, shape, dt):
            return ph.enter_context(nc.sbuf_tensor(name, list(shape), dt))
        wo = sba("wo", [128, 8, D], BF16)
        wst = [sba(f"wost{i}", [128, 2, D], F32) for i in range(2)]
        anw = sba("anw", [128, 4], F32)
        l1w = sba("l1w", [128, D], F32)
        l1b = sba("l1b", [128, D], F32)
        rwf = sba("rwf", [128, 8, N_EXP], F32)
        rwb = sba("rwb", [128, 8, N_EXP], BF16)
        rb = sba("rb", [128, N_EXP], F32)
        base = sba("base", [128, N_EXP], F32)
        SU = sba("SU2", [128, 128], F32)
        ones = sba("ones2", [128, 128], F32)
        ident = sba("ident_m", [128, 128], BF16)
        DM(S, ident[:], C["ident_d"], writes=["ident_m"])
        DM(S, anw[:], anw_d, writes=["anw"])
        DM(S, l1w[:], l1w_d, writes=["l1w"])
        DM(S, l1b[:], l1b_d, writes=["l1b"])
        DM(S, rwf[:], rw_d.rearrange("(c p) e -> p c e", p=128), writes=["rwf"])
        DM(S, rb[:], rb_d, writes=["rb"])
        DM(S, base[:], ecap_d, writes=["base"])
        DM(S, SU[:], su_d, writes=["SU2"])
        DM(S, ones[:], ones_d, writes=["ones2"])
        X(S, "dve", "tensor_copy", ["rwf"], ["rwb"], out=rwb[:], in_=rwf[:])
        X(S, "pool", "memset", [], [f"dest{t}" for t in range(32)], dest_all[:], 0)
        wov = wout_d.rearrange("(c p) f -> p c f", p=128)
        for i in range(4):
            b = i % 2
            DM(S, wst[b][:], wov[:, 2 * i:2 * i + 2, :], writes=[f"wost{b}"])
            if i < 2:
                X(S, "dve", "tensor_tensor", [f"wost{b}", "anw"], ["wo"], out=wo[:, 2 * i:2 * i + 2, :], in0=wst[b][:],
                  in1=anw[:, 2 * i:2 * i + 2].unsqueeze(2).to_broadcast([128, 2, D]), op=ALU.mult)
            else:
                X(S, "dve", "tensor_copy", [f"wost{b}"], ["wo"], out=wo[:, 2 * i:2 * i + 2, :], in_=wst[b][:])
        aT = [sba(f"aT{i}", [128, 4, 128], BF16) for i in range(3)]
        sT = [sba(f"sT{i}", [128, 4, 128], BF16) for i in range(3)]
        xt = [sba(f"xt{i}", [128, D], F32) for i in range(3)]
        gjunk = sba("gjunk", [128, 128], F32)
        identf = sba("identf", [128, 128], F32)
        X(S, "dve", "tensor_copy", ["ident_m"], ["identf"], out=identf[:], in_=ident[:])
        sm = sba("sm", [128, 8], F32)
        sm2 = sba("sm2", [128, 8], F32)
        tt = sba("tmix", [128, D], F32)
        stats = sba("stats", [128, 12], F32)
        mv = sba("mv", [128, 2], F32)
        h1f = [sba(f"h1f{i}", [128, D], F32) for i in range(2)]
        h1b = [sba(f"h1b{i}", [128, D], BF16) for i in range(4)]
        h1T = sba("h1T", [128, D], BF16)
        lg = sba("lg", [128, N_EXP], F32)
        top8 = sba("top8", [128, 8], F32)
        msk = sba("msk", [128, N_EXP], F32)
        ex4 = sba("ex4", [128, 4], F32)
        oh = sba("oh", [128, 4, N_EXP], F32)
        posd = sba("posd", [128, N_EXP], F32)
        destf = sba("destf", [128, 4], F32)
        av = attnT_d.rearrange("(q p) t -> p q t", p=128)
        sv = ssdT_d.rearrange("(q p) t -> p q t", p=128)
        def mix_loads(tg):
            b3 = tg % 3
            tsl = slice(tg * 128, (tg + 1) * 128)
            DM(S, aT[b3][:], av[:, :, tsl], writes=[f"aT{b3}"])
            DM(S, sT[b3][:], sv[:, :, tsl], writes=[f"sT{b3}"])
            DM(S, xt[b3][:], x[tsl, :], writes=[f"xt{b3}"])

        def mix_s1(tg):
            b = tg % 2
            b3 = tg % 3
            tsl = slice(tg * 128, (tg + 1) * 128)
            for q in range(4):
                X(S, "pe", "matmul", [f"aT{b3}"], ["ps6"], psum[6][:, 0:128], lhsT=aT[b3][:, q, :], rhs=aT[b3][:, q, :],
                  start=(q == 0), stop=(q == 3))
            X(S, "dve", "tensor_tensor", ["ps6", "identf"], ["gjunk"], out=gjunk[:], in0=psum[6][:, 0:128], in1=identf[:], op=ALU.mult)
            X(S, "dve", "tensor_reduce", ["gjunk"], ["sm"], out=sm[:, 7:8], in_=gjunk[:], axis=mybir.AxisListType.X, op=ALU.add)
            X(S, "dve", "tensor_scalar", ["sm"], ["sm"], out=sm[:, 0:1], in0=sm[:, 7:8], scalar1=1.0 / 512.0, scalar2=1e-5,
              op0=ALU.mult, op1=ALU.add)
            X(S, "act", "activation", ["sm"], ["sm"], out=sm[:, 1:2], in_=sm[:, 0:1], func=AF.Ln)
            X(S, "act", "activation", ["sm"], ["sm"], out=sm[:, 2:3], in_=sm[:, 1:2], func=AF.Exp, scale=-0.5)
            for hf in range(2):
                for q in range(4):
                    X(S, "pe", "matmul", [f"aT{b3}", "wo"], [f"ps{hf}"], psum[hf][:, :], lhsT=aT[b3][:, q, :],
                      rhs=wo[:, q, hf * 512:(hf + 1) * 512], start=(q == 0), stop=(q == 3))
                for q in range(4):
                    X(S, "pe", "matmul", [f"sT{b3}", "wo"], [f"ps{2 + hf}"], psum[2 + hf][:, :], lhsT=sT[b3][:, q, :],
                      rhs=wo[:, 4 + q, hf * 512:(hf + 1) * 512], start=(q == 0), stop=(q == 3))
            for hf in range(2):
                hs = slice(hf * 512, (hf + 1) * 512)
                X(S, "dve", "scalar_tensor_tensor", [f"xt{b3}", f"ps{2 + hf}"], ["tmix"], out=tt[:, hs], in0=xt[b3][:, hs], scalar=ALPHA,
                  in1=psum[2 + hf][:, :], op0=ALU.mult, op1=ALU.add)
                X(S, "dve", "scalar_tensor_tensor", ["tmix", f"ps{hf}", "sm"], ["tmix"], out=tt[:, hs], in0=psum[hf][:, :],
                  scalar=sm[:, 2:3], in1=tt[:, hs], op0=ALU.mult, op1=ALU.add)
                X(S, "dve", "bn_stats", ["tmix"], ["stats"], out=stats[:, hf * 6:(hf + 1) * 6], in_=tt[:, hs])
            X(S, "dve", "bn_aggr", ["stats"], ["mv"], out=mv[:], in_=stats[:])
            X(S, "act", "activation", ["mv"], ["sm"], out=sm[:, 3:4], in_=mv[:, 1:2], func=AF.Ln, bias=1e-5)
            X(S, "act", "activation", ["sm"], ["sm"], out=sm[:, 4:5], in_=sm[:, 3:4], func=AF.Exp, scale=-0.5)
            X(S, "dve", "tensor_scalar", ["tmix", "mv", "sm"], ["tmix"], out=tt[:], in0=tt[:], scalar1=mv[:, 0:1], scalar2=sm[:, 4:5],
              op0=ALU.subtract, op1=ALU.mult)
            X(S, "dve", "tensor_tensor", ["tmix", "l1w"], ["tmix"], out=tt[:], in0=tt[:], in1=l1w[:], op=ALU.mult)
            X(S, "pool", "tensor_tensor", ["tmix", "l1b"], [f"h1f{b}"], out=h1f[b][:], in0=tt[:], in1=l1b[:], op=ALU.add)
            X(S, "act", "copy", [f"h1f{b}"], [f"h1b{tg % 4}"], out=h1b[tg % 4][:], in_=h1f[b][:])
            DM(S, h1_d[tsl, :], h1f[b][:], reads=[f"h1f{b}"])

        def mix_s2(tg):
            b = tg % 2
            tsl = slice(tg * 128, (tg + 1) * 128)
            for kc in range(8):
                X(S, "pe", "transpose", [f"h1b{tg % 4}", "ident_m"], ["psT"], psT[:, kc * 128:(kc + 1) * 128],
                  in_=h1b[tg % 4][:, kc * 128:(kc + 1) * 128], identity=ident[:])
            X(S, "act", "copy", ["psT"], ["h1T"], out=h1T[:], in_=psT[:, :])
            for kc in range(8):
                X(S, "pe", "matmul", ["h1T", "rwb"], ["ps4"], psum[4][:, 0:N_EXP], lhsT=h1T[:, kc * 128:(kc + 1) * 128],
                  rhs=rwb[:, kc, :], start=(kc == 0), stop=(kc == 7))
            X(S, "dve", "tensor_tensor", ["ps4", "rb"], ["lg"], out=lg[:], in0=psum[4][:, 0:N_EXP], in1=rb[:], op=ALU.add)
            X(S, "dve", "max", ["lg"], ["top8"], out=top8[:], in_=lg[:])
            X(S, "dve", "tensor_scalar", ["lg", "top8"], ["msk"], out=msk[:], in0=lg[:], scalar1=top8[:, 3:4], scalar2=None, op0=ALU.is_ge)
            X(S, "dve", "tensor_scalar", ["top8"], ["sm2"], out=sm2[:, 5:6], in0=top8[:, 0:1], scalar1=-1.0, scalar2=None, op0=ALU.mult)
            X(S, "act", "activation", ["top8", "sm2"], ["ex4"], out=ex4[:], in_=top8[:, 0:4], func=AF.Exp, bias=sm2[:, 5:6])
            X(S, "dve", "tensor_reduce", ["ex4"], ["sm2"], out=sm2[:, 6:7], in_=ex4[:], axis=mybir.AxisListType.X, op=ALU.add)
            X(S, "dve", "reciprocal", ["sm2"], ["sm2"], out=sm2[:, 7:8], in_=sm2[:, 6:7])
            X(S, "dve", "tensor_scalar", ["ex4", "sm2"], ["gates"], out=gates_all[:, tg, :], in0=ex4[:], scalar1=sm2[:, 7:8], scalar2=None,
              op0=ALU.mult)
            X(S, "pe", "matmul", ["SU2", "msk"], ["ps5"], psum[5][:, 0:N_EXP], lhsT=SU[:], rhs=msk[:], start=True, stop=True)
            X(S, "pe", "matmul", ["ones2", "msk"], ["ps5"], psum[5][:, 64:64 + N_EXP], lhsT=ones[:], rhs=msk[:], start=True, stop=True)
            X(S, "dve", "tensor_tensor", ["ps5", "base"], ["posd"], out=posd[:], in0=psum[5][:, 0:N_EXP], in1=base[:], op=ALU.add)
            X(S, "dve", "tensor_tensor", ["ps5", "base"], ["base"], out=base[:], in0=psum[5][:, 64:64 + N_EXP], in1=base[:], op=ALU.add)
            X(S, "dve", "tensor_tensor", ["lg", "top8"], ["oh"], out=oh[:], in0=lg[:].unsqueeze(1).to_broadcast([128, 4, N_EXP]),
              in1=top8[:, 0:4].unsqueeze(2).to_broadcast([128, 4, N_EXP]), op=ALU.is_equal)
            X(S, "dve", "tensor_tensor", ["oh", "posd"], ["oh"], out=oh[:], in0=oh[:],
              in1=posd[:].unsqueeze(1).to_broadcast([128, 4, N_EXP]), op=ALU.mult)
            X(S, "dve", "tensor_reduce", ["oh"], ["destf"], out=destf[:], in_=oh[:], axis=mybir.AxisListType.X, op=ALU.add)
            X(S, "dve", "tensor_copy", ["destf"], [f"dest{tg}"], out=dest_all[:, tg, :], in_=destf[:])

        def mix_scatter(tg):
            b = tg % 4
            for j in range(4):
                idx = dest_all[:, tg, j:j + 1]
                src = h1b[b][:]
                S.dma(lambda e, idx=idx, src=src: e.indirect_dma_start(
                    out=Xg_d, out_offset=bass.IndirectOffsetOnAxis(ap=idx, axis=0), in_=src, in_offset=None,
                    bounds_check=S.bc(e), oob_is_err=False),
                    reads=[f"dest{tg}", f"h1b{b}"], writes=[f"Xg{tg}_{j}"], q="pool")

        mix_loads(0)
        mix_loads(1)
        mix_loads(2)
        mix_s1(0)
        mix_s1(1)
        for tg in range(32):
            if tg + 3 < 32:
                mix_loads(tg + 3)
            if tg + 2 < 32:
                mix_s1(tg + 2)
            mix_s2(tg)
            if tg >= 1:
                mix_scatter(tg - 1)
        mix_scatter(31)
        ecs = sba("ecs", [128, N_EXP], F32)
        cntf = sba("cntf", [128, N_EXP], F32)
        DM(S, ecs[:], ecap_d, writes=["ecs"])
        X(S, "dve", "tensor_tensor", ["base", "ecs"], ["cntf"], out=cntf[:], in0=base[:], in1=ecs[:], op=ALU.subtract)
        flf = sba("flf", [128, N_EXP, 8], F32)
        for t in range(8):
            X(S, "dve", "tensor_scalar", ["cntf"], ["flf"], out=flf[:, :, t], in0=cntf[:], scalar1=float(128 * t), scalar2=None, op0=ALU.is_gt)
        X(S, "dve", "tensor_copy", ["flf"], ["flags"], out=C["flags_all"][:], in_=flf[:].rearrange("p e t -> p (e t)"))
        if C["debug"]:
            dd = dscr("dest_dbg", [128, 128], I32)
            gd = dscr("gates_dbg", [128, 128], F32)
            DM(S, dd, dest_all[:].rearrange("p a b -> p (a b)"), reads=[f"dest{t}" for t in range(32)])
            DM(S, gd, gates_all[:].rearrange("p a b -> p (a b)"), reads=["gates"])
        S.end_phase()


def phase_experts(C):
    nc, S, psum, psT = C["nc"], C["S"], C["psum"], C["psT"]
    din, dscr = C["din"], C["dscr"]
    wg_d = din("w_gate", [N_EXP, D, D])
    wu_d = din("w_up", [N_EXP, D, D])
    wd_d = din("w_down", [N_EXP, D, D])
    bg_d = din("bgT", [128, N_EXP * 8])
    bu_d = din("buT", [128, N_EXP * 8])
    bd_d = din("b_down", [N_EXP, D])
    Yg_d = dscr("Yg_d", [NROWS, D], F32)
    C["Yg_d"] = Yg_d
    Xg_d = C["Xg_d"]
    NT = CAP // 128
    psTs = [psT, psum[6][:, :].bitcast(BF16)]
    psTn = ["psT", "ps6"]
    with contextlib.ExitStack() as ph:
        def sba(name, shape, dt):
            return ph.enter_context(nc.sbuf_tensor(name, list(shape), dt))
        NS = 4
        wsl = [sba(f"wsl{i}", [128, 8, D], BF16) for i in range(NS)]
        wst = [sba(f"west{i}", [128, 2, D], F32) for i in range(2)]
        bg = sba("bg", [128, N_EXP * 8], F32)
        bu = sba("bu", [128, N_EXP * 8], F32)
        bd = [sba(f"bd{i}", [128, D], F32) for i in range(2)]
        ident = sba("ident_e", [128, 128], BF16)
        NXG = 8
        xg = [sba(f"xg{i}", [128, D], BF16) for i in range(NXG)]
        XT = [sba(f"XT{i}", [128, 8, CAP], BF16) for i in range(2)]
        actT = sba("actT", [128, 8, CAP], BF16)
        g1 = [sba(f"g1_{i}", [128, 512], F32) for i in range(2)]
        u1 = [sba(f"u1_{i}", [128, 512], F32) for i in range(2)]
        sg = [sba(f"sg_{i}", [128, 512], F32) for i in range(2)]
        yo = [sba(f"yo{i}", [128, D], F32) for i in range(2)]
        DM(S, ident[:], C["ident_d"], writes=["ident_e"])
        DM(S, bg[:], bg_d, writes=["bg"])
        DM(S, bu[:], bu_d, writes=["bu"])
        st = dict(slot=0, stg=0, xg=0, tp=0)

        def load_w_pieces(src, e):
            sl = st["slot"] % NS
            st["slot"] += 1
            v = src[e].rearrange("(c p) f -> p c f", p=128)

            def piece(i):
                b = st["stg"] % 2
                st["stg"] += 1
                DM(S, wst[b][:], v[:, 2 * i:2 * i + 2, :], writes=[f"west{b}"])
                X(S, "act", "copy", [f"west{b}"], [f"wsl{sl}"], out=wsl[sl][:, 2 * i:2 * i + 2, :], in_=wst[b][:])
            return sl, [lambda i=i: piece(i) for i in range(4)]

        def load_w(src, e):
            sl, ps = load_w_pieces(src, e)
            for p in ps:
                p()
            return sl

        def xg_loads(e):
            for t in range(NT):
                r0 = e * CAP + t * 128
                DM(S, xg[t][:], Xg_d[r0:r0 + 128, :], writes=[f"xg{t}"])

        flags = C["flags_all"]

        def xt_transposes(e, t0, t1):
            xb = e % 2
            for t in range(t0, t1):
                tp = st["tp"] % 2
                st["tp"] += 1
                for kc in range(8):
                    X(S, "pe", "transpose", [f"xg{t}", "ident_e"], [psTn[tp]], psTs[tp][:, kc * 128:(kc + 1) * 128],
                      in_=xg[t][:, kc * 128:(kc + 1) * 128], identity=ident[:])
                pv_ = psTs[tp][:, :].rearrange("p (c t) -> p c t", c=8)
                X(S, "dve", "tensor_copy", [psTn[tp]], [f"XT{xb}"], out=XT[xb][:, :, t * 128:(t + 1) * 128], in_=pv_)

        def fl_(e, t):
            return flags[0:1, e * 8 + t:e * 8 + t + 1]

        def xt_all(e):
            xt_transposes(e, 0, 3)
            for t in range(3, 8):
                S.cond_begin(fl_(e, t))
                xt_transposes(e, t, t + 1)
                S.cond_end()

        def gu_unit_small(e, fc, sg_, su_, xb, n0, nn, q):
            ns = slice(n0, n0 + nn)
            pg, pu = 2 * q, 2 * q + 1
            ar = f"actT{n0 // 256}"
            for kc in range(8):
                X(S, "pe", "matmul", [f"wsl{sg_}", f"XT{xb}"], [f"ps{pg}"], psum[pg][:, 0:nn], lhsT=wsl[sg_][:, kc, fc * 128:(fc + 1) * 128],
                  rhs=XT[xb][:, kc, ns], start=(kc == 0), stop=(kc == 7))
            for kc in range(8):
                X(S, "pe", "matmul", [f"wsl{su_}", f"XT{xb}"], [f"ps{pu}"], psum[pu][:, 0:nn], lhsT=wsl[su_][:, kc, fc * 128:(fc + 1) * 128],
                  rhs=XT[xb][:, kc, ns], start=(kc == 0), stop=(kc == 7))
            bcol = e * 8 + fc
            X(S, "dve", "tensor_scalar", [f"ps{pg}", "bg"], [f"g1_{q}"], out=g1[q][:, 0:nn], in0=psum[pg][:, 0:nn], scalar1=bg[:, bcol:bcol + 1],
              scalar2=7.0, op0=ALU.add, op1=ALU.min)
            X(S, "dve", "tensor_scalar", [f"ps{pu}", "bu"], [f"u1_{q}"], out=u1[q][:, 0:nn], in0=psum[pu][:, 0:nn], scalar1=bu[:, bcol:bcol + 1],
              scalar2=7.0, op0=ALU.add, op1=ALU.min)
            X(S, "dve", "tensor_scalar", [f"u1_{q}"], [f"u1_{q}"], out=u1[q][:, 0:nn], in0=u1[q][:, 0:nn], scalar1=-7.0, scalar2=1.0,
              op0=ALU.max, op1=ALU.add)
            X(S, "act", "activation", [f"g1_{q}"], [f"sg_{q}"], out=sg[q][:, 0:nn], in_=g1[q][:, 0:nn], func=AF.Sigmoid, scale=1.702)
            X(S, "pool", "tensor_tensor", [f"g1_{q}", f"sg_{q}"], [f"sg_{q}"], out=sg[q][:, 0:nn], in0=g1[q][:, 0:nn], in1=sg[q][:, 0:nn], op=ALU.mult)
            X(S, "pool", "tensor_tensor", [f"u1_{q}", f"sg_{q}"], [ar], out=actT[:, fc, ns], in0=sg[q][:, 0:nn], in1=u1[q][:, 0:nn], op=ALU.mult)

        def gu_unit(e, fc, hf, sg_, su_, xb, n0=None, nn=512):
            q = st["ei"] % 2
            st["ei"] += 1
            if n0 is None:
                n0 = hf * 512
            ns = slice(n0, n0 + nn)
            pg, pu = 2 * q, 2 * q + 1
            if nn != 512:
                return gu_unit_small(e, fc, sg_, su_, xb, n0, nn, q)
            for kc in range(8):
                X(S, "pe", "matmul", [f"wsl{sg_}", f"XT{xb}"], [f"ps{pg}"], psum[pg][:, :], lhsT=wsl[sg_][:, kc, fc * 128:(fc + 1) * 128],
                  rhs=XT[xb][:, kc, ns], start=(kc == 0), stop=(kc == 7))
            for kc in range(8):
                X(S, "pe", "matmul", [f"wsl{su_}", f"XT{xb}"], [f"ps{pu}"], psum[pu][:, :], lhsT=wsl[su_][:, kc, fc * 128:(fc + 1) * 128],
                  rhs=XT[xb][:, kc, ns], start=(kc == 0), stop=(kc == 7))
            bcol = e * 8 + fc
            X(S, "dve", "tensor_scalar", [f"ps{pg}", "bg"], [f"g1_{q}"], out=g1[q][:], in0=psum[pg][:, :], scalar1=bg[:, bcol:bcol + 1],
              scalar2=7.0, op0=ALU.add, op1=ALU.min)
            X(S, "dve", "tensor_scalar", [f"ps{pu}", "bu"], [f"u1_{q}"], out=u1[q][:], in0=psum[pu][:, :], scalar1=bu[:, bcol:bcol + 1],
              scalar2=7.0, op0=ALU.add, op1=ALU.min)
            X(S, "dve", "tensor_scalar", [f"u1_{q}"], [f"u1_{q}"], out=u1[q][:], in0=u1[q][:], scalar1=-7.0, scalar2=1.0,
              op0=ALU.max, op1=ALU.add)
            X(S, "act", "activation", [f"g1_{q}"], [f"sg_{q}"], out=sg[q][:], in_=g1[q][:], func=AF.Sigmoid, scale=1.702)
            X(S, "pool", "tensor_tensor", [f"g1_{q}", f"sg_{q}"], [f"sg_{q}"], out=sg[q][:], in0=g1[q][:], in1=sg[q][:], op=ALU.mult)
            X(S, "pool", "tensor_tensor", [f"u1_{q}", f"sg_{q}"], ["actT0", "actT1"], out=actT[:, fc, ns], in0=sg[q][:], in1=u1[q][:], op=ALU.mult)

        def down_tile(e, t, sd_, bb):
            yb_ = t % 2
            for hf in range(2):
                pb = 4 + hf
                for fc in range(8):
                    X(S, "pe", "matmul", [f"wsl{sd_}", f"actT{t // 2}"], [f"ps{pb}"], psum[pb][:, :], lhsT=actT[:, fc, t * 128:(t + 1) * 128],
                      rhs=wsl[sd_][:, fc, hf * 512:(hf + 1) * 512], start=(fc == 0), stop=(fc == 7))
                X(S, "dve", "tensor_tensor", [f"ps{pb}", f"bd{bb}"], [f"yo{yb_}"], out=yo[yb_][:, hf * 512:(hf + 1) * 512], in0=psum[pb][:, :],
                  in1=bd[bb][:, hf * 512:(hf + 1) * 512], op=ALU.add)
            r0 = e * CAP + t * 128
            DM(S, Yg_d[r0:r0 + 128, :], yo[yb_][:], reads=[f"yo{yb_}"], writes=[], q="act")

        st["ei"] = 0
        xg_loads(0)
        sg_, su_ = load_w(wg_d, 0), load_w(wu_d, 0)
        xt_all(0)
        for e in range(N_EXP):
            xb = e % 2
            fl = flags[0:1, e:e + 1]
            if e + 1 < N_EXP:
                xg_loads(e + 1)
            sd_, todo = load_w_pieces(wd_d, e)
            if e + 1 < N_EXP:
                sg_n, todo2 = load_w_pieces(wg_d, e + 1)
                todo = todo + todo2
            bb = e % 2
            DM(S, bd[bb][:], bd_d[e:e + 1, :].partition_broadcast(128), writes=[f"bd{bb}"])
            for fc in range(8):
                gu_unit(e, fc, 0, sg_, su_, xb)
                if todo:
                    todo.pop(0)()
            while todo:
                todo.pop(0)()
            for qq in range(2):
                S.cond_begin(fl_(e, 4 + 2 * qq))
                for fc in range(8):
                    gu_unit(e, fc, 1, sg_, su_, xb, n0=512 + 256 * qq, nn=256)
                S.cond_end()
            todo3 = []
            if e + 1 < N_EXP:
                su_n, todo3 = load_w_pieces(wu_d, e + 1)
                xt_all(e + 1)
            for t in range(3):
                if todo3:
                    todo3.pop(0)()
                down_tile(e, t, sd_, bb)
            while todo3:
                todo3.pop(0)()
            for t in range(3, 8):
                S.cond_begin(fl_(e, t))
                down_tile(e, t, sd_, bb)
                S.cond_end()
            if e + 1 < N_EXP:
                sg_, su_ = sg_n, su_n
        S.end_phase()


def phase_combine(C):
    nc, S = C["nc"], C["S"]
    din, dscr = C["din"], C["dscr"]
    l2w_d = din("ln2w_rep", [128, D])
    l2b_d = din("ln2b_rep", [128, D])
    out_d = nc.dram_tensor("out", [S_TOK, D], F32, kind="ExternalOutput").ap()
    Yg_d, h1_d = C["Yg_d"], C["h1_d"]
    gates_all, dest_all = C["gates_all"], C["dest_all"]
    with contextlib.ExitStack() as ph:
        def sba(name, shape, dt):
            return ph.enter_context(nc.sbuf_tensor(name, list(shape), dt))
        l2w = sba("l2w", [128, D], F32)
        l2b = sba("l2b", [128, D], F32)
        DM(S, l2w[:], l2w_d, writes=["l2w"])
        DM(S, l2b[:], l2b_d, writes=["l2b"])
        yg = [[sba(f"yg{i}_{j}", [128, D], F32) for j in range(4)] for i in range(2)]
        h1 = [sba(f"h1c{i}", [128, D], F32) for i in range(2)]
        acc = sba("cacc", [128, D], F32)
        ot = [sba(f"cot{i}", [128, D], F32) for i in range(2)]
        stats = sba("cstats", [128, 12], F32)
        mv = sba("cmv", [128, 2], F32)
        sm = sba("csm", [128, 4], F32)
        def comb_gather(tg):
            b = tg % 2
            tsl = slice(tg * 128, (tg + 1) * 128)
            DM(S, h1[b][:], h1_d[tsl, :], writes=[f"h1c{b}"])
            for j in range(4):
                idx = dest_all[:, tg, j:j + 1]
                dst = yg[b][j][:]
                S.dma(lambda e, idx=idx, dst=dst: e.indirect_dma_start(
                    out=dst, out_offset=None, in_=Yg_d, in_offset=bass.IndirectOffsetOnAxis(ap=idx, axis=0),
                    bounds_check=S.bc(e), oob_is_err=False),
                    reads=[], writes=[f"yg{b}_{j}"], q="pool")
        comb_gather(0)
        for tg in range(32):
            b = tg % 2
            tsl = slice(tg * 128, (tg + 1) * 128)
            if tg + 1 < 32:
                comb_gather(tg + 1)
            X(S, "dve", "tensor_scalar", [f"h1c{b}"], ["cacc"], out=acc[:], in0=h1[b][:], scalar1=ALPHA, scalar2=None, op0=ALU.mult)
            for j in range(4):
                X(S, "dve", "scalar_tensor_tensor", [f"yg{b}_{j}", "cacc"], ["cacc"], out=acc[:], in0=yg[b][j][:],
                  scalar=gates_all[:, tg, j:j + 1], in1=acc[:], op0=ALU.mult, op1=ALU.add)
            for hf in range(2):
                X(S, "dve", "bn_stats", ["cacc"], ["cstats"], out=stats[:, hf * 6:(hf + 1) * 6], in_=acc[:, hf * 512:(hf + 1) * 512])
            X(S, "dve", "bn_aggr", ["cstats"], ["cmv"], out=mv[:], in_=stats[:])
            X(S, "act", "activation", ["cmv"], ["csm"], out=sm[:, 0:1], in_=mv[:, 1:2], func=AF.Ln, bias=1e-5)
            X(S, "act", "activation", ["csm"], ["csm"], out=sm[:, 1:2], in_=sm[:, 0:1], func=AF.Exp, scale=-0.5)
            X(S, "dve", "tensor_scalar", ["cacc", "cmv", "csm"], ["cacc"], out=acc[:], in0=acc[:], scalar1=mv[:, 0:1], scalar2=sm[:, 1:2],
              op0=ALU.subtract, op1=ALU.mult)
            X(S, "dve", "tensor_tensor", ["cacc", "l2w"], ["cacc"], out=acc[:], in0=acc[:], in1=l2w[:], op=ALU.mult)
            X(S, "dve", "tensor_tensor", ["cacc", "l2b"], [f"cot{b}"], out=ot[b][:], in0=acc[:], in1=l2b[:], op=ALU.add)
            DM(S, out_d[tsl, :], ot[b][:], reads=[f"cot{b}"])
        S.end_phase()


def build(debug=False):
    nc = bass.Bass("TRN2", target_bir_lowering=False)
    es = contextlib.ExitStack()
    with es:
        def din(name, shape, dt=F32):
            return nc.dram_tensor(name, list(shape), dt, kind="ExternalInput").ap()

        def dscr(name, shape, dt, out=False):
            kind = "ExternalOutput" if (out or debug) else "Internal"
            return nc.dram_tensor(name, list(shape), dt, kind=kind).ap()

        xT = din("xT", [D, S_TOK])
        x = din("x", [S_TOK, D])
        w_in = din("w_in_ext", [D, WCOLS])
        cosT = din("cosT", [128, S_TOK])
        sinT = din("sinT", [128, S_TOK])

        qT_d = dscr("qT_d", [512, S_TOK], BF16)
        kT_d = dscr("kT_d", [512, S_TOK], BF16)
        v_d = dscr("v_d", [S_TOK, 512], BF16)
        z_d = dscr("z_d", [S_TOK, 512], F32)
        dt_d = dscr("dt_d", [128, 512], F32)
        xbcT_d = dscr("xbcT_d", [1024, S_TOK + 4], BF16)

        S = Sched(nc, es)
        psum = [es.enter_context(nc.psum_tensor(f"ps{i}", [128, 512], F32)) for i in range(7)]
        psT = es.enter_context(nc.psum_tensor("psT", [128, 1024], BF16))

        def sb(name, shape, dt):
            return es.enter_context(nc.sbuf_tensor(name, list(shape), dt))

        with contextlib.ExitStack() as pa:
            def sba(name, shape, dt):
                return pa.enter_context(nc.sbuf_tensor(name, list(shape), dt))
            wbf = sba("wbf", [128, 8, WCOLS], BF16)
            wst = [sba(f"wst{i}", [128, WCOLS // 2], F32) for i in range(2)]
            cos_sb = sba("cos_sb", [128, S_TOK], F32)
            sin_sb = sba("sin_sb", [128, S_TOK], F32)
            xst = [sba(f"xst{i}", [128, 8, 512], F32) for i in range(2)]
            xb = [sba(f"xb{i}", [128, 8, 512], BF16) for i in range(2)]
            t1 = [sba(f"t1_{i}", [128, 512], F32) for i in range(2)]
            t2 = [sba(f"t2_{i}", [128, 512], F32) for i in range(2)]
            ob = [sba(f"ob{i}", [128, 512], BF16) for i in range(4)]
            of = [sba(f"of{i}", [128, 512], F32) for i in range(2)]
            dts = sba("dts", [128, 512], F32)
            zpad = sba("zpad", [128, 8, 2], BF16)

            S.dma(lambda e: e.dma_start(out=cos_sb[:], in_=cosT), writes=["cos_sb"])
            S.dma(lambda e: e.dma_start(out=sin_sb[:], in_=sinT), writes=["sin_sb"])
            S.op("pool", lambda e: e.memset(zpad[:], 0.0), writes=["zpad"])
            xbc_rows = xbcT_d.rearrange("(c p) t -> p c t", p=128)
            S.dma(lambda e: e.dma_start(out=xbc_rows[:, :, 0:2], in_=zpad[:]), reads=["zpad"], writes=["xbcpadL"])
            S.dma(lambda e: e.dma_start(out=xbc_rows[:, :, S_TOK + 2:S_TOK + 4], in_=zpad[:]), reads=["zpad"], writes=["xbcpadR"])
            H = WCOLS // 2
            n = 0
            for hf in range(2):
                for kc in range(8):
                    st = wst[n % 2]
                    S.dma(lambda e, st=st, kc=kc, hf=hf: e.dma_start(out=st[:], in_=w_in[kc * 128:(kc + 1) * 128, hf * H:(hf + 1) * H]),
                          writes=[f"wst{n % 2}"])
                    if n % 2 == 0:
                        S.op("dve", lambda e, st=st, kc=kc, hf=hf: e.tensor_copy(out=wbf[:, kc, hf * H:(hf + 1) * H], in_=st[:]),
                             reads=[f"wst{n % 2}"], writes=[f"wbf{kc}_{hf}"])
                    else:
                        S.op("act", lambda e, st=st, kc=kc, hf=hf: e.copy(out=wbf[:, kc, hf * H:(hf + 1) * H], in_=st[:]),
                             reads=[f"wst{n % 2}"], writes=[f"wbf{kc}_{hf}"])
                    n += 1
            def wres_for(kc, c0, c1):
                return [f"wbf{kc}_{hf}" for hf in range(2) if c0 < (hf + 1) * H and c1 > hf * H]
            xT_r = xT.rearrange("(c p) t -> p c t", p=128)
            pb = 0
            obi = 0
            for ch in range(8):
                t0 = ch * 512
                bi = ch % 2
                S.dma(lambda e, bi=bi, t0=t0: e.dma_start(out=xst[bi][:], in_=xT_r[:, :, t0:t0 + 512]), writes=[f"xst{bi}"])
                S.op("dve", lambda e, bi=bi: e.tensor_copy(out=xb[bi][:], in_=xst[bi][:]), reads=[f"xst{bi}"], writes=[f"xb{bi}"])
                xres = f"xb{bi}"

                def fm_tile(j, bank, bi=bi):
                    for kc in range(8):
                        S.op("pe", lambda e, j=j, kc=kc, bank=bank: e.matmul(
                            psum[bank][:, :], lhsT=wbf[:, kc, j * 128:(j + 1) * 128], rhs=xb[bi][:, kc, :],
                            start=(kc == 0), stop=(kc == 7)), reads=[xres] + wres_for(kc, j * 128, (j + 1) * 128), writes=[f"ps{bank}"])
                for which, dst in ((0, qT_d), (8, kT_d)):
                    for j in range(4):
                        ba, bb = pb % 6, (pb + 1) % 6
                        pb += 2
                        fm_tile(which + j, ba)
                        fm_tile(which + 4 + j, bb)
                        ti = j % 2
                        S.op("dve", lambda e, ba=ba, ti=ti, t0=t0: e.tensor_tensor(
                            out=t1[ti][:], in0=psum[ba][:, :], in1=cos_sb[:, t0:t0 + 512], op=ALU.mult),
                            reads=[f"ps{ba}", "cos_sb"], writes=[f"t1_{ti}"])
                        S.op("dve", lambda e, bb=bb, ti=ti, t0=t0: e.tensor_tensor(
                            out=t2[ti][:], in0=psum[bb][:, :], in1=sin_sb[:, t0:t0 + 512], op=ALU.mult),
                            reads=[f"ps{bb}", "sin_sb"], writes=[f"t2_{ti}"])
                        o = obi % 4
                        obi += 1
                        S.op("pool", lambda e, ti=ti, o=o: e.tensor_tensor(
                            out=ob[o][:], in0=t1[ti][:], in1=t2[ti][:], op=ALU.add),
                            reads=[f"t1_{ti}", f"t2_{ti}"], writes=[f"ob{o}"])
                        S.dma(lambda e, o=o, j=j, dst=dst, t0=t0: e.dma_start(
                            out=dst[j * 128:(j + 1) * 128, t0:t0 + 512], in_=ob[o][:]), reads=[f"ob{o}"], writes=[])
                for j in range(8):
                    ba = pb % 6
                    pb += 1
                    fm_tile(16 + j, ba)
                    o = obi % 4
                    obi += 1
                    S.op("act", lambda e, ba=ba, o=o: e.copy(out=ob[o][:], in_=psum[ba][:, :]),
                         reads=[f"ps{ba}"], writes=[f"ob{o}"])
                    S.dma(lambda e, o=o, j=j, t0=t0: e.dma_start(
                        out=xbcT_d[j * 128:(j + 1) * 128, 2 + t0:2 + t0 + 512], in_=ob[o][:]), reads=[f"ob{o}"], writes=[])
                for tt in range(4):
                    tg = ch * 4 + tt
                    for which in range(2):
                        ba = pb % 6
                        pb += 1
                        c0 = 3072 + which * 512
                        for kc in range(8):
                            S.op("pe", lambda e, kc=kc, ba=ba, tt=tt, c0=c0, bi=bi: e.matmul(
                                psum[ba][:, :], lhsT=xb[bi][:, kc, tt * 128:(tt + 1) * 128], rhs=wbf[:, kc, c0:c0 + 512],
                                start=(kc == 0), stop=(kc == 7)), reads=[xres] + wres_for(kc, c0, c0 + 512), writes=[f"ps{ba}"])
                        if which == 0:
                            o = obi % 4
                            obi += 1
                            S.op("act", lambda e, ba=ba, o=o: e.copy(out=ob[o][:], in_=psum[ba][:, :]),
                                 reads=[f"ps{ba}"], writes=[f"ob{o}"])
                            S.dma(lambda e, o=o, tg=tg: e.dma_start(out=v_d[tg * 128:(tg + 1) * 128, :], in_=ob[o][:]),
                                  reads=[f"ob{o}"], writes=[])
                        else:
                            o = tg % 2
                            S.op("act", lambda e, ba=ba, o=o: e.copy(out=of[o][:], in_=psum[ba][:, :]),
                                 reads=[f"ps{ba}"], writes=[f"of{o}"])
                            S.dma(lambda e, o=o, tg=tg: e.dma_start(out=z_d[tg * 128:(tg + 1) * 128, :], in_=of[o][:]),
                                  reads=[f"of{o}"], writes=[])
                    for kc in range(8):
                        S.op("pe", lambda e, kc=kc, tt=tt, tg=tg, bi=bi: e.matmul(
                            psum[6][:, tg * 16:(tg + 1) * 16], lhsT=xb[bi][:, kc, tt * 128:(tt + 1) * 128],
                            rhs=wbf[:, kc, 4096:4112], start=(kc == 0), stop=(kc == 7)),
                            reads=[xres] + wres_for(kc, 4096, 4112), writes=["ps6"])
            S.op("act", lambda e: e.copy(out=dts[:], in_=psum[6][:, :]), reads=["ps6"], writes=["dts"])
            S.dma(lambda e: e.dma_start(out=dt_d, in_=dts[:]), reads=["dts"], writes=[])

            S.end_phase()

        C = dict(nc=nc, S=S, psum=psum, debug=debug, din=din, dscr=dscr)
        C.update(qT_d=qT_d, kT_d=kT_d, v_d=v_d, z_d=z_d, dt_d=dt_d, xbcT_d=xbcT_d, x=x)
        C["psT"] = psT
        phase_attn(C)
        phase_conv(C)
        phase_ssd(C)
        C["gates_all"] = es.enter_context(nc.sbuf_tensor("gates_all", [128, 32, 4], F32))
        C["dest_all"] = es.enter_context(nc.sbuf_tensor("dest_all", [128, 32, 4], I32))
        C["flags_all"] = es.enter_context(nc.sbuf_tensor("flags_all", [128, N_EXP * 8], I32))
        phase_mix(C)
        phase_experts(C)
        phase_combine(C)
        S.streams["sp"].append(("wait", "c_pe", S.cnt["pe"])) if False else None
        S.finish()
        S.emit()
    return nc


_CACHE = {}


def kernel(**inputs):
    debug = bool(inputs.pop("_debug", False))
    x = np.asarray(inputs["x"], dtype=np.float32)
    w_in = np.asarray(inputs["w_in"], dtype=np.float32)[0]
    perm = np.concatenate([np.concatenate([np.arange(32, 64), np.arange(0, 32)]) + 64 * h for h in range(8)])
    wq, wk, wv = w_in[:, 0:512], w_in[:, 512:1024], w_in[:, 1024:1536]
    wz, wxbc, wdt = w_in[:, 1536:2048], w_in[:, 2048:3072], w_in[:, 3072:3088]
    w_ext = np.ascontiguousarray(np.concatenate([wq, wq[:, perm], wk, wk[:, perm], wxbc, wv, wz, wdt], axis=1))
    consts = host_consts()
    g = lambda k: np.asarray(inputs[k], dtype=np.float32)[0]
    rep = lambda v: np.ascontiguousarray(np.broadcast_to(v[None, :], (128, v.shape[0])))
    consts["conv_wT"] = np.ascontiguousarray(g("conv_w").reshape(5, 8, 128).transpose(2, 1, 0).reshape(128, 40))
    consts["conv_bT"] = np.ascontiguousarray(g("conv_b").reshape(8, 128).T)
    consts["conv_b_row"] = np.ascontiguousarray(g("conv_b").reshape(1, 1024))
    consts["dtb_rep"] = rep(np.tile(np.concatenate([g("dt_bias_fwd"), g("dt_bias_bwd")]), 32))
    consts["alog_rep"] = rep(np.tile(np.concatenate([g("a_log_fwd"), g("a_log_bwd")]), 32))
    consts["dskip_rep"] = rep(np.repeat(g("d_skip"), 64))
    consts["ssdnw_rep"] = rep(g("ssd_norm_w"))
    consts["w_out"] = g("w_out")
    consts["anwT"] = np.ascontiguousarray(g("attn_norm_w").reshape(4, 128).T)
    consts["ln1w_rep"] = rep(g("ln1_w"))
    consts["ln1b_rep"] = rep(g("ln1_b"))
    consts["router_w"] = g("router_w")
    consts["rb_rep"] = rep(g("router_b"))
    consts["ecap_rep"] = rep((np.arange(N_EXP) * CAP).astype(np.float32))
    consts["w_gate"] = g("w_gate")
    consts["w_up"] = g("w_up")
    consts["w_down"] = g("w_down")
    consts["bgT"] = np.ascontiguousarray(g("b_gate").reshape(N_EXP, 8, 128).transpose(2, 0, 1).reshape(128, N_EXP * 8))
    consts["buT"] = np.ascontiguousarray(g("b_up").reshape(N_EXP, 8, 128).transpose(2, 0, 1).reshape(128, N_EXP * 8))
    consts["b_down"] = g("b_down")
    consts["ln2w_rep"] = rep(g("ln2_w"))
    consts["ln2b_rep"] = rep(g("ln2_b"))
    consts["c_SU2"] = consts["c_SU"]
    consts["c_ones2"] = consts["c_ones"]
    nc = build(debug=debug)
    in_maps = []
    for b in range(8):
        m = {"xT": np.ascontiguousarray(x[b].T), "x": np.ascontiguousarray(x[b]), "w_in_ext": w_ext}
        m.update(consts)
        in_maps.append(m)
    ncores = int(inputs.pop("_ncores", 8)) if "_ncores" in inputs else 8
    res = run_bass_kernel_spmd(nc, in_maps[:ncores], core_ids=list(range(ncores)))
    if debug:
        return res.results
    return np.stack([r["out"] for r in res.results], axis=0)
```

```python
import contextlib
import numpy as np
import ml_dtypes
import concourse.bass as bass
import concourse.mybir as mybir
from concourse.bass_utils import run_bass_kernel_spmd

F32 = mybir.dt.float32
BF16 = mybir.dt.bfloat16
U32 = mybir.dt.uint32
I32 = mybir.dt.int32
AF = mybir.ActivationFunctionType
ALU = mybir.AluOpType

S_TOK = 4096
D = 1024
NCH = 32
WCOLS = 2048 + 1040
N_EXP = 32
CAP = 1024


class Sched:
    COMPUTE = ("pe", "act", "dve", "pool")

    def __init__(self, nc, es, n_ring=12):
        self.nc = nc
        self.streams = {e: [] for e in ("pe", "act", "dve", "pool", "sp")}
        self.sem = {}
        for e in self.COMPUTE:
            self.sem[e] = es.enter_context(nc.semaphore("sem_" + e))
        self.cnt = {e: 0 for e in self.COMPUTE}
        self.ring = {}
        self.ring_i = {}
        self.ring_tot = {}
        for q, n in (("sp", n_ring), ("pool", 8), ("act", 6), ("dve", 6)):
            self.ring[q] = [es.enter_context(nc.semaphore(f"dq_{q}_{i}")) for i in range(n)]
            self.ring_i[q] = 0
        self.last_writer = {}
        self.readers = {}
        self.waited = {e: {} for e in self.streams}
        self.n_ins = 0

    def _deps(self, reads, writes):
        deps = {}

        def add(tok):
            k, v, e = tok
            if k not in deps or deps[k][0] < v:
                deps[k] = (v, e)
        for r in reads:
            if r in self.last_writer:
                add(self.last_writer[r])
        for w in writes:
            if w in self.last_writer:
                add(self.last_writer[w])
            for k, (v, e) in self.readers.get(w, {}).items():
                add((k, v, e))
        return deps

    def _emit_waits(self, eng, deps):
        for k, (v, src) in deps.items():
            if src == eng and eng == "pe":
                continue
            if self.waited[eng].get(k, 0) >= v:
                continue
            self.waited[eng][k] = v
            self.streams[eng].append(("wait", k, v))

    def _commit(self, tok, reads, writes):
        k, v, e = tok
        for w in writes:
            self.last_writer[w] = tok
            self.readers[w] = {}
        for r in reads:
            d = self.readers.setdefault(r, {})
            if k not in d or d[k][0] < v:
                d[k] = (v, e)

    def cond_begin(self, flag_ap):
        import copy
        self._cond = dict(n={e: 0 for e in self.streams}, rings={e: {} for e in self.streams},
                          snap=copy.deepcopy(self.waited))
        for e in self.streams:
            self.streams[e].append(("cond_begin", flag_ap))

    def cond_end(self):
        c = self._cond
        for e in self.streams:
            self.streams[e].append(("cond_end", c["n"][e], c["rings"][e]))
        self.waited = c["snap"]
        self._cond = None

    def op(self, eng, fn, reads=(), writes=()):
        deps = self._deps(reads, writes)
        self._emit_waits(eng, deps)
        if getattr(self, "_cond", None):
            self._cond["n"][eng] += 1
        self.cnt[eng] += 1
        key = "c_" + eng
        tok = (key, self.cnt[eng], eng)
        self.streams[eng].append(("ins", fn, self.sem[eng], 1))
        self._commit(tok, reads, writes)
        self.n_ins += 1

    def dma(self, fn, reads=(), writes=(), q="sp"):
        deps = self._deps(reads, writes)
        self._emit_waits(q, deps)
        i = self.ring_i[q]
        self.ring_i[q] = (i + 1) % len(self.ring[q])
        key = f"d_{q}_{i}"
        tot = self.ring_tot.get(key, 0)
        if tot > 0 and self.waited[q].get(key, 0) < tot:
            self.waited[q][key] = tot
            self.streams[q].append(("wait", key, tot))
        tot += 16
        self.ring_tot[key] = tot
        if getattr(self, "_cond", None):
            ent = self._cond["rings"][q].setdefault(key, [0, tot - 16])
            ent[0] += 1
        tok = (key, tot, "dma")
        self.streams[q].append(("ins", fn, self.ring[q][i], 16))
        self._commit(tok, reads, writes)
        self.n_ins += 1

    def _semh(self, key):
        if key.startswith("c_"):
            return self.sem[key[2:]]
        _, q, i = key.split("_")
        return self.ring[q][int(i)]

    def finish(self):
        for key, tot in self.ring_tot.items():
            if self.waited["sp"].get(key, 0) < tot:
                self.streams["sp"].append(("wait", key, tot))
                self.waited["sp"][key] = tot
        for e in self.COMPUTE:
            if self.cnt[e] and self.waited["sp"].get("c_" + e, 0) < self.cnt[e]:
                self.streams["sp"].append(("wait", "c_" + e, self.cnt[e]))

    def barrier(self):
        for eng in self.streams:
            for key, tot in self.ring_tot.items():
                if self.waited[eng].get(key, 0) < tot:
                    self.streams[eng].append(("wait", key, tot))
                    self.waited[eng][key] = tot
            for e in self.COMPUTE:
                if e != eng and self.cnt[e] and self.waited[eng].get("c_" + e, 0) < self.cnt[e]:
                    self.streams[eng].append(("wait", "c_" + e, self.cnt[e]))
                    self.waited[eng]["c_" + e] = self.cnt[e]

    def bc(self, e):
        if getattr(self, "_bc", None) is None:
            self._bc = e.to_reg(NROWS - 1)
        return self._bc

    def end_phase(self):
        self._bc_prev = getattr(self, "_bc", None)
        print("phase end n_ins", self.n_ins, {k: len(v) for k, v in self.streams.items()}, flush=True)
        self.barrier()
        self.emit()
        self._bc = None
        self.streams = {e: [] for e in self.streams}
        self.last_writer = {}
        self.readers = {}

    def emit(self):
        nc = self.nc
        with nc.Block() as block:
            def mk(name):
                def body(eng):
                    items = self.streams[name]
                    reg = None
                    guard = None
                    i = 0
                    while i < len(items):
                        it = items[i]
                        if it[0] == "wait":
                            eng.wait_ge(self._semh(it[1]), it[2])
                        elif it[0] == "ins":
                            it[1](eng).then_inc(it[2], it[3])
                        elif it[0] == "cond_begin":
                            j = i + 1
                            while items[j][0] != "cond_end":
                                j += 1
                            if j == i + 1:
                                i = j + 1
                                continue
                            if reg is None:
                                self._uid = getattr(self, "_uid", 0) + 1
                                reg = eng.alloc_register(f"cr_{name}_{self._uid}")
                            eng.reg_load(reg, it[1])
                            guard = eng.If_ne(reg, 0)
                            guard.__enter__()
                        elif it[0] == "cond_end":
                            guard.__exit__(None, None, None)
                            g2 = eng.Else()
                            g2.__enter__()
                            if it[1] and name in self.sem:
                                left = it[1]
                                while left > 0:
                                    k_ = min(16, left)
                                    eng.drain().then_inc(self.sem[name], k_)
                                    left -= k_
                            for key, (cnt_, before_) in it[2].items():
                                if before_ > 0:
                                    eng.wait_ge(self._semh(key), before_)
                                for _ in range(cnt_):
                                    eng.drain().then_inc(self._semh(key), 16)
                            g2.__exit__(None, None, None)
                            guard = None
                        i += 1
                return body
            if self.streams["sp"]:
                block.sync(mk("sp"))
            if self.streams["pe"]:
                block.tensor(mk("pe"))
            if self.streams["act"]:
                block.scalar(mk("act"))
            if self.streams["dve"]:
                block.vector(mk("dve"))
            if self.streams["pool"]:
                block.gpsimd(mk("pool"))


def host_consts():
    c = {}
    pos = np.arange(S_TOK, dtype=np.float32)
    inv = (np.float32(10000.0) ** (-np.arange(0, 64, 2, dtype=np.float32) / np.float32(64))).astype(np.float32)
    ang = (pos[:, None] * inv[None, :]).astype(np.float32)
    cos = np.cos(ang).astype(np.float32).T
    sin = np.sin(ang).astype(np.float32).T
    cosT = np.concatenate([cos, cos, cos, cos], 0)
    sinT = np.concatenate([-sin, sin, -sin, sin], 0)
    kk = np.arange(128)[:, None]
    nn = np.arange(256)[None, :]
    c["negmask"] = np.where((kk <= nn) & (nn <= kk + 128), 0.0, -30000.0).astype(ml_dtypes.bfloat16)
    c["ident_bf"] = np.eye(128, dtype=np.float32).astype(ml_dtypes.bfloat16)
    sel = np.zeros((65, 64), np.float32)
    sel[64, :] = 1.0
    c["sel65"] = sel
    t_ = np.arange(128)
    c["c_SL"] = (t_[:, None] > t_[None, :]).astype(np.float32)
    c["c_UI"] = (t_[:, None] <= t_[None, :]).astype(np.float32)
    c["c_SU"] = (t_[:, None] < t_[None, :]).astype(np.float32)
    c["c_LI"] = (t_[:, None] >= t_[None, :]).astype(np.float32)
    c["c_ones"] = np.ones((128, 128), np.float32)
    pm = np.zeros((128, 128), np.float32)
    for m in range(128):
        pm[(m // 64) * 64 + ((m % 64) + 32) % 64, m] = 1.0
    c["permT"] = pm.astype(ml_dtypes.bfloat16)
    c["cosT"] = np.ascontiguousarray(cosT)
    c["sinT"] = np.ascontiguousarray(sinT)
    return c


def phase_attn(C):
    nc, S, psum = C["nc"], C["S"], C["psum"]
    qT_d, kT_d, v_d = C["qT_d"], C["kT_d"], C["v_d"]
    negmask_d = C["din"]("negmask", [128, 256], BF16)
    ident_d = C["din"]("ident_bf", [128, 128], BF16)
    sel_d = C["din"]("sel65", [65, 64], F32)
    attnT_d = C["dscr"]("attnT_d", [512, S_TOK], BF16)
    C["attnT_d"] = attnT_d
    C["ident_d"] = ident_d
    with contextlib.ExitStack() as ph:
        def sba(name, shape, dt):
            return ph.enter_context(nc.sbuf_tensor(name, list(shape), dt))
        negmask = sba("negmask_sb", [128, 256], BF16)
        ident = sba("ident_sb", [128, 128], BF16)
        sel = sba("sel_sb", [65, 64], F32)
        V = [sba(f"V{i}", [128, 32, 4, 65], BF16) for i in range(3)]
        vst = [sba(f"vst{i}", [128, 8, 256], BF16) for i in range(2)]
        acc = [sba(f"acc{i}", [128, S_TOK], F32) for i in range(2)]
        qh = [sba(f"qh{i}", [128, S_TOK], BF16) for i in range(2)]
        kh = [sba(f"kh{i}", [128, S_TOK], BF16) for i in range(2)]
        pT = [sba(f"pT{i}", [128, 256], BF16) for i in range(4)]
        rec = [sba(f"rec{i}", [64, 512], F32) for i in range(2)]
        outb = [sba(f"aob{i}", [64, 512], BF16) for i in range(2)]
        S.dma(lambda e: e.dma_start(out=negmask[:], in_=negmask_d), writes=["negmask"])
        S.dma(lambda e: e.dma_start(out=ident[:], in_=ident_d), writes=["ident"])
        S.dma(lambda e: e.dma_start(out=sel[:], in_=sel_d), writes=["sel"])
        for i in range(3):
            S.op("pool", lambda e, i=i: e.memset(V[i][:], 1.0), writes=[f"V{i}"])
        PAT = (1, 4, 16)
        scb = [psum[0], psum[1], C["psT"][:, :].bitcast(F32)]
        out_banks = (2, 3, 4)
        cnt = dict(vs=0, sc=0, pt=0, ob=0, nb=0)

        for i in range(2):
            S.op("pool", lambda e, i=i: e.memset(kh[i][:], 0.0), writes=[f"kh{i}"])

        def load_qk(h):
            b = h % 2
            if h % 2 == 0:
                p = h // 2
                S.dma(lambda e: e.dma_start(out=qh[p % 2][:], in_=qT_d[p * 128:(p + 1) * 128, :]), writes=[f"qh{p % 2}"])
            S.dma(lambda e: e.dma_start(out=kh[b][b * 64:(b + 1) * 64, :], in_=kT_d[h * 64:(h + 1) * 64, :]), writes=[f"kh{b}"])

        def load_V(hh):
            for pi, r in enumerate(PAT):
                n_t = 32 // r
                view4 = v_d.rearrange("(u k r) c -> k r u c", r=r, k=128)
                for gi in range(4):
                    b = cnt["vs"] % 2
                    cnt["vs"] += 1
                    cs = slice(hh * 256, (hh + 1) * 256)
                    if r == 1:
                        src = view4[:, 0, 8 * gi:8 * gi + 8, cs]
                        dst = vst[b][:]
                    elif r == 4:
                        src = view4[:, gi, :, cs]
                        dst = vst[b][:]
                    else:
                        src = None
                        for j in range(4):
                            srcj = view4[:, 4 * gi + j, :, cs]
                            dstj = vst[b][:, 2 * j:2 * j + 2, :]
                            S.dma(lambda e, src=srcj, dst=dstj: e.dma_start(out=dst, in_=src), writes=[f"vst{b}"])
                    if src is not None:
                        S.dma(lambda e, src=src, dst=dst: e.dma_start(out=dst, in_=src), writes=[f"vst{b}"])
                    eng = ("pool", "dve")[gi % 2]
                    S.op(eng, lambda e, pi=pi, gi=gi, b=b: e.tensor_copy(
                        out=V[pi][:, 8 * gi:8 * gi + 8, :, 0:64],
                        in_=vst[b][:].rearrange("p t (h d) -> p t h d", h=4)),
                        reads=[f"vst{b}"], writes=[f"V{pi}"])

        def head(h):
            hb = h % 2
            hl = h % 4
            ab = h % 2
            accr = f"acc{ab}"
            tiles = []
            for pi, r in enumerate(PAT):
                L = S_TOK // r
                n_t = L // 128
                for rho in range(r):
                    nbanks = (L + 64 + 511) // 512
                    base = cnt["nb"]
                    cnt["nb"] += nbanks
                    for u in range(n_t):
                        tiles.append((pi, r, rho, u, n_t, L, base, nbanks))

            def score(tl):
                pi, r, rho, u, n_t, L, base, nbanks = tl
                m_lo = max(0, 128 * u - 64)
                m_hi = min(L, 128 * u + 192)
                N = m_hi - m_lo
                c0 = m_lo - (128 * u - 64)
                sl_ = cnt["sc"] % 3
                cnt["sc"] += 1
                sbk, so = sl_, 0
                pb_ = cnt["pt"] % 4
                cnt["pt"] += 1
                k0 = rho + r * 128 * u
                kc = kh[hb][:, k0:k0 + r * 127 + 1:r]
                qb_ = (h // 2) % 2
                qc = qh[qb_][:, rho + r * m_lo:rho + r * (m_hi - 1) + 1:r]
                S.op("pe", lambda e: e.matmul(scb[sbk][:, so:so + N], lhsT=kc, rhs=qc, start=True, stop=False),
                     reads=[f"qh{qb_}", f"kh{hb}"], writes=[f"sc{sl_}"])
                S.op("pe", lambda e: e.matmul(scb[sbk][:, so:so + N], lhsT=ident[:], rhs=negmask[:, c0:c0 + N],
                                               start=False, stop=True),
                     reads=["ident", "negmask"], writes=[f"sc{sl_}"])
                S.op("act", lambda e: e.activation(out=pT[pb_][:, 0:N], in_=scb[sbk][:, so:so + N], func=AF.Exp, scale=0.125),
                     reads=[f"sc{sl_}"], writes=[f"pT{pb_}"])
                return (pb_, m_lo, m_hi)

            def pv(tl, info):
                pi, r, rho, u, n_t, L, base, nbanks = tl
                pb_, m_lo, m_hi = info

                def phys(gb):
                    return out_banks[(base + gb) % 3]
                ti = rho * n_t + u
                vl = V[pi][:, ti, hl, :]
                nA = (128 * u + 64) - m_lo
                cA = m_lo + 64
                gA, colA = cA // 512, cA % 512
                pA = phys(gA)
                S.op("pe", lambda e: e.matmul(psum[pA][0:65, colA:colA + nA], lhsT=vl, rhs=pT[pb_][:, 0:nA],
                                               start=(u == 0), stop=True),
                     reads=[f"V{pi}", f"pT{pb_}"], writes=[f"ps{pA}"])
                nB = m_hi - (128 * u + 64)
                cB = 128 * (u + 1)
                gB, colB = cB // 512, cB % 512
                pB = phys(gB)
                lastu = (u == n_t - 1)
                S.op("pe", lambda e: e.matmul(psum[pB][0:65, colB:colB + nB], lhsT=vl, rhs=pT[pb_][:, nA:nA + nB],
                                               start=True, stop=lastu),
                     reads=[f"V{pi}", f"pT{pb_}"], writes=[f"ps{pB}"])
                for gb in range(nbanks):
                    if min(4 * gb + 3, n_t - 1) == u:
                        ma = max(0, 512 * gb - 64)
                        mb = min(L, 512 * gb + 448)
                        ca = ma + 64 - 512 * gb
                        n = mb - ma
                        asl = acc[ab][0:65, rho + r * ma:rho + r * (mb - 1) + 1:r]
                        pg_ = phys(gb)
                        psl = psum[pg_][0:65, ca:ca + n]
                        if pi == 0:
                            S.op("dve", lambda e, asl=asl, psl=psl: e.tensor_copy(out=asl, in_=psl),
                                 reads=[f"ps{pg_}"], writes=[accr])
                        else:
                            S.op("dve", lambda e, asl=asl, psl=psl: e.tensor_tensor(out=asl, in0=asl, in1=psl, op=ALU.add),
                                 reads=[f"ps{pg_}", accr], writes=[accr])

            LA = 2
            infos = []
            for k_ in range(min(LA, len(tiles))):
                infos.append(score(tiles[k_]))
            for k_ in range(len(tiles)):
                if k_ + LA < len(tiles):
                    infos.append(score(tiles[k_ + LA]))
                pv(tiles[k_], infos[k_])
            for c in range(8):
                db = 5 + (c % 2)
                rb = c % 2
                S.op("pe", lambda e, c=c, db=db: e.matmul(psum[db][0:64, :], lhsT=sel[:], rhs=acc[ab][0:65, c * 512:(c + 1) * 512],
                                                           start=True, stop=True),
                     reads=[accr, "sel"], writes=[f"ps{db}"])
                S.op("dve", lambda e, db=db, rb=rb: e.reciprocal(out=rec[rb][:], in_=psum[db][0:64, :]),
                     reads=[f"ps{db}"], writes=[f"rec{rb}"])
                S.op("pool", lambda e, c=c, rb=rb: e.tensor_tensor(out=outb[rb][:], in0=acc[ab][0:64, c * 512:(c + 1) * 512],
                                                                     in1=rec[rb][:], op=ALU.mult),
                     reads=[accr, f"rec{rb}"], writes=[f"aob{rb}"])
                S.dma(lambda e, c=c, rb=rb: e.dma_start(out=attnT_d[h * 64:(h + 1) * 64, c * 512:(c + 1) * 512], in_=outb[rb][:]),
                      reads=[f"aob{rb}"], writes=[])

        load_qk(0)
        for hh in range(2):
            load_V(hh)
            for hl_ in range(4):
                h = hh * 4 + hl_
                if h + 1 < 8:
                    load_qk(h + 1)
                head(h)
        S.end_phase()


def X(S, eng, meth, reads, writes, *a, **kw):
    S.op(eng, lambda e: getattr(e, meth)(*a, **kw), reads, writes)


def DM(S, out, in_, reads=(), writes=(), q="sp"):
    S.dma(lambda e: e.dma_start(out=out, in_=in_), reads, writes, q)


def phase_conv(C):
    nc, S, psum = C["nc"], C["S"], C["psum"]
    cwT_d = C["din"]("conv_wT", [128, 40])
    cbT_d = C["din"]("conv_bT", [128, 8])
    cbrow_d = C["din"]("conv_b_row", [1, 1024])
    bcT_d = C["dscr"]("bcT_d", [512, S_TOK], BF16)
    xsB_d = C["dscr"]("xsB_d", [S_TOK, 768], BF16)
    C["bcT_d"], C["xsB_d"] = bcT_d, xsB_d
    with contextlib.ExitStack() as ph:
        def sba(name, shape, dt):
            return ph.enter_context(nc.sbuf_tensor(name, list(shape), dt))
        xpre = sba("xpre", [128, 8, S_TOK + 4], BF16)
        dg = sba("dg", [128, 40, 128], BF16)
        ident = sba("ident_c", [128, 128], BF16)
        cwT = sba("cwT", [128, 40], F32)
        cbT = sba("cbT", [128, 8], F32)
        cbrow = sba("cbrow", [1, 1024], F32)
        cbrow_bf = sba("cbrow_bf", [1, 1024], BF16)
        ones1 = sba("ones1", [1, 128], BF16)
        ob = [sba(f"cob{i}", [128, 512], BF16) for i in range(3)]
        DM(S, ident[:], C["ident_d"], writes=["ident_c"])
        DM(S, cwT[:], cwT_d, writes=["cwT"])
        DM(S, cbT[:], cbT_d, writes=["cbT"])
        DM(S, cbrow[:], cbrow_d, writes=["cbrow"])
        xv = C["xbcT_d"].rearrange("(c p) t -> p c t", p=128)
        for j in range(8):
            DM(S, xpre[:, j, :], xv[:, j, :], writes=[f"xpre{j}"])
        X(S, "dve", "tensor_copy", ["cbrow"], ["cbrow_bf"], out=cbrow_bf[:], in_=cbrow[:])
        X(S, "pool", "memset", [], ["ones1"], ones1[:], 1.0)
        for jk in range(40):
            X(S, "dve", "tensor_scalar", ["ident_c", "cwT"], ["dg"], out=dg[:, jk, :], in0=ident[:],
              scalar1=cwT[:, jk:jk + 1], scalar2=None, op0=ALU.mult)
        bi = 0
        oi = 0
        for j in range(4, 8):
            for tc in range(8):
                b = bi % 2
                bi += 1
                for k in range(5):
                    X(S, "pe", "matmul", [f"xpre{j}", "dg"], [f"ps{b}"], psum[b][:, :], lhsT=dg[:, j * 5 + k, :],
                      rhs=xpre[:, j, tc * 512 + k:tc * 512 + k + 512], start=(k == 0), stop=(k == 4))
                o = oi % 3
                oi += 1
                X(S, "act", "activation", [f"ps{b}", "cbT"], [f"cob{o}"], out=ob[o][:], in_=psum[b][:, :], func=AF.Silu,
                  bias=cbT[:, j:j + 1])
                DM(S, bcT_d[(j - 4) * 128:(j - 3) * 128, tc * 512:(tc + 1) * 512], ob[o][:], reads=[f"cob{o}"])
        sz_d = C["dscr"]("sz_d", [S_TOK, 512], F32)
        C["sz_d"] = sz_d
        zin = [sba(f"zin{i}", [128, 512], F32) for i in range(2)]
        zou = [sba(f"zou{i}", [128, 512], F32) for i in range(2)]
        for tg in range(32):
            zb_ = tg % 2
            DM(S, zin[zb_][:], C["z_d"][tg * 128:(tg + 1) * 128, :], writes=[f"zin{zb_}"])
            X(S, "act", "activation", [f"zin{zb_}"], [f"zou{zb_}"], out=zou[zb_][:], in_=zin[zb_][:], func=AF.Silu)
            DM(S, sz_d[tg * 128:(tg + 1) * 128, :], zou[zb_][:], reads=[f"zou{zb_}"])
            bA = 2 + (tg % 2) * 2
            bB = bA + 1
            for j in range(6):
                bank = bA if j < 4 else bB
                col = (j % 4) * 128
                for k in range(5):
                    X(S, "pe", "matmul", [f"xpre{j}", "dg"], [f"ps{bank}"], psum[bank][:, col:col + 128],
                      lhsT=xpre[:, j, tg * 128 + k:tg * 128 + k + 128], rhs=dg[:, j * 5 + k, :], start=(k == 0), stop=False)
                X(S, "pe", "matmul", ["ones1", "cbrow_bf"], [f"ps{bank}"], psum[bank][:, col:col + 128],
                  lhsT=ones1[0:1, :], rhs=cbrow_bf[0:1, j * 128:(j + 1) * 128], start=False, stop=True)
            o = oi % 3
            oi += 1
            X(S, "act", "activation", [f"ps{bA}"], [f"cob{o}"], out=ob[o][:], in_=psum[bA][:, :], func=AF.Silu)
            DM(S, xsB_d[tg * 128:(tg + 1) * 128, 0:512], ob[o][:], reads=[f"cob{o}"])
            o = oi % 3
            oi += 1
            X(S, "act", "activation", [f"ps{bB}"], [f"cob{o}"], out=ob[o][:, 0:256], in_=psum[bB][:, 0:256], func=AF.Silu)
            DM(S, xsB_d[tg * 128:(tg + 1) * 128, 512:768], ob[o][:, 0:256], reads=[f"cob{o}"])
        S.end_phase()


def phase_ssd(C):
    nc, S, psum = C["nc"], C["S"], C["psum"]
    psT = C["psT"]
    din = C["din"]
    cst = {k: din(k, [128, 128]) for k in ("c_SL", "c_UI", "c_SU", "c_LI", "c_ones")}
    dtb_d = din("dtb_rep", [128, 512])
    alog_d = din("alog_rep", [128, 512])
    dskip_d = din("dskip_rep", [128, 512])
    nw_d = din("ssdnw_rep", [128, 512])
    ssdT_d = C["dscr"]("ssdT_d", [512, S_TOK], BF16)
    C["ssdT_d"] = ssdT_d
    xsB_d, bcT_d, z_d, dt_d = C["xsB_d"], C["bcT_d"], C["sz_d"], C["dt_d"]
    with contextlib.ExitStack() as ph:
        def sba(name, shape, dt):
            return ph.enter_context(nc.sbuf_tensor(name, list(shape), dt))
        K = {k: sba("k_" + k, [128, 128], F32) for k in cst}
        for k in cst:
            DM(S, K[k][:], cst[k], writes=[k])
        ident = sba("ident_s", [128, 128], BF16)
        DM(S, ident[:], C["ident_d"], writes=["ident_s"])
        G = {}
        for nm in ("dtr", "dtb", "alog", "dskip", "nw", "u", "au", "dt", "eA", "a", "tot", "acs", "cd", "d1", "d2",
                   "sarg", "oarg", "sdec", "odec", "wdt"):
            G[nm] = sba("g_" + nm, [128, 512], F32)
        DM(S, G["dtr"][:], dt_d, writes=["dtr"])
        DM(S, G["dtb"][:], dtb_d, writes=["dtb"])
        DM(S, G["alog"][:], alog_d, writes=["alog"])
        DM(S, G["dskip"][:], dskip_d, writes=["dskip"])
        DM(S, G["nw"][:], nw_d, writes=["nw"])

        def g(nm):
            return G[nm][:]

        def g3(nm):
            return G[nm][:].rearrange("p (c h) -> p c h", h=16)
        X(S, "dve", "tensor_tensor", ["dtr", "dtb"], ["u"], out=g("u"), in0=g("dtr"), in1=g("dtb"), op=ALU.add)
        X(S, "act", "activation", ["u"], ["au"], out=g("au"), in_=g("u"), func=AF.Exp)
        X(S, "act", "activation", ["au"], ["dt"], out=g("dt"), in_=g("au"), func=AF.Ln, bias=1.0)
        X(S, "act", "activation", ["alog"], ["eA"], out=g("eA"), in_=g("alog"), func=AF.Exp)
        X(S, "dve", "scalar_tensor_tensor", ["dt", "eA"], ["a"], out=g("a"), in0=g("dt"), scalar=-1.0, in1=g("eA"),
          op0=ALU.mult, op1=ALU.mult)
        X(S, "pe", "matmul", ["c_ones", "a"], ["ps0"], psum[0][:, :], lhsT=K["c_ones"][:], rhs=g("a"), start=True, stop=True)
        X(S, "pe", "matmul", ["c_UI", "a"], ["ps1"], psum[1][:, :], lhsT=K["c_UI"][:], rhs=g("a"), start=True, stop=True)
        X(S, "act", "copy", ["ps0"], ["tot"], out=g("tot"), in_=psum[0][:, :])
        X(S, "act", "copy", ["ps1"], ["acs"], out=g("acs"), in_=psum[1][:, :])
        X(S, "act", "activation", ["tot"], ["cd"], out=g("cd"), in_=g("tot"), func=AF.Exp)
        X(S, "dve", "tensor_tensor", ["tot", "acs"], ["d1"], out=g("d1"), in0=g("tot"), in1=g("acs"), op=ALU.subtract)
        X(S, "dve", "tensor_tensor", ["acs", "a"], ["d2"], out=g("d2"), in0=g("acs"), in1=g("a"), op=ALU.subtract)
        X(S, "pool", "tensor_copy", ["d1"], ["sarg"], out=g3("sarg")[:, :, 0:8], in_=g3("d1")[:, :, 0:8])
        X(S, "pool", "tensor_copy", ["d2"], ["sarg"], out=g3("sarg")[:, :, 8:16], in_=g3("d2")[:, :, 8:16])
        X(S, "pool", "tensor_copy", ["acs"], ["oarg"], out=g3("oarg")[:, :, 0:8], in_=g3("acs")[:, :, 0:8])
        X(S, "dve", "tensor_tensor", ["d1", "a"], ["oarg"], out=g3("oarg")[:, :, 8:16], in0=g3("d1")[:, :, 8:16],
          in1=g3("a")[:, :, 8:16], op=ALU.add)
        X(S, "act", "activation", ["sarg"], ["sdec"], out=g("sdec"), in_=g("sarg"), func=AF.Exp)
        X(S, "act", "activation", ["oarg"], ["odec"], out=g("odec"), in_=g("oarg"), func=AF.Exp)
        X(S, "dve", "tensor_tensor", ["dt", "sdec"], ["wdt"], out=g("wdt"), in0=g("dt"), in1=g("sdec"), op=ALU.mult)

        Fs = [[sba(f"Fs{d}_{k}", [128, 512], F32) for k in range(2)] for d in range(2)]
        Fstore = [sba(f"Fstore{d}", [128, 32, 512], BF16) for d in range(2)]
        xsB = [sba(f"xsB{i}", [128, 768], BF16) for i in range(4)]
        xsd = [sba(f"xsd{i}", [128, 512], BF16) for i in range(4)]
        for d in range(2):
            X(S, "pool", "memset", [], [f"Fs{d}_0"], Fs[d][0][:], 0.0)

        def p1_load(i_):
            for d in range(2):
                c = i_ if d == 0 else 31 - i_
                b = (i_ % 2) * 2 + d
                DM(S, xsB[b][:], xsB_d[c * 128:(c + 1) * 128, :], writes=[f"xsB{b}"])
        p1_load(0)
        for i_ in range(32):
            if i_ + 1 < 32:
                p1_load(i_ + 1)
            for d in range(2):
                c = i_ if d == 0 else 31 - i_
                b = (i_ % 2) * 2 + d
                cur, nxt = Fs[d][i_ % 2], Fs[d][(i_ + 1) % 2]
                rc, rn = f"Fs{d}_{i_ % 2}", f"Fs{d}_{(i_ + 1) % 2}"
                wv = g3("wdt")[:, c, d * 8:d * 8 + 8].unsqueeze(2).to_broadcast([128, 8, 64])
                X(S, "pool" if d == 0 else "dve", "tensor_tensor", [f"xsB{b}", "wdt"], [f"xsd{b}"],
                  out=xsd[b][:].rearrange("p (h d) -> p h d", h=8), in0=xsB[b][:, 0:512].rearrange("p (h d) -> p h d", h=8),
                  in1=wv, op=ALU.mult)
                pb = 1 + d
                for gq in range(2):
                    X(S, "pe", "matmul", [f"xsB{b}", f"xsd{b}"], [f"ps{pb}"], psum[pb][:, gq * 256:(gq + 1) * 256],
                      lhsT=xsB[b][:, 512 + gq * 128:512 + (gq + 1) * 128], rhs=xsd[b][:, gq * 256:(gq + 1) * 256],
                      start=True, stop=True)
                X(S, "act", "copy", [rc], [f"Fst{d}_{c}"], out=Fstore[d][:, c, :], in_=cur[:])
                cv = g3("cd")[:, c, d * 8:d * 8 + 8].unsqueeze(2).to_broadcast([128, 8, 64])
                X(S, "dve", "tensor_tensor", [rc, "cd"], [rn], out=nxt[:].rearrange("p (h d) -> p h d", h=8),
                  in0=cur[:].rearrange("p (h d) -> p h d", h=8), in1=cv, op=ALU.mult)
                X(S, "dve", "tensor_tensor", [rn, f"ps{pb}"], [rn], out=nxt[:], in0=nxt[:], in1=psum[pb][:, :], op=ALU.add)

        bc = [sba(f"bc{i}", [128, 4, 128], BF16) for i in range(3)]
        zt = [sba(f"zt{i}", [128, 512], F32) for i in range(3)]
        xdt = [sba(f"xdt{i}", [128, 512], BF16) for i in range(2)]
        Gm = [sba(f"Gm{i}", [128, 2, 128], BF16) for i in range(2)]
        lhs4 = [sba(f"lhs4_{i}", [128, 4, 128], F32) for i in range(2)]
        E4 = [sba(f"E4_{i}", [128, 4, 128], BF16) for i in range(2)]
        M4 = [sba(f"M4_{i}", [128, 4, 128], BF16) for i in range(2)]
        tA = sba("tA", [128, 512], F32)
        tB = sba("tB", [128, 512], F32)
        tU = sba("tU", [128, 512], F32)
        ez = sba("ez", [128, 512], F32)
        junk = sba("junk", [128, 256], F32)
        ss = sba("ss", [128, 2], F32)
        yb = sba("yb", [128, 512], BF16)
        yT = [sba(f"yT{i}", [128, 4, 128], BF16) for i in range(3)]
        bcv = bcT_d.rearrange("(q p) t -> p q t", p=128)
        ssdv = ssdT_d.rearrange("(q p) t -> p q t", p=128)
        qs = dict(qi=0)
        def p2_loads(c):
            b = c % 3
            DM(S, xsB[b][:], xsB_d[c * 128:(c + 1) * 128, :], writes=[f"xsB{b}"])
            DM(S, bc[b][:], bcv[:, :, c * 128:(c + 1) * 128], writes=[f"bc{b}"])
            DM(S, zt[b][:], z_d[c * 128:(c + 1) * 128, :], writes=[f"zt{b}"])
        def p2_front(c):
            b = c % 3
            xs3 = xsB[b][:, 0:512].rearrange("p (h d) -> p h d", h=8)
            for d in range(2):
                dv = g3("dt")[:, c, d * 8:d * 8 + 8].unsqueeze(2).to_broadcast([128, 8, 64])
                X(S, "dve", "tensor_tensor", [f"xsB{b}", "dt"], [f"xdt{d}"], out=xdt[d][:].rearrange("p (h d) -> p h d", h=8),
                  in0=xs3, in1=dv, op=ALU.mult)
            for gq in range(2):
                X(S, "pe", "matmul", [f"bc{b}"], ["ps0"], psum[0][:, gq * 128:(gq + 1) * 128], lhsT=bc[b][:, gq, :],
                  rhs=bc[b][:, 2 + gq, :], start=True, stop=True)
            p0v = psum[0][:, 0:256].rearrange("p (g l) -> p g l", g=2)
            X(S, "dve", "tensor_tensor", ["ps0", "c_UI"], ["Gm0"], out=Gm[0][:], in0=p0v,
              in1=K["c_UI"][:].unsqueeze(1).to_broadcast([128, 2, 128]), op=ALU.mult)
            X(S, "dve", "tensor_tensor", ["ps0", "c_LI"], ["Gm1"], out=Gm[1][:], in0=p0v,
              in1=K["c_LI"][:].unsqueeze(1).to_broadcast([128, 2, 128]), op=ALU.mult)
            quads = []
            for d in range(2):
                for gq in range(2):
                    q = qs["qi"] % 2
                    qs["qi"] += 1
                    quads.append((d, gq, q))

            def stA(d, gq, q):
                sb_ = 1 + q
                h0 = c * 16 + d * 8 + gq * 4
                av = G["a"][:, h0:h0 + 4].unsqueeze(2).to_broadcast([128, 4, 128])
                tri = K["c_SL" if d == 0 else "c_SU"][:].unsqueeze(1).to_broadcast([128, 4, 128])
                X(S, "dve", "tensor_tensor", ["a", "c_SL", "c_SU"], [f"lhs4_{q}"], out=lhs4[q][:], in0=tri, in1=av, op=ALU.mult)
                rk = K["c_UI" if d == 0 else "c_LI"]
                for i in range(4):
                    X(S, "pe", "matmul", [f"lhs4_{q}", "c_UI", "c_LI"], [f"ps{sb_}"], psum[sb_][:, i * 128:(i + 1) * 128],
                      lhsT=lhs4[q][:, i, :], rhs=rk[:], start=True, stop=True)
                X(S, "act", "activation", [f"ps{sb_}"], [f"E4_{q}"], out=E4[q][:].rearrange("p a b -> p (a b)"),
                  in_=psum[sb_][:, :], func=AF.Exp)
                X(S, "pool", "tensor_tensor", [f"E4_{q}", f"Gm{d}"], [f"M4_{q}"], out=M4[q][:], in0=E4[q][:],
                  in1=Gm[d][:, gq, :].unsqueeze(1).to_broadcast([128, 4, 128]), op=ALU.mult)

            def stB(d, gq, q):
                yb_ = 3 + d
                for i in range(4):
                    h = gq * 4 + i
                    X(S, "pe", "matmul", [f"M4_{q}", f"xdt{d}"], [f"ps{yb_}"], psum[yb_][:, h * 64:(h + 1) * 64],
                      lhsT=M4[q][:, i, :], rhs=xdt[d][:, h * 64:(h + 1) * 64], start=True, stop=True)
                X(S, "pe", "matmul", [f"bc{b}", f"Fst{d}_{c}"], [f"ps{5 + d}"], psum[5 + d][:, gq * 256:(gq + 1) * 256],
                  lhsT=bc[b][:, 2 + gq, :], rhs=Fstore[d][:, c, gq * 256:(gq + 1) * 256], start=True, stop=True)

            stA(*quads[0])
            stA(*quads[1])
            stB(*quads[0])
            stA(*quads[2])
            stB(*quads[1])
            stA(*quads[3])
            stB(*quads[2])
            stB(*quads[3])

        def p2_epi1(c):
            b = c % 3
            t3 = lambda t: t[:].rearrange("p (h d) -> p h d", h=8)
            for d, tt in ((0, tA), (1, tB)):
                ov = g3("odec")[:, c, d * 8:d * 8 + 8].unsqueeze(2).to_broadcast([128, 8, 64])
                X(S, "dve", "tensor_tensor", [f"ps{5 + d}", "odec"], [tt.name], out=t3(tt),
                  in0=psum[5 + d][:, :].rearrange("p (h d) -> p h d", h=8), in1=ov, op=ALU.mult)
                X(S, "dve", "tensor_tensor", [f"ps{3 + d}", tt.name], [tt.name], out=tt[:], in0=tt[:], in1=psum[3 + d][:, :], op=ALU.add)

        def p2_epi2(c):
            b = c % 3
            X(S, "pool", "tensor_tensor", ["tA", "tB"], ["tA"], out=tA[:], in0=tA[:], in1=tB[:], op=ALU.add)
            X(S, "pool", "tensor_tensor", [f"xsB{b}", "dskip"], ["tU"], out=tU[:], in0=xsB[b][:, 0:512], in1=g("dskip"), op=ALU.mult)
            X(S, "pool", "tensor_tensor", ["tA", "tU"], ["tA"], out=tA[:], in0=tA[:], in1=tU[:], op=ALU.add)
            X(S, "dve", "tensor_tensor", ["tA", f"zt{b}"], ["tA"], out=tA[:], in0=tA[:], in1=zt[b][:], op=ALU.mult)
            X(S, "dve", "tensor_tensor", ["tA"], ["tU"], out=tU[:], in0=tA[:], in1=tA[:], op=ALU.mult)
            X(S, "dve", "tensor_reduce", ["tU"], ["ss"], out=ss[:], in_=tU[:].rearrange("p (g f) -> p g f", g=2),
              axis=mybir.AxisListType.X, op=ALU.add)
            X(S, "dve", "tensor_scalar", ["ss"], ["ss"], out=ss[:], in0=ss[:], scalar1=1.0 / 256.0, scalar2=1e-5, op0=ALU.mult, op1=ALU.add)
            X(S, "act", "activation", ["ss"], ["ss"], out=ss[:], in_=ss[:], func=AF.Ln)
            X(S, "act", "activation", ["ss"], ["ss"], out=ss[:], in_=ss[:], func=AF.Exp, scale=-0.5)
            for gq in range(2):
                X(S, "dve", "scalar_tensor_tensor", ["tA", "ss", "nw"], ["yb"], out=yb[:, gq * 256:(gq + 1) * 256],
                  in0=tA[:, gq * 256:(gq + 1) * 256], scalar=ss[:, gq:gq + 1], in1=G["nw"][:, gq * 256:(gq + 1) * 256],
                  op0=ALU.mult, op1=ALU.mult)
            for q4 in range(4):
                X(S, "pe", "transpose", ["yb", "ident_s"], ["psT"], psT[:, q4 * 128:(q4 + 1) * 128], in_=yb[:, q4 * 128:(q4 + 1) * 128],
                  identity=ident[:])
            X(S, "act", "copy", ["psT"], [f"yT{b}"], out=yT[b][:].rearrange("p a b -> p (a b)"), in_=psT[:, 0:512])
            DM(S, ssdv[:, :, c * 128:(c + 1) * 128], yT[b][:], reads=[f"yT{b}"])

        p2_loads(0)
        p2_loads(1)
        p2_front(0)
        for c in range(32):
            if c + 2 < 32:
                p2_loads(c + 2)
            p2_epi1(c)
            if c + 1 < 32:
                p2_front(c + 1)
            p2_epi2(c)
        S.end_phase()


ALPHA = float((2.0 * 1) ** 0.25)
NROWS = N_EXP * CAP


def phase_mix(C):
    nc, S, psum, psT = C["nc"], C["S"], C["psum"], C["psT"]
    din, dscr = C["din"], C["dscr"]
    wout_d = din("w_out", [D, D])
    anw_d = din("anwT", [128, 4])
    l1w_d = din("ln1w_rep", [128, D])
    l1b_d = din("ln1b_rep", [128, D])
    rw_d = din("router_w", [D, N_EXP])
    rb_d = din("rb_rep", [128, N_EXP])
    ecap_d = din("ecap_rep", [128, N_EXP])
    su_d = din("c_SU2", [128, 128])
    ones_d = din("c_ones2", [128, 128])
    h1_d = dscr("h1_d", [S_TOK, D], F32)
    Xg_d = dscr("Xg_d", [NROWS, D], BF16)
    C["h1_d"], C["Xg_d"] = h1_d, Xg_d
    gates_all, dest_all = C["gates_all"], C["dest_all"]
    attnT_d, ssdT_d, x = C["attnT_d"], C["ssdT_d"], C["x"]
    with contextlib.ExitStack() as ph:
        def sba(name, shape, dt):
            return ph.enter_context(nc.sbuf_tensor(name, list(shape), dt))
        wo = sba("wo", [128, 8, D], BF16)
        wst = [sba(f"wost{i}", [128, 2, D], F32) for i in range(2)]
        anw = sba("anw", [128, 4], F32)
        l1w = sba("l1w", [128, D], F32)
        l1b = sba("l1b", [128, D], F32)
        rwf = sba("rwf", [128, 8, N_EXP], F32)
        rwb = sba("rwb", [128, 8, N_EXP], BF16)
        rb = sba("rb", [128, N_EXP], F32)
        base = sba("base", [128, N_EXP], F32)
        SU = sba("SU2", [128, 128], F32)
        ones = sba("ones2", [128, 128], F32)
        ident = sba("ident_m", [128, 128], BF16)
        DM(S, ident[:], C["ident_d"], writes=["ident_m"])
        DM(S, anw[:], anw_d, writes=["anw"])
        DM(S, l1w[:], l1w_d, writes=["l1w"])
        DM(S, l1b[:], l1b_d, writes=["l1b"])
        DM(S, rwf[:], rw_d.rearrange("(c p) e -> p c e", p=128), writes=["rwf"])
        DM(S, rb[:], rb_d, writes=["rb"])
        DM(S, base[:], ecap_d, writes=["base"])
        DM(S, SU[:], su_d, writes=["SU2"])
        DM(S, ones[:], ones_d, writes=["ones2"])
        X(S, "dve", "tensor_copy", ["rwf"], ["rwb"], out=rwb[:], in_=rwf[:])
        X(S, "pool", "memset", [], [f"dest{t}" for t in range(32)], dest_all[:], 0)
        wov = wout_d.rearrange("(c p) f -> p c f", p=128)
        for i in range(4):
            b = i % 2
            DM(S, wst[b][:], wov[:, 2 * i:2 * i + 2, :], writes=[f"wost{b}"])
            if i < 2:
                X(S, "dve", "tensor_tensor", [f"wost{b}", "anw"], ["wo"], out=wo[:, 2 * i:2 * i + 2, :], in0=wst[b][:],
                  in1=anw[:, 2 * i:2 * i + 2].unsqueeze(2).to_broadcast([128, 2, D]), op=ALU.mult)
            else:
                X(S, "dve", "tensor_copy", [f"wost{b}"], ["wo"], out=wo[:, 2 * i:2 * i + 2, :], in_=wst[b][:])
        aT = [sba(f"aT{i}", [128, 4, 128], BF16) for i in range(3)]
        sT = [sba(f"sT{i}", [128, 4, 128], BF16) for i in range(3)]
        xt = [sba(f"xt{i}", [128, D], F32) for i in range(3)]
        gjunk = sba("gjunk", [128, 128], F32)
        identf = sba("identf", [128, 128], F32)
        X(S, "dve", "tensor_copy", ["ident_m"], ["identf"], out=identf[:], in_=ident[:])
        sm = sba("sm", [128, 8], F32)
        sm2 = sba("sm2", [128, 8], F32)
        tt = sba("tmix", [128, D], F32)
        stats = sba("stats", [128, 12], F32)
        mv = sba("mv", [128, 2], F32)
        h1f = [sba(f"h1f{i}", [128, D], F32) for i in range(2)]
        h1b = [sba(f"h1b{i}", [128, D], BF16) for i in range(4)]
        h1T = sba("h1T", [128, D], BF16)
        lg = sba("lg", [128, N_EXP], F32)
        top8 = sba("top8", [128, 8], F32)
        msk = sba("msk", [128, N_EXP], F32)
        ex4 = sba("ex4", [128, 4], F32)
        oh = sba("oh", [128, 4, N_EXP], F32)
        posd = sba("posd", [128, N_EXP], F32)
        destf = sba("destf", [128, 4], F32)
        av = attnT_d.rearrange("(q p) t -> p q t", p=128)
        sv = ssdT_d.rearrange("(q p) t -> p q t", p=128)
        def mix_loads(tg):
            b3 = tg % 3
            tsl = slice(tg * 128, (tg + 1) * 128)
            DM(S, aT[b3][:], av[:, :, tsl], writes=[f"aT{b3}"])
            DM(S, sT[b3][:], sv[:, :, tsl], writes=[f"sT{b3}"])
            DM(S, xt[b3][:], x[tsl, :], writes=[f"xt{b3}"])

        def mix_s1(tg):
            b = tg % 2
            b3 = tg % 3
            tsl = slice(tg * 128, (tg + 1) * 128)
            for q in range(4):
                X(S, "pe", "matmul", [f"aT{b3}"], ["ps6"], psum[6][:, 0:128], lhsT=aT[b3][:, q, :], rhs=aT[b3][:, q, :],
                  start=(q == 0), stop=(q == 3))
            X(S, "dve", "tensor_tensor", ["ps6", "identf"], ["gjunk"], out=gjunk[:], in0=psum[6][:, 0:128], in1=identf[:], op=ALU.mult)
            X(S, "dve", "tensor_reduce", ["gjunk"], ["sm"], out=sm[:, 7:8], in_=gjunk[:], axis=mybir.AxisListType.X, op=ALU.add)
            X(S, "dve", "tensor_scalar", ["sm"], ["sm"], out=sm[:, 0:1], in0=sm[:, 7:8], scalar1=1.0 / 512.0, scalar2=1e-5,
              op0=ALU.mult, op1=ALU.add)
            X(S, "act", "activation", ["sm"], ["sm"], out=sm[:, 1:2], in_=sm[:, 0:1], func=AF.Ln)
            X(S, "act", "activation", ["sm"], ["sm"], out=sm[:, 2:3], in_=sm[:, 1:2], func=AF.Exp, scale=-0.5)
            for hf in range(2):
                for q in range(4):
                    X(S, "pe", "matmul", [f"aT{b3}", "wo"], [f"ps{hf}"], psum[hf][:, :], lhsT=aT[b3][:, q, :],
                      rhs=wo[:, q, hf * 512:(hf + 1) * 512], start=(q == 0), stop=(q == 3))
                for q in range(4):
                    X(S, "pe", "matmul", [f"sT{b3}", "wo"], [f"ps{2 + hf}"], psum[2 + hf][:, :], lhsT=sT[b3][:, q, :],
                      rhs=wo[:, 4 + q, hf * 512:(hf + 1) * 512], start=(q == 0), stop=(q == 3))
            for hf in range(2):
                hs = slice(hf * 512, (hf + 1) * 512)
                X(S, "dve", "scalar_tensor_tensor", [f"xt{b3}", f"ps{2 + hf}"], ["tmix"], out=tt[:, hs], in0=xt[b3][:, hs], scalar=ALPHA,
                  in1=psum[2 + hf][:, :], op0=ALU.mult, op1=ALU.add)
                X(S, "dve", "scalar_tensor_tensor", ["tmix", f"ps{hf}", "sm"], ["tmix"], out=tt[:, hs], in0=psum[hf][:, :],
                  scalar=sm[:, 2:3], in1=tt[:, hs], op0=ALU.mult, op1=ALU.add)
                X(S, "dve", "bn_stats", ["tmix"], ["stats"], out=stats[:, hf * 6:(hf + 1) * 6], in_=tt[:, hs])
            X(S, "dve", "bn_aggr", ["stats"], ["mv"], out=mv[:], in_=stats[:])
            X(S, "act", "activation", ["mv"], ["sm"], out=sm[:, 3:4], in_=mv[:, 1:2], func=AF.Ln, bias=1e-5)
            X(S, "act", "activation", ["sm"], ["sm"], out=sm[:, 4:5], in_=sm[:, 3:4], func=AF.Exp, scale=-0.5)
            X(S, "dve", "tensor_scalar", ["tmix", "mv", "sm"], ["tmix"], out=tt[:], in0=tt[:], scalar1=mv[:, 0:1], scalar2=sm[:, 4:5],
              op0=ALU.subtract, op1=ALU.mult)
            X(S, "dve", "tensor_tensor", ["tmix", "l1w"], ["tmix"], out=tt[:], in0=tt[:], in1=l1w[:], op=ALU.mult)
            X(S, "pool", "tensor_tensor", ["tmix", "l1b"], [f"h1f{b}"], out=h1f[b][:], in0=tt[:], in1=l1b[:], op=ALU.add)
            X(S, "act", "copy", [f"h1f{b}"], [f"h1b{tg % 4}"], out=h1b[tg % 4][:], in_=h1f[b][:])
            DM(S, h1_d[tsl, :], h1f[b][:], reads=[f"h1f{b}"])

        def mix_s2(tg):
            b = tg % 2
            tsl = slice(tg * 128, (tg + 1) * 128)
            for kc in range(8):
                X(S, "pe", "transpose", [f"h1b{tg % 4}", "ident_m"], ["psT"], psT[:, kc * 128:(kc + 1) * 128],
                  in_=h1b[tg % 4][:, kc * 128:(kc + 1) * 128], identity=ident[:])
            X(S, "act", "copy", ["psT"], ["h1T"], out=h1T[:], in_=psT[:, :])
            for kc in range(8):
                X(S, "pe", "matmul", ["h1T", "rwb"], ["ps4"], psum[4][:, 0:N_EXP], lhsT=h1T[:, kc * 128:(kc + 1) * 128],
                  rhs=rwb[:, kc, :], start=(kc == 0), stop=(kc == 7))
            X(S, "dve", "tensor_tensor", ["ps4", "rb"], ["lg"], out=lg[:], in0=psum[4][:, 0:N_EXP], in1=rb[:], op=ALU.add)
            X(S, "dve", "max", ["lg"], ["top8"], out=top8[:], in_=lg[:])
            X(S, "dve", "tensor_scalar", ["lg", "top8"], ["msk"], out=msk[:], in0=lg[:], scalar1=top8[:, 3:4], scalar2=None, op0=ALU.is_ge)
            X(S, "dve", "tensor_scalar", ["top8"], ["sm2"], out=sm2[:, 5:6], in0=top8[:, 0:1], scalar1=-1.0, scalar2=None, op0=ALU.mult)
            X(S, "act", "activation", ["top8", "sm2"], ["ex4"], out=ex4[:], in_=top8[:, 0:4], func=AF.Exp, bias=sm2[:, 5:6])
            X(S, "dve", "tensor_reduce", ["ex4"], ["sm2"], out=sm2[:, 6:7], in_=ex4[:], axis=mybir.AxisListType.X, op=ALU.add)
            X(S, "dve", "reciprocal", ["sm2"], ["sm2"], out=sm2[:, 7:8], in_=sm2[:, 6:7])
            X(S, "dve", "tensor_scalar", ["ex4", "sm2"], ["gates"], out=gates_all[:, tg, :], in0=ex4[:], scalar1=sm2[:, 7:8], scalar2=None,
              op0=ALU.mult)
            X(S, "pe", "matmul", ["SU2", "msk"], ["ps5"], psum[5][:, 0:N_EXP], lhsT=SU[:], rhs=msk[:], start=True, stop=True)
            X(S, "pe", "matmul", ["ones2", "msk"], ["ps5"], psum[5][:, 64:64 + N_EXP], lhsT=ones[:], rhs=msk[:], start=True, stop=True)
            X(S, "dve", "tensor_tensor", ["ps5", "base"], ["posd"], out=posd[:], in0=psum[5][:, 0:N_EXP], in1=base[:], op=ALU.add)
            X(S, "dve", "tensor_tensor", ["ps5", "base"], ["base"], out=base[:], in0=psum[5][:, 64:64 + N_EXP], in1=base[:], op=ALU.add)
            X(S, "dve", "tensor_tensor", ["lg", "top8"], ["oh"], out=oh[:], in0=lg[:].unsqueeze(1).to_broadcast([128, 4, N_EXP]),
              in1=top8[:, 0:4].unsqueeze(2).to_broadcast([128, 4, N_EXP]), op=ALU.is_equal)
            X(S, "dve", "tensor_tensor", ["oh", "posd"], ["oh"], out=oh[:], in0=oh[:],
              in1=posd[:].unsqueeze(1).to_broadcast([128, 4, N_EXP]), op=ALU.mult)
            X(S, "dve", "tensor_reduce", ["oh"], ["destf"], out=destf[:], in_=oh[:], axis=mybir.AxisListType.X, op=ALU.add)
            X(S, "dve", "tensor_copy", ["destf"], [f"dest{tg}"], out=dest_all[:, tg, :], in_=destf[:])

        def mix_scatter(tg):
            b = tg % 4
            for j in range(4):
                idx = dest_all[:, tg, j:j + 1]
                src = h1b[b][:]
                S.dma(lambda e, idx=idx, src=src: e.indirect_dma_start(
                    out=Xg_d, out_offset=bass.IndirectOffsetOnAxis(ap=idx, axis=0), in_=src, in_offset=None,
                    bounds_check=S.bc(e), oob_is_err=False),
                    reads=[f"dest{tg}", f"h1b{b}"], writes=[f"Xg{tg}_{j}"], q="pool")

        mix_loads(0)
        mix_loads(1)
        mix_loads(2)
        mix_s1(0)
        mix_s1(1)
        for tg in range(32):
            if tg + 3 < 32:
                mix_loads(tg + 3)
            if tg + 2 < 32:
                mix_s1(tg + 2)
            mix_s2(tg)
            if tg >= 1:
                mix_scatter(tg - 1)
        mix_scatter(31)
        ecs = sba("ecs", [128, N_EXP], F32)
        cntf = sba("cntf", [128, N_EXP], F32)
        DM(S, ecs[:], ecap_d, writes=["ecs"])
        X(S, "dve", "tensor_tensor", ["base", "ecs"], ["cntf"], out=cntf[:], in0=base[:], in1=ecs[:], op=ALU.subtract)
        flf = sba("flf", [128, N_EXP, 8], F32)
        for t in range(8):
            X(S, "dve", "tensor_scalar", ["cntf"], ["flf"], out=flf[:, :, t], in0=cntf[:], scalar1=float(128 * t), scalar2=None, op0=ALU.is_gt)
        X(S, "dve", "tensor_copy", ["flf"], ["flags"], out=C["flags_all"][:], in_=flf[:].rearrange("p e t -> p (e t)"))
        if C["debug"]:
            dd = dscr("dest_dbg", [128, 128], I32)
            gd = dscr("gates_dbg", [128, 128], F32)
            DM(S, dd, dest_all[:].rearrange("p a b -> p (a b)"), reads=[f"dest{t}" for t in range(32)])
            DM(S, gd, gates_all[:].rearrange("p a b -> p (a b)"), reads=["gates"])
        S.end_phase()


def phase_experts(C):
    nc, S, psum, psT = C["nc"], C["S"], C["psum"], C["psT"]
    din, dscr = C["din"], C["dscr"]
    wg_d = din("w_gate", [N_EXP, D, D])
    wu_d = din("w_up", [N_EXP, D, D])
    wd_d = din("w_down", [N_EXP, D, D])
    bg_d = din("bgT", [128, N_EXP * 8])
    bu_d = din("buT", [128, N_EXP * 8])
    bd_d = din("b_down", [N_EXP, D])
    Yg_d = dscr("Yg_d", [NROWS, D], F32)
    C["Yg_d"] = Yg_d
    Xg_d = C["Xg_d"]
    NT = CAP // 128
    psTs = [psT, psum[6][:, :].bitcast(BF16)]
    psTn = ["psT", "ps6"]
    with contextlib.ExitStack() as ph:
        def sba(name, shape, dt):
            return ph.enter_context(nc.sbuf_tensor(name, list(shape), dt))
        NS = 4
        wsl = [sba(f"wsl{i}", [128, 8, D], BF16) for i in range(NS)]
        wst = [sba(f"west{i}", [128, 2, D], F32) for i in range(2)]
        bg = sba("bg", [128, N_EXP * 8], F32)
        bu = sba("bu", [128, N_EXP * 8], F32)
        bd = [sba(f"bd{i}", [128, D], F32) for i in range(2)]
        ident = sba("ident_e", [128, 128], BF16)
        NXG = 8
        xg = [sba(f"xg{i}", [128, D], BF16) for i in range(NXG)]
        XT = [sba(f"XT{i}", [128, 8, CAP], BF16) for i in range(2)]
        actT = sba("actT", [128, 8, CAP], BF16)
        g1 = [sba(f"g1_{i}", [128, 512], F32) for i in range(2)]
        u1 = [sba(f"u1_{i}", [128, 512], F32) for i in range(2)]
        sg = [sba(f"sg_{i}", [128, 512], F32) for i in range(2)]
        yo = [sba(f"yo{i}", [128, D], F32) for i in range(2)]
        DM(S, ident[:], C["ident_d"], writes=["ident_e"])
        DM(S, bg[:], bg_d, writes=["bg"])
        DM(S, bu[:], bu_d, writes=["bu"])
        st = dict(slot=0, stg=0, xg=0, tp=0)

        def load_w_pieces(src, e):
            sl = st["slot"] % NS
            st["slot"] += 1
            v = src[e].rearrange("(c p) f -> p c f", p=128)

            def piece(i):
                b = st["stg"] % 2
                st["stg"] += 1
                DM(S, wst[b][:], v[:, 2 * i:2 * i + 2, :], writes=[f"west{b}"])
                X(S, "act", "copy", [f"west{b}"], [f"wsl{sl}"], out=wsl[sl][:, 2 * i:2 * i + 2, :], in_=wst[b][:])
            return sl, [lambda i=i: piece(i) for i in range(4)]

        def load_w(src, e):
            sl, ps = load_w_pieces(src, e)
            for p in ps:
                p()
            return sl

        def xg_loads(e):
            for t in range(NT):
                r0 = e * CAP + t * 128
                DM(S, xg[t][:], Xg_d[r0:r0 + 128, :], writes=[f"xg{t}"])

        flags = C["flags_all"]

        def xt_transposes(e, t0, t1):
            xb = e % 2
            for t in range(t0, t1):
                tp = st["tp"] % 2
                st["tp"] += 1
                for kc in range(8):
                    X(S, "pe", "transpose", [f"xg{t}", "ident_e"], [psTn[tp]], psTs[tp][:, kc * 128:(kc + 1) * 128],
                      in_=xg[t][:, kc * 128:(kc + 1) * 128], identity=ident[:])
                pv_ = psTs[tp][:, :].rearrange("p (c t) -> p c t", c=8)
                X(S, "dve", "tensor_copy", [psTn[tp]], [f"XT{xb}"], out=XT[xb][:, :, t * 128:(t + 1) * 128], in_=pv_)

        def fl_(e, t):
            return flags[0:1, e * 8 + t:e * 8 + t + 1]

        def xt_all(e):
            xt_transposes(e, 0, 3)
            for t in range(3, 8):
                S.cond_begin(fl_(e, t))
                xt_transposes(e, t, t + 1)
                S.cond_end()

        def gu_unit_small(e, fc, sg_, su_, xb, n0, nn, q):
            ns = slice(n0, n0 + nn)
            pg, pu = 2 * q, 2 * q + 1
            ar = f"actT{n0 // 256}"
            for kc in range(8):
                X(S, "pe", "matmul", [f"wsl{sg_}", f"XT{xb}"], [f"ps{pg}"], psum[pg][:, 0:nn], lhsT=wsl[sg_][:, kc, fc * 128:(fc + 1) * 128],
                  rhs=XT[xb][:, kc, ns], start=(kc == 0), stop=(kc == 7))
            for kc in range(8):
                X(S, "pe", "matmul", [f"wsl{su_}", f"XT{xb}"], [f"ps{pu}"], psum[pu][:, 0:nn], lhsT=wsl[su_][:, kc, fc * 128:(fc + 1) * 128],
                  rhs=XT[xb][:, kc, ns], start=(kc == 0), stop=(kc == 7))
            bcol = e * 8 + fc
            X(S, "dve", "tensor_scalar", [f"ps{pg}", "bg"], [f"g1_{q}"], out=g1[q][:, 0:nn], in0=psum[pg][:, 0:nn], scalar1=bg[:, bcol:bcol + 1],
              scalar2=7.0, op0=ALU.add, op1=ALU.min)
            X(S, "dve", "tensor_scalar", [f"ps{pu}", "bu"], [f"u1_{q}"], out=u1[q][:, 0:nn], in0=psum[pu][:, 0:nn], scalar1=bu[:, bcol:bcol + 1],
              scalar2=7.0, op0=ALU.add, op1=ALU.min)
            X(S, "dve", "tensor_scalar", [f"u1_{q}"], [f"u1_{q}"], out=u1[q][:, 0:nn], in0=u1[q][:, 0:nn], scalar1=-7.0, scalar2=1.0,
              op0=ALU.max, op1=ALU.add)
            X(S, "act", "activation", [f"g1_{q}"], [f"sg_{q}"], out=sg[q][:, 0:nn], in_=g1[q][:, 0:nn], func=AF.Sigmoid, scale=1.702)
            X(S, "pool", "tensor_tensor", [f"g1_{q}", f"sg_{q}"], [f"sg_{q}"], out=sg[q][:, 0:nn], in0=g1[q][:, 0:nn], in1=sg[q][:, 0:nn], op=ALU.mult)
            X(S, "pool", "tensor_tensor", [f"u1_{q}", f"sg_{q}"], [ar], out=actT[:, fc, ns], in0=sg[q][:, 0:nn], in1=u1[q][:, 0:nn], op=ALU.mult)

        def gu_unit(e, fc, hf, sg_, su_, xb, n0=None, nn=512):
            q = st["ei"] % 2
            st["ei"] += 1
            if n0 is None:
                n0 = hf * 512
            ns = slice(n0, n0 + nn)
            pg, pu = 2 * q, 2 * q + 1
            if nn != 512:
                return gu_unit_small(e, fc, sg_, su_, xb, n0, nn, q)
            for kc in range(8):
                X(S, "pe", "matmul", [f"wsl{sg_}", f"XT{xb}"], [f"ps{pg}"], psum[pg][:, :], lhsT=wsl[sg_][:, kc, fc * 128:(fc + 1) * 128],
                  rhs=XT[xb][:, kc, ns], start=(kc == 0), stop=(kc == 7))
            for kc in range(8):
                X(S, "pe", "matmul", [f"wsl{su_}", f"XT{xb}"], [f"ps{pu}"], psum[pu][:, :], lhsT=wsl[su_][:, kc, fc * 128:(fc + 1) * 128],
                  rhs=XT[xb][:, kc, ns], start=(kc == 0), stop=(kc == 7))
            bcol = e * 8 + fc
            X(S, "dve", "tensor_scalar", [f"ps{pg}", "bg"], [f"g1_{q}"], out=g1[q][:], in0=psum[pg][:, :], scalar1=bg[:, bcol:bcol + 1],
              scalar2=7.0, op0=ALU.add, op1=ALU.min)
            X(S, "dve", "tensor_scalar", [f"ps{pu}", "bu"], [f"u1_{q}"], out=u1[q][:], in0=psum[pu][:, :], scalar1=bu[:, bcol:bcol + 1],
              scalar2=7.0, op0=ALU.add, op1=ALU.min)
            X(S, "dve", "tensor_scalar", [f"u1_{q}"], [f"u1_{q}"], out=u1[q][:], in0=u1[q][:], scalar1=-7.0, scalar2=1.0,
              op0=ALU.max, op1=ALU.add)
            X(S, "act", "activation", [f"g1_{q}"], [f"sg_{q}"], out=sg[q][:], in_=g1[q][:], func=AF.Sigmoid, scale=1.702)
            X(S, "pool", "tensor_tensor", [f"g1_{q}", f"sg_{q}"], [f"sg_{q}"], out=sg[q][:], in0=g1[q][:], in1=sg[q][:], op=ALU.mult)
            X(S, "pool", "tensor_tensor", [f"u1_{q}", f"sg_{q}"], ["actT0", "actT1"], out=actT[:, fc, ns], in0=sg[q][:], in1=u1[q][:], op=ALU.mult)

        def down_tile(e, t, sd_, bb):
            yb_ = t % 2
            for hf in range(2):
                pb = 4 + hf
                for fc in range(8):
                    X(S, "pe", "matmul", [f"wsl{sd_}", f"actT{t // 2}"], [f"ps{pb}"], psum[pb][:, :], lhsT=actT[:, fc, t * 128:(t + 1) * 128],
                      rhs=wsl[sd_][:, fc, hf * 512:(hf + 1) * 512], start=(fc == 0), stop=(fc == 7))
                X(S, "dve", "tensor_tensor", [f"ps{pb}", f"bd{bb}"], [f"yo{yb_}"], out=yo[yb_][:, hf * 512:(hf + 1) * 512], in0=psum[pb][:, :],
                  in1=bd[bb][:, hf * 512:(hf + 1) * 512], op=ALU.add)
            r0 = e * CAP + t * 128
            DM(S, Yg_d[r0:r0 + 128, :], yo[yb_][:], reads=[f"yo{yb_}"], writes=[], q="act")

        st["ei"] = 0
        xg_loads(0)
        sg_, su_ = load_w(wg_d, 0), load_w(wu_d, 0)
        xt_all(0)
        for e in range(N_EXP):
            xb = e % 2
            fl = flags[0:1, e:e + 1]
            if e + 1 < N_EXP:
                xg_loads(e + 1)
            sd_, todo = load_w_pieces(wd_d, e)
            if e + 1 < N_EXP:
                sg_n, todo2 = load_w_pieces(wg_d, e + 1)
                todo = todo + todo2
            bb = e % 2
            DM(S, bd[bb][:], bd_d[e:e + 1, :].partition_broadcast(128), writes=[f"bd{bb}"])
            for fc in range(8):
                gu_unit(e, fc, 0, sg_, su_, xb)
                if todo:
                    todo.pop(0)()
            while todo:
                todo.pop(0)()
            for qq in range(2):
                S.cond_begin(fl_(e, 4 + 2 * qq))
                for fc in range(8):
                    gu_unit(e, fc, 1, sg_, su_, xb, n0=512 + 256 * qq, nn=256)
                S.cond_end()
            todo3 = []
            if e + 1 < N_EXP:
                su_n, todo3 = load_w_pieces(wu_d, e + 1)
                xt_all(e + 1)
            for t in range(3):
                if todo3:
                    todo3.pop(0)()
                down_tile(e, t, sd_, bb)
            while todo3:
                todo3.pop(0)()
            for t in range(3, 8):
                S.cond_begin(fl_(e, t))
                down_tile(e, t, sd_, bb)
                S.cond_end()
            if e + 1 < N_EXP:
                sg_, su_ = sg_n, su_n
        S.end_phase()


def phase_combine(C):
    nc, S = C["nc"], C["S"]
    din, dscr = C["din"], C["dscr"]
    l2w_d = din("ln2w_rep", [128, D])
    l2b_d = din("ln2b_rep", [128, D])
    out_d = nc.dram_tensor("out", [S_TOK, D], F32, kind="ExternalOutput").ap()
    Yg_d, h1_d = C["Yg_d"], C["h1_d"]
    gates_all, dest_all = C["gates_all"], C["dest_all"]
    with contextlib.ExitStack() as ph:
        def sba(name, shape, dt):
            return ph.enter_context(nc.sbuf_tensor(name, list(shape), dt))
        l2w = sba("l2w", [128, D], F32)
        l2b = sba("l2b", [128, D], F32)
        DM(S, l2w[:], l2w_d, writes=["l2w"])
        DM(S, l2b[:], l2b_d, writes=["l2b"])
        yg = [[sba(f"yg{i}_{j}", [128, D], F32) for j in range(4)] for i in range(2)]
        h1 = [sba(f"h1c{i}", [128, D], F32) for i in range(2)]
        acc = sba("cacc", [128, D], F32)
        ot = [sba(f"cot{i}", [128, D], F32) for i in range(2)]
        stats = sba("cstats", [128, 12], F32)
        mv = sba("cmv", [128, 2], F32)
        sm = sba("csm", [128, 4], F32)
        def comb_gather(tg):
            b = tg % 2
            tsl = slice(tg * 128, (tg + 1) * 128)
            DM(S, h1[b][:], h1_d[tsl, :], writes=[f"h1c{b}"])
            for j in range(4):
                idx = dest_all[:, tg, j:j + 1]
                dst = yg[b][j][:]
                S.dma(lambda e, idx=idx, dst=dst: e.indirect_dma_start(
                    out=dst, out_offset=None, in_=Yg_d, in_offset=bass.IndirectOffsetOnAxis(ap=idx, axis=0),
                    bounds_check=S.bc(e), oob_is_err=False),
                    reads=[], writes=[f"yg{b}_{j}"], q="pool")
        comb_gather(0)
        for tg in range(32):
            b = tg % 2
            tsl = slice(tg * 128, (tg + 1) * 128)
            if tg + 1 < 32:
                comb_gather(tg + 1)
            X(S, "dve", "tensor_scalar", [f"h1c{b}"], ["cacc"], out=acc[:], in0=h1[b][:], scalar1=ALPHA, scalar2=None, op0=ALU.mult)
            for j in range(4):
                X(S, "dve", "scalar_tensor_tensor", [f"yg{b}_{j}", "cacc"], ["cacc"], out=acc[:], in0=yg[b][j][:],
                  scalar=gates_all[:, tg, j:j + 1], in1=acc[:], op0=ALU.mult, op1=ALU.add)
            for hf in range(2):
                X(S, "dve", "bn_stats", ["cacc"], ["cstats"], out=stats[:, hf * 6:(hf + 1) * 6], in_=acc[:, hf * 512:(hf + 1) * 512])
            X(S, "dve", "bn_aggr", ["cstats"], ["cmv"], out=mv[:], in_=stats[:])
            X(S, "act", "activation", ["cmv"], ["csm"], out=sm[:, 0:1], in_=mv[:, 1:2], func=AF.Ln, bias=1e-5)
            X(S, "act", "activation", ["csm"], ["csm"], out=sm[:, 1:2], in_=sm[:, 0:1], func=AF.Exp, scale=-0.5)
            X(S, "dve", "tensor_scalar", ["cacc", "cmv", "csm"], ["cacc"], out=acc[:], in0=acc[:], scalar1=mv[:, 0:1], scalar2=sm[:, 1:2],
              op0=ALU.subtract, op1=ALU.mult)
            X(S, "dve", "tensor_tensor", ["cacc", "l2w"], ["cacc"], out=acc[:], in0=acc[:], in1=l2w[:], op=ALU.mult)
            X(S, "dve", "tensor_tensor", ["cacc", "l2b"], [f"cot{b}"], out=ot[b][:], in0=acc[:], in1=l2b[:], op=ALU.add)
            DM(S, out_d[tsl, :], ot[b][:], reads=[f"cot{b}"])
        S.end_phase()


def build(debug=False):
    nc = bass.Bass("TRN2", target_bir_lowering=False)
    es = contextlib.ExitStack()
    with es:
        def din(name, shape, dt=F32):
            return nc.dram_tensor(name, list(shape), dt, kind="ExternalInput").ap()

        def dscr(name, shape, dt, out=False):
            kind = "ExternalOutput" if (out or debug) else "Internal"
            return nc.dram_tensor(name, list(shape), dt, kind=kind).ap()

        xT = din("xT", [D, S_TOK])
        x = din("x", [S_TOK, D])
        w_in = din("w_in_ext", [D, WCOLS])
        cosT = din("cosT", [128, S_TOK])
        sinT = din("sinT", [128, S_TOK])

        qT_d = dscr("qT_d", [512, S_TOK], BF16)
        kT_d = dscr("kT_d", [512, S_TOK], BF16)
        v_d = dscr("v_d", [S_TOK, 512], BF16)
        z_d = dscr("z_d", [S_TOK, 512], F32)
        dt_d = dscr("dt_d", [128, 512], F32)
        xbcT_d = dscr("xbcT_d", [1024, S_TOK + 4], BF16)

        S = Sched(nc, es)
        psum = [es.enter_context(nc.psum_tensor(f"ps{i}", [128, 512], F32)) for i in range(7)]
        psT = es.enter_context(nc.psum_tensor("psT", [128, 1024], BF16))

        def sb(name, shape, dt):
            return es.enter_context(nc.sbuf_tensor(name, list(shape), dt))

        with contextlib.ExitStack() as pa:
            def sba(name, shape, dt):
                return pa.enter_context(nc.sbuf_tensor(name, list(shape), dt))
            wbf = sba("wbf", [128, 8, WCOLS], BF16)
            wst = [sba(f"wst{i}", [128, WCOLS // 2], F32) for i in range(2)]
            cos_sb = sba("cos_sb", [128, S_TOK], F32)
            sin_sb = sba("sin_sb", [128, S_TOK], F32)
            xst = [sba(f"xst{i}", [128, 8, 512], F32) for i in range(2)]
            xb = [sba(f"xb{i}", [128, 8, 512], BF16) for i in range(2)]
            t1 = [sba(f"t1_{i}", [128, 512], F32) for i in range(2)]
            t2 = [sba(f"t2_{i}", [128, 512], F32) for i in range(2)]
            ob = [sba(f"ob{i}", [128, 512], BF16) for i in range(4)]
            of = [sba(f"of{i}", [128, 512], F32) for i in range(2)]
            dts = sba("dts", [128, 512], F32)
            zpad = sba("zpad", [128, 8, 2], BF16)
            qsb = [sba(f"qsb{i}", [128, 512], BF16) for i in range(2)]
            permT = sba("permT_sb", [128, 128], BF16)
            permT_d = din("permT", [128, 128], BF16)
            S.dma(lambda e: e.dma_start(out=permT[:], in_=permT_d), writes=["permT"])
            psT_f = psT[:, :].bitcast(F32)
            psdt = psT_f

            S.dma(lambda e: e.dma_start(out=cos_sb[:], in_=cosT), writes=["cos_sb"])
            S.dma(lambda e: e.dma_start(out=sin_sb[:], in_=sinT), writes=["sin_sb"])
            S.op("pool", lambda e: e.memset(zpad[:], 0.0), writes=["zpad"])
            xbc_rows = xbcT_d.rearrange("(c p) t -> p c t", p=128)
            S.dma(lambda e: e.dma_start(out=xbc_rows[:, :, 0:2], in_=zpad[:]), reads=["zpad"], writes=["xbcpadL"])
            S.dma(lambda e: e.dma_start(out=xbc_rows[:, :, S_TOK + 2:S_TOK + 4], in_=zpad[:]), reads=["zpad"], writes=["xbcpadR"])
            H = WCOLS // 2
            n = 0
            for hf in range(2):
                for kc in range(8):
                    st = wst[n % 2]
                    S.dma(lambda e, st=st, kc=kc, hf=hf: e.dma_start(out=st[:], in_=w_in[kc * 128:(kc + 1) * 128, hf * H:(hf + 1) * H]),
                          writes=[f"wst{n % 2}"])
                    if n % 2 == 0:
                        S.op("dve", lambda e, st=st, kc=kc, hf=hf: e.tensor_copy(out=wbf[:, kc, hf * H:(hf + 1) * H], in_=st[:]),
                             reads=[f"wst{n % 2}"], writes=[f"wbf{kc}_{hf}"])
                    else:
                        S.op("act", lambda e, st=st, kc=kc, hf=hf: e.copy(out=wbf[:, kc, hf * H:(hf + 1) * H], in_=st[:]),
                             reads=[f"wst{n % 2}"], writes=[f"wbf{kc}_{hf}"])
                    n += 1
            def wres_for(kc, c0, c1):
                return [f"wbf{kc}_{hf}" for hf in range(2) if c0 < (hf + 1) * H and c1 > hf * H]
            xT_r = xT.rearrange("(c p) t -> p c t", p=128)
            pb = 0
            obi = 0
            for ch in range(8):
                t0 = ch * 512
                bi = ch % 2
                S.dma(lambda e, bi=bi, t0=t0: e.dma_start(out=xst[bi][:], in_=xT_r[:, :, t0:t0 + 512]), writes=[f"xst{bi}"])
                S.op("dve", lambda e, bi=bi: e.tensor_copy(out=xb[bi][:], in_=xst[bi][:]), reads=[f"xst{bi}"], writes=[f"xb{bi}"])
                xres = f"xb{bi}"

                def fm_tile(j, bank, bi=bi):
                    for kc in range(8):
                        S.op("pe", lambda e, j=j, kc=kc, bank=bank: e.matmul(
                            psum[bank][:, :], lhsT=wbf[:, kc, j * 128:(j + 1) * 128], rhs=xb[bi][:, kc, :],
                            start=(kc == 0), stop=(kc == 7)), reads=[xres] + wres_for(kc, j * 128, (j + 1) * 128), writes=[f"ps{bank}"])
                qk = [(which, dst, j) for which, dst in ((0, qT_d), (4, kT_d)) for j in range(4)]
                banks_q = []
                for idx_, (which, dst, j) in enumerate(qk):
                    ba = pb % 5
                    pb += 1
                    banks_q.append(ba)
                ba0 = banks_q[0]
                fm_tile(qk[0][0] + qk[0][2], ba0)
                for idx_, (which, dst, j) in enumerate(qk):
                    ba = banks_q[idx_]
                    if idx_ + 1 < len(qk):
                        fm_tile(qk[idx_ + 1][0] + qk[idx_ + 1][2], banks_q[idx_ + 1])
                    qs_ = idx_ % 2
                    S.op("act", lambda e, ba=ba, qs_=qs_: e.copy(out=qsb[qs_][:], in_=psum[ba][:, :]),
                         reads=[f"ps{ba}"], writes=[f"qsb{qs_}"])
                    bb = 5 if idx_ % 2 == 0 else 6
                    pbank = psum[bb]
                    S.op("pe", lambda e, pbank=pbank, qs_=qs_: e.matmul(pbank[:, :], lhsT=permT[:], rhs=qsb[qs_][:], start=True, stop=True),
                         reads=[f"qsb{qs_}", "permT"], writes=[f"pq{bb}"])
                    ti = idx_ % 2
                    S.op("dve", lambda e, qs_=qs_, ti=ti, t0=t0: e.tensor_tensor(
                        out=t1[ti][:], in0=qsb[qs_][:], in1=cos_sb[:, t0:t0 + 512], op=ALU.mult),
                        reads=[f"qsb{qs_}", "cos_sb"], writes=[f"t1_{ti}"])
                    S.op("dve", lambda e, pbank=pbank, ti=ti, t0=t0: e.tensor_tensor(
                        out=t2[ti][:], in0=pbank[:, :], in1=sin_sb[:, t0:t0 + 512], op=ALU.mult),
                        reads=[f"pq{bb}", "sin_sb"], writes=[f"t2_{ti}"])
                    o = obi % 4
                    obi += 1
                    S.op("pool", lambda e, ti=ti, o=o: e.tensor_tensor(
                        out=ob[o][:], in0=t1[ti][:], in1=t2[ti][:], op=ALU.add),
                        reads=[f"t1_{ti}", f"t2_{ti}"], writes=[f"ob{o}"])
                    S.dma(lambda e, o=o, j=j, dst=dst, t0=t0: e.dma_start(
                        out=dst[j * 128:(j + 1) * 128, t0:t0 + 512], in_=ob[o][:]), reads=[f"ob{o}"], writes=[])
                for j in range(8):
                    ba = pb % 5
                    pb += 1
                    fm_tile(8 + j, ba)
                    o = obi % 4
                    obi += 1
                    S.op("act", lambda e, ba=ba, o=o: e.copy(out=ob[o][:], in_=psum[ba][:, :]),
                         reads=[f"ps{ba}"], writes=[f"ob{o}"])
                    S.dma(lambda e, o=o, j=j, t0=t0: e.dma_start(
                        out=xbcT_d[j * 128:(j + 1) * 128, 2 + t0:2 + t0 + 512], in_=ob[o][:]), reads=[f"ob{o}"], writes=[])
                for tt in range(4):
                    tg = ch * 4 + tt
                    for which in range(2):
                        ba = pb % 5
                        pb += 1
                        c0 = 2048 + which * 512
                        for kc in range(8):
                            S.op("pe", lambda e, kc=kc, ba=ba, tt=tt, c0=c0, bi=bi: e.matmul(
                                psum[ba][:, :], lhsT=xb[bi][:, kc, tt * 128:(tt + 1) * 128], rhs=wbf[:, kc, c0:c0 + 512],
                                start=(kc == 0), stop=(kc == 7)), reads=[xres] + wres_for(kc, c0, c0 + 512), writes=[f"ps{ba}"])
                        if which == 0:
                            o = obi % 4
                            obi += 1
                            S.op("act", lambda e, ba=ba, o=o: e.copy(out=ob[o][:], in_=psum[ba][:, :]),
                                 reads=[f"ps{ba}"], writes=[f"ob{o}"])
                            S.dma(lambda e, o=o, tg=tg: e.dma_start(out=v_d[tg * 128:(tg + 1) * 128, :], in_=ob[o][:]),
                                  reads=[f"ob{o}"], writes=[])
                        else:
                            o = tg % 2
                            S.op("act", lambda e, ba=ba, o=o: e.copy(out=of[o][:], in_=psum[ba][:, :]),
                                 reads=[f"ps{ba}"], writes=[f"of{o}"])
                            S.dma(lambda e, o=o, tg=tg: e.dma_start(out=z_d[tg * 128:(tg + 1) * 128, :], in_=of[o][:]),
                                  reads=[f"of{o}"], writes=[])
                    for kc in range(8):
                        S.op("pe", lambda e, kc=kc, tt=tt, tg=tg, bi=bi: e.matmul(
                            psdt[:, tg * 16:(tg + 1) * 16], lhsT=xb[bi][:, kc, tt * 128:(tt + 1) * 128],
                            rhs=wbf[:, kc, 3072:3088], start=(kc == 0), stop=(kc == 7)),
                            reads=[xres] + wres_for(kc, 3072, 3088), writes=["psdt"])
            S.op("act", lambda e: e.copy(out=dts[:], in_=psdt[:, :]), reads=["psdt"], writes=["dts"])
            S.dma(lambda e: e.dma_start(out=dt_d, in_=dts[:]), reads=["dts"], writes=[])

            S.end_phase()

        C = dict(nc=nc, S=S, psum=psum, debug=debug, din=din, dscr=dscr)
        C.update(qT_d=qT_d, kT_d=kT_d, v_d=v_d, z_d=z_d, dt_d=dt_d, xbcT_d=xbcT_d, x=x)
        C["psT"] = psT
        phase_attn(C)
        phase_conv(C)
        phase_ssd(C)
        C["gates_all"] = es.enter_context(nc.sbuf_tensor("gates_all", [128, 32, 4], F32))
        C["dest_all"] = es.enter_context(nc.sbuf_tensor("dest_all", [128, 32, 4], I32))
        C["flags_all"] = es.enter_context(nc.sbuf_tensor("flags_all", [128, N_EXP * 8], I32))
        phase_mix(C)
        phase_experts(C)
        phase_combine(C)
        S.streams["sp"].append(("wait", "c_pe", S.cnt["pe"])) if False else None
        S.finish()
        S.emit()
    return nc


_CACHE = {}


def kernel(**inputs):
    debug = bool(inputs.pop("_debug", False))
    x = np.asarray(inputs["x"], dtype=np.float32)
    w_in = np.asarray(inputs["w_in"], dtype=np.float32)[0]
    perm = np.concatenate([np.concatenate([np.arange(32, 64), np.arange(0, 32)]) + 64 * h for h in range(8)])
    wq, wk, wv = w_in[:, 0:512], w_in[:, 512:1024], w_in[:, 1024:1536]
    wz, wxbc, wdt = w_in[:, 1536:2048], w_in[:, 2048:3072], w_in[:, 3072:3088]
    w_ext = np.ascontiguousarray(np.concatenate([wq, wk, wxbc, wv, wz, wdt], axis=1))
    consts = host_consts()
    g = lambda k: np.asarray(inputs[k], dtype=np.float32)[0]
    rep = lambda v: np.ascontiguousarray(np.broadcast_to(v[None, :], (128, v.shape[0])))
    consts["conv_wT"] = np.ascontiguousarray(g("conv_w").reshape(5, 8, 128).transpose(2, 1, 0).reshape(128, 40))
    consts["conv_bT"] = np.ascontiguousarray(g("conv_b").reshape(8, 128).T)
    consts["conv_b_row"] = np.ascontiguousarray(g("conv_b").reshape(1, 1024))
    consts["dtb_rep"] = rep(np.tile(np.concatenate([g("dt_bias_fwd"), g("dt_bias_bwd")]), 32))
    consts["alog_rep"] = rep(np.tile(np.concatenate([g("a_log_fwd"), g("a_log_bwd")]), 32))
    consts["dskip_rep"] = rep(np.repeat(g("d_skip"), 64))
    consts["ssdnw_rep"] = rep(g("ssd_norm_w"))
    consts["w_out"] = g("w_out")
    consts["anwT"] = np.ascontiguousarray(g("attn_norm_w").reshape(4, 128).T)
    consts["ln1w_rep"] = rep(g("ln1_w"))
    consts["ln1b_rep"] = rep(g("ln1_b"))
    consts["router_w"] = g("router_w")
    consts["rb_rep"] = rep(g("router_b"))
    consts["ecap_rep"] = rep((np.arange(N_EXP) * CAP).astype(np.float32))
    consts["w_gate"] = g("w_gate")
    consts["w_up"] = g("w_up")
    consts["w_down"] = g("w_down")
    consts["bgT"] = np.ascontiguousarray(g("b_gate").reshape(N_EXP, 8, 128).transpose(2, 0, 1).reshape(128, N_EXP * 8))
    consts["buT"] = np.ascontiguousarray(g("b_up").reshape(N_EXP, 8, 128).transpose(2, 0, 1).reshape(128, N_EXP * 8))
    consts["b_down"] = g("b_down")
    consts["ln2w_rep"] = rep(g("ln2_w"))
    consts["ln2b_rep"] = rep(g("ln2_b"))
    consts["c_SU2"] = consts["c_SU"]
    consts["c_ones2"] = consts["c_ones"]
    nc = build(debug=debug)
    in_maps = []
    for b in range(8):
        m = {"xT": np.ascontiguousarray(x[b].T), "x": np.ascontiguousarray(x[b]), "w_in_ext": w_ext}
        m.update(consts)
        in_maps.append(m)
    ncores = int(inputs.pop("_ncores", 8)) if "_ncores" in inputs else 8
    res = run_bass_kernel_spmd(nc, in_maps[:ncores], core_ids=list(range(ncores)))
    if debug:
        return res.results
    return np.stack([r["out"] for r in res.results], axis=0)
```
